# Optimizing a Trainium2 kernel written in Bass

```python
import math
import jax
import jax.numpy as jnp
from jax import lax
import numpy as np

D_MODEL = 4096
BATCH = 4
SEQ = 4096
DEPTH = 1

GRID_W = 64
CTX_LEN = 256

DA_HEADS = 8
DA_HEAD_DIM = 128
DA_WIDTH = DA_HEADS * 2 * DA_HEAD_DIM

MLA_HEADS = 16
Q_LORA = 1024
KV_LORA = 512
QK_NOPE = 128
QK_ROPE = 64
V_DIM = 128
MLA_WIDTH = MLA_HEADS * V_DIM

SPLIT_SIZES = (DA_WIDTH, DA_WIDTH, DA_WIDTH, Q_LORA, KV_LORA + QK_ROPE, D_MODEL, D_MODEL)
IN_COLS = 2048 * 3 + Q_LORA + KV_LORA + QK_ROPE + 2 * D_MODEL

N_GROUPS = 8
EXPERTS_PER_GROUP = 8
N_EXPERTS = N_GROUPS * EXPERTS_PER_GROUP
TOP_K = 2
D_EXPERT = 512
EXPERT_BLOCK = 128

Q_BLOCK = 128
ROPE_BASE = 10000.0
EPS = 1e-6

kernel_name = 'hybrid_diff_mla_hmoe_dit_block'


def _rmsnorm(x, g):
    xf = x.astype(jnp.float32)
    y = xf * lax.rsqrt(jnp.mean(xf * xf, axis=-1, keepdims=True) + EPS)
    return (y * g.astype(jnp.float32)).astype(x.dtype)


def _axial_angles(n, rot_dim):
    quarter = rot_dim // 4
    inv_freq = ROPE_BASE ** (-jnp.arange(quarter, dtype=jnp.float32) / quarter)
    rows = n // GRID_W
    row = jnp.repeat(jnp.arange(rows, dtype=jnp.float32), GRID_W)
    col = jnp.tile(jnp.arange(GRID_W, dtype=jnp.float32), rows)
    return jnp.concatenate([row[:, None] * inv_freq, col[:, None] * inv_freq], axis=-1)


def _rope(x, ang):
    ang = ang.reshape((1, ang.shape[0]) + (1,) * (x.ndim - 3) + (ang.shape[-1],))
    cos, sin = jnp.cos(ang), jnp.sin(ang)
    xf = x.astype(jnp.float32)
    x1, x2 = jnp.split(xf, 2, axis=-1)
    return jnp.concatenate([x1 * cos - x2 * sin, x1 * sin + x2 * cos], axis=-1).astype(x.dtype)


def _sweep_queries(fn, *q_arrays):
    b, n = q_arrays[0].shape[:2]
    nb = n // Q_BLOCK
    blocks = tuple(jnp.swapaxes(q.reshape((b, nb, Q_BLOCK) + q.shape[2:]), 0, 1) for q in q_arrays)
    out = lax.map(lambda qs: fn(*qs), blocks)
    out = jnp.swapaxes(out, 0, 1)
    return out.reshape((b, n) + out.shape[3:])


def _features(proj, g_q, w_uq, g_kv, w_ukv, ang_da, ang_mla):
    b, n, _ = proj.shape
    points, acc = [], 0
    for s in SPLIT_SIZES[:-1]:
        acc += s
        points.append(acc)
    dq, dk, dv, cq, ckv, gate_a, gate_b = jnp.split(proj, points, axis=-1)
    dq = dq.reshape(b, n, 2, DA_HEADS, DA_HEAD_DIM)
    dk = dk.reshape(b, n, 2, DA_HEADS, DA_HEAD_DIM)
    dv = dv.reshape(b, n, DA_HEADS, 2 * DA_HEAD_DIM)
    q = (_rmsnorm(cq, g_q) @ w_uq).reshape(b, n, MLA_HEADS, QK_NOPE + QK_ROPE)
    qn, qr = q[..., :QK_NOPE], q[..., QK_NOPE:]
    kv = (_rmsnorm(ckv[..., :KV_LORA], g_kv) @ w_ukv).reshape(b, n, MLA_HEADS, QK_NOPE + V_DIM)
    kn, mv = kv[..., :QK_NOPE], kv[..., QK_NOPE:]
    kr = ckv[..., KV_LORA:]
    if ang_da is not None:
        dq, dk = _rope(dq, ang_da), _rope(dk, ang_da)
        qr, kr = _rope(qr, ang_mla), _rope(kr, ang_mla)
    return (dq, qn, qr, gate_a, gate_b), (dk, dv, kn, kr, mv)


def _mix(queries, keys, lam, lambda_init, g_subln, w_br_a, w_br_b, w_out):
    dq, qn, qr, gate_a, gate_b = queries
    dk, dv, kn, kr, mv = (k.astype(jnp.float32) for k in keys)
    b, n = dq.shape[:2]
    dt = gate_a.dtype

    def diff_block(q_b):
        s = jnp.einsum('bqmhd,bkmhd->bmhqk', q_b.astype(jnp.float32), dk) * (DA_HEAD_DIM ** -0.5)
        p = jax.nn.softmax(s, axis=-1)
        a = p[:, 0] - lam * p[:, 1]
        return jnp.einsum('bhqk,bkhe->bqhe', a, dv)

    o_a = _sweep_queries(diff_block, dq)
    o_a = _rmsnorm(o_a, g_subln) * (1.0 - lambda_init)

    def mla_block(qn_b, qr_b):
        s = (jnp.einsum('bqhd,bkhd->bhqk', qn_b.astype(jnp.float32), kn)
             + jnp.einsum('bqhr,bkr->bhqk', qr_b.astype(jnp.float32), kr)) * ((QK_NOPE + QK_ROPE) ** -0.5)
        p = jax.nn.softmax(s, axis=-1)
        return jnp.einsum('bhqk,bkhd->bqhd', p, mv)

    o_b = _sweep_queries(mla_block, qn, qr)
    y_a = o_a.reshape(b, n, DA_WIDTH).astype(dt) @ w_br_a
    y_b = o_b.reshape(b, n, MLA_WIDTH).astype(dt) @ w_br_b
    merged = jax.nn.sigmoid(gate_a) * y_a + jax.nn.sigmoid(gate_b) * y_b
    return merged @ w_out


def _moe(h, w_grp, b_grp, w_exp, b_exp, w1, w3, w2):
    b, n, d = h.shape
    t = h.reshape(-1, d)
    n_tok = t.shape[0]
    tf = t.astype(jnp.float32)
    grp_p = jax.nn.softmax(tf @ w_grp.astype(jnp.float32) + b_grp.astype(jnp.float32), axis=-1)
    g_idx = jnp.argmax(grp_p, axis=-1)
    g_w = jnp.take_along_axis(grp_p, g_idx[:, None], axis=1)[:, 0]
    e_logits = (tf @ w_exp.astype(jnp.float32) + b_exp.astype(jnp.float32)).reshape(n_tok, N_GROUPS, EXPERTS_PER_GROUP)
    sel = jnp.take_along_axis(e_logits, g_idx[:, None, None], axis=1)[:, 0]
    top_p, top_i = lax.top_k(jax.nn.softmax(sel, axis=-1), TOP_K)
    weights = g_w[:, None] * top_p / jnp.sum(top_p, axis=-1, keepdims=True)
    expert_ids = g_idx[:, None] * EXPERTS_PER_GROUP + top_i

    n_asg = n_tok * TOP_K
    eid = expert_ids.reshape(-1).astype(jnp.int32)
    wts = weights.reshape(-1)
    tok = jnp.repeat(jnp.arange(n_tok, dtype=jnp.int32), TOP_K)
    order = jnp.argsort(eid)
    e_s, tok_s, w_s = eid[order], tok[order], wts[order]
    counts = jnp.bincount(eid, length=N_EXPERTS)
    starts = jnp.cumsum(counts) - counts
    pcounts = (counts + EXPERT_BLOCK - 1) // EXPERT_BLOCK * EXPERT_BLOCK
    pends = jnp.cumsum(pcounts)
    pstarts = pends - pcounts
    pos = pstarts[e_s] + jnp.arange(n_asg, dtype=jnp.int32) - starts[e_s]
    nb = -(-n_asg // EXPERT_BLOCK) + N_EXPERTS
    n_pad = nb * EXPERT_BLOCK
    tok_pad = jnp.full((n_pad,), n_tok, jnp.int32).at[pos].set(tok_s)
    w_pad = jnp.zeros((n_pad,), jnp.float32).at[pos].set(w_s)
    blk_e = jnp.minimum(jnp.searchsorted(pends, jnp.arange(nb, dtype=pends.dtype) * EXPERT_BLOCK, side='right'),
                        N_EXPERTS - 1)
    t_pad = jnp.concatenate([t, jnp.zeros((1, d), t.dtype)], axis=0)

    def one_block(args):
        tok_b, w_b, e = args
        xb = t_pad[tok_b]
        hid = jax.nn.silu(xb @ w1[e]) * (xb @ w3[e])
        return (hid @ w2[e]) * w_b[:, None].astype(t.dtype)

    y = lax.map(one_block, (tok_pad.reshape(nb, EXPERT_BLOCK), w_pad.reshape(nb, EXPERT_BLOCK), blk_e))
    out = jnp.zeros((n_tok + 1, d), y.dtype).at[tok_pad].add(y.reshape(n_pad, d))
    return out[:n_tok].reshape(b, n, d)


def setup_inputs(seed: int = 0) -> dict:
    key = jax.random.key(seed)
    ks = jax.random.split(key, 32)
    f32 = jnp.float32
    L, D = DEPTH, D_MODEL

    def nrm(k, shape, scale):
        return jax.random.normal(k, shape, f32) * scale

    def gain(k, shape):
        return 1.0 + 0.05 * jax.random.normal(k, shape, f32)

    return {
        'x': nrm(ks[0], (BATCH, SEQ, D), 1.0),
        'c': nrm(ks[1], (BATCH, D), 1.0),
        'ctx': nrm(ks[2], (BATCH, CTX_LEN, D), 1.0),
        'c_ctx': nrm(ks[3], (D,), 1.0),
        'w_mod': nrm(ks[4], (L, D, 6 * D), 0.5 * D ** -0.5),
        'b_mod': nrm(ks[5], (L, 6 * D), 0.02),
        'g_attn': gain(ks[6], (L, D)),
        'w_in': nrm(ks[7], (L, D, IN_COLS), D ** -0.5),
        'g_q': gain(ks[8], (L, Q_LORA)),
        'w_uq': nrm(ks[9], (L, Q_LORA, MLA_HEADS * (QK_NOPE + QK_ROPE)), Q_LORA ** -0.5),
        'g_kv': gain(ks[10], (L, KV_LORA)),
        'w_ukv': nrm(ks[11], (L, KV_LORA, MLA_HEADS * (QK_NOPE + V_DIM)), KV_LORA ** -0.5),
        'lam_q1': nrm(ks[12], (L, DA_HEAD_DIM), 0.1),
        'lam_k1': nrm(ks[13], (L, DA_HEAD_DIM), 0.1),
        'lam_q2': nrm(ks[14], (L, DA_HEAD_DIM), 0.1),
        'lam_k2': nrm(ks[15], (L, DA_HEAD_DIM), 0.1),
        'g_subln': gain(ks[16], (L, 2 * DA_HEAD_DIM)),
        'w_br_a': nrm(ks[17], (L, DA_WIDTH, D), DA_WIDTH ** -0.5),
        'w_br_b': nrm(ks[18], (L, MLA_WIDTH, D), MLA_WIDTH ** -0.5),
        'w_out': nrm(ks[19], (L, D, D), D ** -0.5),
        'g_ffn': gain(ks[20], (L, D)),
        'w_grp': nrm(ks[21], (L, D, N_GROUPS), D ** -0.5),
        'b_grp': nrm(ks[22], (L, N_GROUPS), 0.01),
        'w_exp': nrm(ks[23], (L, D, N_EXPERTS), D ** -0.5),
        'b_exp': nrm(ks[24], (L, N_EXPERTS), 0.01),
        'w1': nrm(ks[25], (L, N_EXPERTS, D, D_EXPERT), D ** -0.5),
        'w3': nrm(ks[26], (L, N_EXPERTS, D, D_EXPERT), D ** -0.5),
        'w2': nrm(ks[27], (L, N_EXPERTS, D_EXPERT, D), D_EXPERT ** -0.5),
        'g_final': gain(ks[28], (D,)),
    }


def reference(x, c, ctx, c_ctx, w_mod, b_mod, g_attn, w_in, g_q, w_uq, g_kv, w_ukv,
              lam_q1, lam_k1, lam_q2, lam_k2, g_subln, w_br_a, w_br_b, w_out,
              g_ffn, w_grp, b_grp, w_exp, b_exp, w1, w3, w2, g_final):
    n = x.shape[1]
    ang_da = _axial_angles(n, DA_HEAD_DIM)
    ang_mla = _axial_angles(n, QK_ROPE)
    silu_c = jax.nn.silu(c)
    silu_cc = jax.nn.silu(c_ctx)
    for l in range(DEPTH):
        lambda_init = 0.8 - 0.6 * math.exp(-0.3 * l)
        mod_x = (silu_c @ w_mod[l] + b_mod[l])[:, None, :]
        sh1, sc1, gt1, sh2, sc2, gt2 = jnp.split(mod_x, 6, axis=-1)
        mod_c = silu_cc @ w_mod[l] + b_mod[l]
        csh1, csc1, cgt1, csh2, csc2, cgt2 = jnp.split(mod_c, 6, axis=-1)
        lam = (jnp.exp(jnp.sum(lam_q1[l].astype(jnp.float32) * lam_k1[l].astype(jnp.float32)))
               - jnp.exp(jnp.sum(lam_q2[l].astype(jnp.float32) * lam_k2[l].astype(jnp.float32)))
               + lambda_init)

        h_x = _rmsnorm(x, g_attn[l]) * (1 + sc1) + sh1
        h_c = _rmsnorm(ctx, g_attn[l]) * (1 + csc1) + csh1
        q_x, k_x = _features(h_x @ w_in[l], g_q[l], w_uq[l], g_kv[l], w_ukv[l], ang_da, ang_mla)
        q_c, k_c = _features(h_c @ w_in[l], g_q[l], w_uq[l], g_kv[l], w_ukv[l], None, None)
        keys_x = tuple(jnp.concatenate([kc, kx], axis=1) for kc, kx in zip(k_c, k_x))
        mix_args = (lam, lambda_init, g_subln[l], w_br_a[l], w_br_b[l], w_out[l])
        moe_args = (w_grp[l], b_grp[l], w_exp[l], b_exp[l], w1[l], w3[l], w2[l])

        x = x + gt1 * _mix(q_x, keys_x, *mix_args)
        x = x + gt2 * _moe(_rmsnorm(x, g_ffn[l]) * (1 + sc2) + sh2, *moe_args)
        if l < DEPTH - 1:
            ctx = ctx + cgt1 * _mix(q_c, k_c, *mix_args)
            ctx = ctx + cgt2 * _moe(_rmsnorm(ctx, g_ffn[l]) * (1 + csc2) + csh2, *moe_args)
    return _rmsnorm(x, g_final)
```

```python
import math
from contextlib import ExitStack
import numpy as np
import concourse.bass as bass
import concourse.mybir as mybir
from concourse.bass_utils import run_bass_kernel_spmd

F32 = mybir.dt.float32
BF16 = mybir.dt.bfloat16
I32 = mybir.dt.int32
AF = mybir.ActivationFunctionType
ALU = mybir.AluOpType
AX = mybir.AxisListType


class Cfg:
    def __init__(self, **kw):
        self.D = 4096; self.B = 4; self.S = 4096; self.GW = 64; self.C = 256
        self.DAH = 8; self.MH = 16; self.QL = 1024; self.KVL = 512
        self.NG = 8; self.EPG = 8; self.DE = 512; self.NCORES = 8
        self.lambda_init = 0.8 - 0.6 * math.exp(-0.3 * 0)
        for k, v in kw.items():
            setattr(self, k, v)
        self.KD = self.D // 128
        self.NQ = self.S // 2
        self.NK = self.S + self.C
        self.NE = self.NG * self.EPG
        self.DAW = self.DAH * 256
        self.MW = self.MH * 128
        self.INC = 3 * self.DAW + self.QL + self.KVL + 64 + 2 * self.D
        self.o_dq = 0; self.o_dk = self.DAW; self.o_dv = 2 * self.DAW
        self.o_cq = 3 * self.DAW; self.o_ckv = self.o_cq + self.QL
        self.o_ga = self.o_ckv + self.KVL + 64; self.o_gb = self.o_ga + self.D


class Buf:
    def __init__(self, name, sem=None):
        self.name = name
        self.w = {}
        self.r = {}
        self.sem = sem
        self.ndma = 0


def _merge(d, s):
    for k, v in s.items():
        if d.get(k, 0) < v:
            d[k] = v


class Eng:
    def __init__(self, sched, name, e, sem):
        self.s = sched; self.name = name; self.e = e; self.sem = sem
        self.cnt = 0
        self.seen = {}
        self.pend_r = []; self.pend_w = []

    def wait(self, deps):
        for k, v in deps.items():
            if self.seen.get(k, 0) < v:
                self.e.wait_ge(self.s.sems[k], v)
                self.seen[k] = v

    def _deps(self, reads, writes, partial):
        deps = {}
        for t in reads:
            _merge(deps, t.w)
        for t in writes:
            _merge(deps, t.r)
            if not partial:
                _merge(deps, t.w)
        return deps

    def op(self, fn, reads=(), writes=(), partial=False, signal=True):
        self.wait(self._deps(reads, writes, partial))
        ins = fn(self.e)
        self.pend_r += list(reads); self.pend_w += list(writes)
        if signal:
            self.cnt += 1
            ins.then_inc(self.sem_h, 1)
            ev = {self.sem: self.cnt}
            self.s.last[self.sem] = self.cnt
            for t in self.pend_r:
                _merge(t.r, ev)
            for t in self.pend_w:
                _merge(t.w, ev)
            self.pend_r = []; self.pend_w = []
        return ins

    def dma(self, fn, src, dst, via, partial=True, inc=16):
        self.wait(self._deps([src], [dst], partial))
        ins = fn(self.e)
        k = self.s.sem_of(via, self.name == 'pool')
        self.s.semcnt[k] = self.s.semcnt.get(k, 0) + inc
        ins.then_inc(self.s.sems[k], inc)
        ev = {k: self.s.semcnt[k]}
        self.s.last[k] = self.s.semcnt[k]
        _merge(src.r, ev)
        _merge(dst.w, ev)
        return ins


class Sched:
    def __init__(self, nc, stack):
        self.nc = nc
        self.sems = {}
        self.last = {}
        self.stack = stack
        self.n = 0
        self.dpool = []
        self.dpool_sw = []
        self.n_sw = 0
        self.semcnt = {}
        self.pe = self._eng("pe", nc.tensor)
        self.act = self._eng("act", nc.scalar)
        self.dve = self._eng("dve", nc.vector)
        self.pool = self._eng("pool", nc.gpsimd)
        self.sp = self._eng("sp", nc.sync)
        self.engs = [self.pe, self.act, self.dve, self.pool, self.sp]
        self.ccsem = self._newsem('ccsem'); self.ccn = 0

    def _newsem(self, name):
        h = self.stack.enter_context(self.nc.semaphore(name))
        k = name
        self.sems[k] = h
        return k

    def _eng(self, name, e):
        k = self._newsem("e_" + name)
        en = Eng(self, name, e, k)
        en.sem_h = self.sems[k]
        return en

    def sem_of(self, buf, sw=False):
        key = "sem_sw" if sw else "sem"
        if getattr(buf, key, None) is None:
            pl = self.dpool_sw if sw else self.dpool
            if len(pl) < 40:
                pl.append(self._newsem(("s%d" if sw else "d%d") % len(pl)))
            cnt = self.n_sw if sw else self.n
            setattr(buf, key, pl[cnt % 40])
            if sw:
                self.n_sw += 1
            else:
                self.n += 1
        return getattr(buf, key)

    def barrier(self, engs=None):
        for en in (engs or self.engs):
            en.wait(dict(self.last))


class K:
    def __init__(self, cfg, gather):
        self.cfg = cfg
        self.gather = gather
        self.nc = bass.Bass("TRN2", target_bir_lowering=False)
        self.ins = {}
        self.dbufs = {}

    def dram(self, name, shape, dt, kind="Internal"):
        t = self.nc.dram_tensor(name, list(shape), dt, kind=kind).ap()
        self.dbufs[name] = Buf(name)
        return t

    def db(self, name):
        return self.dbufs[name]


def build_program(cfg, gather=True, upto=99, debug_outs=()):
    k = K(cfg, gather)
    nc = k.nc
    c = cfg
    D, KD, NQ, NK, S, C = c.D, c.KD, c.NQ, c.NK, c.S, c.C
    with ExitStack() as top:
        sc = Sched(nc, top)
        pe, act, dve, pool, sp = sc.pe, sc.act, sc.dve, sc.pool, sc.sp
        _build(k, sc, top, upto, debug_outs)
    return nc


def _build(k, sc, top, upto, debug_outs):
    nc = k.nc; c = k.cfg
    D, KD, NQ, NK, S, C = c.D, c.KD, c.NQ, c.NK, c.S, c.C
    pe, act, dve, pool, sp = sc.pe, sc.act, sc.dve, sc.pool, sc.sp
    dbg = lambda n: ("ExternalOutput" if n in debug_outs else "Internal")

    def inp(name, shape, dt=F32):
        t = nc.dram_tensor(name, list(shape), dt, kind="ExternalInput").ap()
        k.dbufs[name] = Buf(name)
        return t

    def scratch(name, shape, dt):
        return k.dram(name, shape, dt, kind=dbg(name))

    def weight(name, rows, cols):
        if not k.gather:
            return inp(name, [rows, cols])
        r8 = rows // 8
        u = gather_unit(rows, cols); nch = r8 // u
        sh = inp(name + "_sh", [r8, cols])
        shi = k.dram(name + "_shi", [r8, cols], F32)
        pair = k.dram(name + "_pr", [2 * r8, cols], F32)
        full = k.dram(name, [rows, cols], F32)
        hc = Buf("cp_" + name)
        sp.dma(lambda e: e.dma_start(out=shi, in_=sh), k.db(name + "_sh"), k.db(name + "_shi"), hc)
        pool.wait(dict(k.db(name + "_shi").w))
        s1 = []
        for i in range(nch):
            ins = pool.e.collective_compute("AllGather", ALU.bypass, replica_groups=[[0, 4], [1, 5], [2, 6], [3, 7]],
                                            ins=[shi[i * u:(i + 1) * u, :]], outs=[pair[i * 2 * u:(i + 1) * 2 * u, :]])
            sc.ccn += 1
            ins.then_inc(sc.sems[sc.ccsem], 1)
            s1.append(sc.ccn)
        for i in range(nch):
            pool.wait({sc.ccsem: s1[i]})
            ins = pool.e.collective_compute("AllGather", ALU.bypass, replica_groups=[[0, 1, 2, 3], [4, 5, 6, 7]],
                                            ins=[pair[i * 2 * u:(i + 1) * 2 * u, :]], outs=[full[i * 8 * u:(i + 1) * 8 * u, :]])
            sc.ccn += 1
            ins.then_inc(sc.sems[sc.ccsem], 1)
        sc.last[sc.ccsem] = sc.ccn
        _merge(k.db(name).w, {sc.ccsem: sc.ccn})
        return full

    xk = inp("xk", [NK, D])
    c_b = inp("c_b", [1, D]); c_ctx = inp("c_ctx", [1, D])
    b_mod = inp("b_mod", [1, 6 * D]); g_attn = inp("g_attn", [1, D])
    out = nc.dram_tensor("out", [NQ, D], F32, kind="ExternalOutput").ap(); k.dbufs["out"] = Buf("out")
    w_mod_p = [weight("w_mod_%d" % i, D, 3 * D) for i in range(2)]

    uid = [0]
    def S_(name, shape, dt, st):
        uid[0] += 1
        return st.enter_context(nc.sbuf_tensor("%s_%d" % (name, uid[0]), list(shape), dt))
    def P_(name, shape, dt, st):
        uid[0] += 1
        return st.enter_context(nc.psum_tensor("%s_%d" % (name, uid[0]), list(shape), dt))

    mod_bc = scratch("mod_bc", [128, 4 * D], F32)
    A1T = S_("A1T", [128, KD, 2], F32, top); B1T = S_("B1T", [128, KD, 2], F32, top)
    bA1T = Buf("A1T"); bB1T = Buf("B1T")
    ones_f = S_("ones_f", [128, 128], F32, top); b_ones = Buf("ones_f")
    dve.op(lambda e: e.memset(ones_f[:], 1.0), writes=[b_ones])

    with ExitStack() as ph:
        GC = 256
        cT = S_("cT", [128, KD, 2], F32, ph); bcT = Buf("cT")
        sT = S_("sT", [128, KD, 2], F32, ph); bsT = Buf("sT")
        srep = S_("srep", [128, KD, 128], F32, ph); bsrep = Buf("srep")
        bmT = S_("bmT", [128, 2 * KD], F32, ph); bbmT = Buf("bmT")
        gaT = S_("gaT", [128, KD], F32, ph); bgaT = Buf("gaT")
        brow = S_("brow", [1, 4 * D], F32, ph); bbrow = Buf("brow")
        modT = S_("modT", [128, 2 * KD, 2], F32, ph); bmodT = Buf("modT")
        wts = [S_("wm%d" % i, [128, KD, GC], F32, ph) for i in range(2)]; bwts = [Buf("wm%d" % i) for i in range(2)]
        evs = [S_("mev%d" % i, [128, GC], F32, ph) for i in range(2)]; bevs = [Buf("mev%d" % i) for i in range(2)]
        pss = [P_("mps%d" % i, [128, 512], F32, ph) for i in range(2)]; bpss = [Buf("mps%d" % i) for i in range(2)]
        sp.dma(lambda e: e.dma_start(out=cT[:, :, 0], in_=c_b.rearrange("o (k p) -> p (o k)", p=128), allow_slow_non_contiguous=True),
               k.db("c_b"), bcT, bcT)
        sp.dma(lambda e: e.dma_start(out=cT[:, :, 1], in_=c_ctx.rearrange("o (k p) -> p (o k)", p=128), allow_slow_non_contiguous=True),
               k.db("c_ctx"), bcT, bcT)
        sp.dma(lambda e: e.dma_start(out=bmT[:], in_=b_mod[:, 0:2 * D].rearrange("o (k p) -> p (o k)", p=128), allow_slow_non_contiguous=True),
               k.db("b_mod"), bbmT, bbmT)
        sp.dma(lambda e: e.dma_start(out=gaT[:], in_=g_attn.rearrange("o (k p) -> p (o k)", p=128), allow_slow_non_contiguous=True),
               k.db("g_attn"), bgaT, bgaT)
        sp.dma(lambda e: e.dma_start(out=brow[:], in_=b_mod[:, 2 * D:6 * D]), k.db("b_mod"), bbrow, bbrow)
        act.op(lambda e: e.activation(out=sT[:], in_=cT[:], func=AF.Silu), reads=[bcT], writes=[bsT])
        for kc in range(KD):
            dve.op(lambda e: e.tensor_copy(out=srep[:, kc, :], in_=sT[:, kc, 0:1].to_broadcast([128, 128])),
                   reads=[bsT], writes=[bsrep], partial=True)
        ng = 6 * D // GC
        for g in range(ng):
            wt = wts[g % 2]; bwt = bwts[g % 2]; ps = pss[g % 2]; bps = bpss[g % 2]
            pi_ = (g * GC) // (3 * D); pc0 = g * GC - pi_ * 3 * D
            sp.dma(lambda e: e.dma_start(out=wt[:], in_=w_mod_p[pi_][:, pc0:pc0 + GC].rearrange("(k p) n -> p k n", p=128)),
                   k.db("w_mod_%d" % pi_), bwt, bwt, partial=False)
            if g * GC < 2 * D:
                for j in range(GC // 128):
                    for kc in range(KD):
                        pe.op(lambda e: e.matmul(ps[:, j * 2:j * 2 + 2], lhsT=wt[:, kc, j * 128:(j + 1) * 128], rhs=sT[:, kc, :],
                                                 start=(kc == 0), stop=(kc == KD - 1)),
                              reads=[bwt, bsT], writes=[bps], partial=(kc > 0 or j > 0), signal=(kc == KD - 1 and j == GC // 128 - 1))
                ch0 = g * GC // 128
                dve.op(lambda e: e.tensor_tensor(out=modT[:, ch0:ch0 + GC // 128, :],
                                                 in0=ps[:, 0:2 * (GC // 128)].rearrange("p (j t) -> p j t", t=2),
                                                 in1=bmT[:, ch0:ch0 + GC // 128].unsqueeze(2).to_broadcast([128, GC // 128, 2]), op=ALU.add),
                       reads=[bps, bbmT], writes=[bmodT], partial=True)
            else:
                ev = evs[g % 2]; bev = bevs[g % 2]
                c0 = g * GC - 2 * D
                for kc in range(KD):
                    pe.op(lambda e: e.matmul(ps[:, 0:GC], lhsT=srep[:, kc, :], rhs=wt[:, kc, :], start=(kc == 0), stop=False),
                          reads=[bwt, bsrep], writes=[bps], partial=(kc > 0), signal=False)
                pe.op(lambda e: e.matmul(ps[:, 0:GC], lhsT=ones_f[0:1, :], rhs=brow[0:1, c0:c0 + GC], start=False, stop=True),
                      reads=[b_ones, bbrow], writes=[bps], partial=True)
                dve.op(lambda e: e.tensor_copy(out=ev[:], in_=ps[:, 0:GC]), reads=[bps], writes=[bev])
                sp.dma(lambda e: e.dma_start(out=mod_bc[:, c0:c0 + GC], in_=ev[:]), bev, k.db("mod_bc"), bev)
        dve.op(lambda e: e.tensor_scalar(out=A1T[:], in0=modT[:, KD:2 * KD, :], scalar1=1.0, scalar2=None, op0=ALU.add),
               reads=[bmodT], writes=[bA1T])
        dve.op(lambda e: e.tensor_tensor(out=A1T[:], in0=A1T[:], in1=gaT[:].unsqueeze(2).to_broadcast([128, KD, 2]), op=ALU.mult),
               reads=[bA1T, bgaT], writes=[bA1T])
        dve.op(lambda e: e.tensor_copy(out=B1T[:], in_=modT[:, 0:KD, :]), reads=[bmodT], writes=[bB1T])
        if "A1T" in debug_outs:
            o1 = nc.dram_tensor("dbg_A1T", [128, KD * 2], F32, kind="ExternalOutput").ap(); k.dbufs["dbg_A1T"] = Buf("dbg_A1T")
            o2 = nc.dram_tensor("dbg_B1T", [128, KD * 2], F32, kind="ExternalOutput").ap(); k.dbufs["dbg_B1T"] = Buf("dbg_B1T")
            sp.dma(lambda e: e.dma_start(out=o1, in_=A1T[:].rearrange("p k t -> p (k t)")), bA1T, k.db("dbg_A1T"), bA1T)
            sp.dma(lambda e: e.dma_start(out=o2, in_=B1T[:].rearrange("p k t -> p (k t)")), bB1T, k.db("dbg_B1T"), bB1T)
        sc.barrier()
    if upto <= 1:
        return

    class Ring:
        def __init__(self, name, shape, dt, n, st, psum=False):
            mk = P_ if psum else S_
            self.t = [mk("%s%d" % (name, i), shape, dt, st) for i in range(n)]
            self.b = [Buf("%s%d" % (name, i)) for i in range(n)]
            self.i = -1
        def next(self):
            self.i = (self.i + 1) % len(self.t)
            return self.t[self.i], self.b[self.i]

    ident = S_("ident", [128, 128], BF16, top); b_ident = Buf("ident")
    identf = S_("identf", [128, 128], F32, top); b_identf = Buf("identf")
    pool.op(lambda e: e.memset(identf[:], 0.0), writes=[b_identf])
    pool.op(lambda e: e.affine_select(out=identf[:], in_=ones_f[:], pattern=[[-1, 128]], compare_op=ALU.is_equal, fill=0.0,
                                      base=0, channel_multiplier=1), reads=[b_ones], writes=[b_identf])
    dve.op(lambda e: e.tensor_copy(out=ident[:], in_=identf[:]), reads=[b_identf], writes=[b_ident])
    ones_b = S_("ones_b", [128, 128], BF16, top); b_onesb = Buf("ones_b")
    dve.op(lambda e: e.memset(ones_b[:], 1.0), writes=[b_onesb])

    hT_d = scratch("hT_d", [KD, 128, NK], BF16)
    with ExitStack() as ph:
        xts = Ring("xt", [128, D], F32, 2, ph)
        xns = Ring("xn", [128, D], BF16, 2, ph)
        junk = S_("junk", [128, D], BF16, ph); bjunk = Buf("junk")
        sss = Ring("ss", [128, 2], F32, 2, ph)
        hts = Ring("hts", [128, KD, 128], BF16, 2, ph)
        pst = Ring("pst", [128, 1024], BF16, 2, ph, psum=True)
        import os as _os
        for t in range(int(_os.environ.get('P2T', NK // 128))):
            col = 0 if t * 128 < S else 1
            xt, bxt = xts.next(); xn, bxn = xns.next(); ss, bss = sss.next(); ht, bht = hts.next()
            sp.dma(lambda e: e.dma_start(out=xt[:], in_=xk[t * 128:(t + 1) * 128, :]), k.db("xk"), bxt, bxt, partial=False)
            act.op(lambda e: e.activation(out=junk[:], in_=xt[:], func=AF.Square, accum_out=ss[:, 0:1]), reads=[bxt], writes=[bjunk, bss])
            dve.op(lambda e: e.tensor_scalar(out=ss[:, 1:2], in0=ss[:, 0:1], scalar1=1.0 / D, scalar2=1e-6, op0=ALU.mult, op1=ALU.add),
                   reads=[bss], writes=[bss])
            act.op(lambda e: e.activation(out=ss[:, 1:2], in_=ss[:, 1:2], func=AF.Ln), reads=[bss], writes=[bss]); act.op(lambda e: e.activation(out=ss[:, 1:2], in_=ss[:, 1:2], func=AF.Exp, scale=-0.5), reads=[bss], writes=[bss])
            dve.op(lambda e: e.tensor_scalar(out=xn[:], in0=xt[:], scalar1=ss[:, 1:2], scalar2=None, op0=ALU.mult), reads=[bxt, bss], writes=[bxn])
            _stg = int(_os.environ.get('P2STAGE', 5))
            for g0 in range(0, KD if _stg >= 3 else 0, 8):
                ps, bps = pst.next()
                ng_ = min(8, KD - g0)
                for j in range(ng_):
                    kc = g0 + j
                    pe.op(lambda e: e.transpose(out=ps[:, j * 128:(j + 1) * 128], in_=xn[:, kc * 128:(kc + 1) * 128], identity=ident[:]),
                          reads=[bxn, b_ident], writes=[bps], partial=(j > 0), signal=(j == ng_ - 1))
                for j in range(ng_ if _stg >= 4 else 0):
                    kc = g0 + j
                    if True:
                        dve.op(lambda e: e.tensor_scalar(out=ht[:, kc, :], in0=ps[:, j * 128:(j + 1) * 128], scalar1=A1T[:, kc, col:col + 1],
                                                         scalar2=B1T[:, kc, col:col + 1], op0=ALU.mult, op1=ALU.add),
                               reads=[bps, bA1T, bB1T], writes=[bht], partial=True)
                    else:
                        act.op(lambda e: e.activation(out=ht[:, kc, :], in_=ps[:, j * 128:(j + 1) * 128], func=AF.Identity,
                                                      scale=A1T[:, kc, col:col + 1], bias=B1T[:, kc, col:col + 1]),
                               reads=[bps, bA1T, bB1T], writes=[bht], partial=True)
            if _stg >= 5:
                sp.dma(lambda e: e.dma_start(out=hT_d[:, :, t * 128:(t + 1) * 128].rearrange("k p n -> p k n"), in_=ht[:]),
                       bht, k.db("hT_d"), bht)
        sc.barrier()
    if upto <= 2:
        return

    w_in = weight("w_in", D, c.INC)
    cosd = inp("cosd", [128, NK]); sind = inp("sind", [128, NK]); cosm = inp("cosm", [64, NK]); sinm = inp("sinm", [64, NK])
    DAH, MH, QL, KVL = c.DAH, c.MH, c.QL, c.KVL
    QT_d = scratch("QT_d", [2 * DAH, 128, NQ], BF16); KT_d = scratch("KT_d", [2 * DAH, 128, NK], BF16)
    V_d = scratch("V_d", [NK, DAH * 256], BF16)
    CQ_d = scratch("CQ_d", [QL // 128, 128, NQ], F32); CKV_d = scratch("CKV_d", [KVL // 128, 128, NK], F32)
    KR_d = scratch("KR_d", [64, NK], BF16)
    GA_d = scratch("GA_d", [KD, 128, NQ], BF16); GB_d = scratch("GB_d", [KD, 128, NQ], BF16)

    def rope_evac(st, ps, bps, M, t0, tw, cos_d, sin_d, cname, sname, dst_ap, dst_name, rings):
        ct, bct = rings["cos"].next(); sn, bsn = rings["sin"].next()
        t1, bt1 = rings["t1"].next(); t2, bt2 = rings["t2"].next(); ob, bob = rings["ob"].next()
        h = M // 2
        sp.dma(lambda e: e.dma_start(out=ct[0:M, 0:tw], in_=cos_d[0:M, t0:t0 + tw]), k.db(cname), bct, bct, partial=False)
        sp.dma(lambda e: e.dma_start(out=sn[0:M, 0:tw], in_=sin_d[0:M, t0:t0 + tw]), k.db(sname), bsn, bsn, partial=False)
        dve.op(lambda e: e.tensor_tensor(out=t1[0:M, 0:tw], in0=ps[0:M, 0:tw], in1=ct[0:M, 0:tw], op=ALU.mult), reads=[bps, bct], writes=[bt1])
        dve.op(lambda e: e.tensor_tensor(out=t2[0:h, 0:tw], in0=ps[h:M, 0:tw], in1=sn[0:h, 0:tw], op=ALU.mult), reads=[bps, bsn], writes=[bt2])
        dve.op(lambda e: e.tensor_tensor(out=t2[h:M, 0:tw], in0=ps[0:h, 0:tw], in1=sn[h:M, 0:tw], op=ALU.mult), reads=[bps, bsn], writes=[bt2], partial=True)
        pool.op(lambda e: e.tensor_tensor(out=ob[0:M, 0:tw], in0=t1[0:M, 0:tw], in1=t2[0:M, 0:tw], op=ALU.add), reads=[bt1, bt2], writes=[bob])
        act.dma(lambda e: e.dma_start(out=dst_ap, in_=ob[0:M, 0:tw]), bob, k.db(dst_name), bob)

    def mk_rope_rings(st):
        return dict(cos=Ring("rcos", [128, 512], F32, 2, st), sin=Ring("rsin", [128, 512], F32, 2, st),
                    t1=Ring("rt1", [128, 512], F32, 2, st), t2=Ring("rt2", [128, 512], F32, 2, st),
                    ob=Ring("rob", [128, 512], BF16, 3, st))

    with ExitStack() as ph:
        wgs = Ring("wg", [128, KD, 512], BF16, 2, ph)
        hxs = Ring("hx", [128, KD, 512], BF16, 2, ph)
        pss = Ring("pj", [128, 512], F32, 6, ph, psum=True)
        rr = mk_rope_rings(ph)
        evf = Ring("evf", [128, 512], F32, 3, ph)
        evb = Ring("evb", [128, 512], BF16, 3, ph)
        segs = [(c.o_dq, c.DAW, NQ, "dq"), (c.o_dk, c.DAW, NK, "dk"), (c.o_dv, c.DAW, NK, "dv"),
                (c.o_cq, QL, NQ, "cq"), (c.o_ckv, KVL + 64, NK, "ckv"), (c.o_ga, D, NQ, "ga"), (c.o_gb, D, NQ, "gb")]
        groups = []
        for (c0, ncol, ntok, kind) in segs:
            for g0 in range(0, ncol, 512):
                groups.append((c0, g0, min(512, ncol - g0), ntok, kind))
        wl = {}
        def loadW(gi):
            if gi < len(groups) and gi not in wl:
                c0, g0, gw, ntok, kind = groups[gi]
                wg, bwg = wgs.next()
                pool.dma(lambda e: e.dma_start(out=wg[:, :, 0:gw], in_=w_in[:, c0 + g0:c0 + g0 + gw].rearrange("(k p) n -> p k n", p=128)),
                         k.db("w_in"), bwg, bwg, partial=False)
                wl[gi] = (wg, bwg)
        items = [(gi, t0) for gi, g in enumerate(groups) for t0 in range(0, g[3], 512)]
        hl = {}
        def loadH(ii):
            if ii < len(items) and ii not in hl:
                gi, t0 = items[ii]
                tw = min(512, groups[gi][3] - t0)
                hx, bhx = hxs.next()
                sp.dma(lambda e: e.dma_start(out=hx[:, :, 0:tw], in_=hT_d[:, :, t0:t0 + tw].rearrange("k p n -> p k n")),
                       k.db("hT_d"), bhx, bhx, partial=False)
                hl[ii] = (hx, bhx)
        loadW(0); loadH(0)
        for ii, (gi, t0) in enumerate(items):
            c0, g0, gw, ntok, kind = groups[gi]
            tw = min(512, ntok - t0)
            if t0 == 0:
                loadW(gi + 1)
            loadH(ii + 1)
            wg, bwg = wl[gi]; hx, bhx = hl[ii]
            if kind == "dv":
                for tt in range(tw // 128):
                    ps, bps = pss.next()
                    for kc in range(KD):
                        pe.op(lambda e: e.matmul(ps[:, 0:gw], lhsT=hx[:, kc, tt * 128:(tt + 1) * 128], rhs=wg[:, kc, 0:gw],
                                                 start=(kc == 0), stop=(kc == KD - 1)),
                              reads=[bhx, bwg], writes=[bps], partial=(kc > 0), signal=(kc == KD - 1))
                    ob, bob = evb.next()
                    act.op(lambda e: e.copy(out=ob[:, 0:gw], in_=ps[:, 0:gw]), reads=[bps], writes=[bob])
                    r0 = t0 + tt * 128
                    act.dma(lambda e: e.dma_start(out=V_d[r0:r0 + 128, g0:g0 + gw], in_=ob[:, 0:gw]), bob, k.db("V_d"), bob)
                continue
            nch = (gw + 127) // 128
            for j in range(nch):
                M = min(128, gw - j * 128)
                ch = (g0 + j * 128) // 128
                ps, bps = pss.next()
                for kc in range(KD):
                    pe.op(lambda e: e.matmul(ps[0:M, 0:tw], lhsT=wg[:, kc, j * 128:j * 128 + M], rhs=hx[:, kc, 0:tw],
                                             start=(kc == 0), stop=(kc == KD - 1)),
                          reads=[bhx, bwg], writes=[bps], partial=(kc > 0), signal=(kc == KD - 1))
                if kind == "dq":
                    rope_evac(ph, ps, bps, 128, t0, tw, cosd, sind, "cosd", "sind", QT_d[ch, :, t0:t0 + tw], "QT_d", rr)
                elif kind == "dk":
                    rope_evac(ph, ps, bps, 128, t0, tw, cosd, sind, "cosd", "sind", KT_d[ch, :, t0:t0 + tw], "KT_d", rr)
                elif kind == "ckv" and M == 64:
                    rope_evac(ph, ps, bps, 64, t0, tw, cosm, sinm, "cosm", "sinm", KR_d[:, t0:t0 + tw], "KR_d", rr)
                elif kind in ("cq", "ckv"):
                    ob, bob = evf.next()
                    dve.op(lambda e: e.tensor_copy(out=ob[:, 0:tw], in_=ps[:, 0:tw]), reads=[bps], writes=[bob])
                    dst, dn = (CQ_d, "CQ_d") if kind == "cq" else (CKV_d, "CKV_d")
                    act.dma(lambda e: e.dma_start(out=dst[ch, :, t0:t0 + tw], in_=ob[:, 0:tw]), bob, k.db(dn), bob)
                else:
                    ob, bob = evb.next()
                    act.op(lambda e: e.activation(out=ob[:, 0:tw], in_=ps[:, 0:tw], func=AF.Sigmoid), reads=[bps], writes=[bob])
                    dst, dn = (GA_d, "GA_d") if kind == "ga" else (GB_d, "GB_d")
                    act.dma(lambda e: e.dma_start(out=dst[ch, :, t0:t0 + tw], in_=ob[:, 0:tw]), bob, k.db(dn), bob)
        sc.barrier()
    if upto <= 3:
        return

    w_uq = weight("w_uq", QL, MH * 192); w_ukv = weight("w_ukv", KVL, MH * 256)
    g_q = inp("g_q", [1, QL]); g_kv = inp("g_kv", [1, KVL])
    QN_d = scratch("QN_d", [MH, 128, NQ], BF16); QR_d = scratch("QR_d", [MH, 64, NQ], BF16)
    KN_d = scratch("KN_d", [MH, 128, NK], BF16); MV_d = scratch("MV_d", [NK, MH * 128], BF16)

    def latent_up(lat_d, lat_name, nch, ntok, g_ap, g_name, W, Wname, wcols, emit):
        with ExitStack() as ph:
            wsb = S_("lw", [128, nch, wcols], BF16, ph); bw = Buf("lw")
            pool.dma(lambda e: e.dma_start(out=wsb[:], in_=W.rearrange("(k p) n -> p k n", p=128)), k.db(Wname), bw, bw, partial=False)
            gT = S_("lg", [128, nch], F32, ph); bg = Buf("lg")
            sp.dma(lambda e: e.dma_start(out=gT[:], in_=g_ap.rearrange("o (k p) -> p (o k)", p=128), allow_slow_non_contiguous=True),
                   k.db(g_name), bg, bg)
            Ls = Ring("lL", [128, nch, 512], F32, 2, ph)
            sq = S_("lsq", [128, nch, 512], F32, ph); bsq = Buf("lsq")
            Lns = Ring("lLn", [128, nch, 512], BF16, 2, ph)
            rs = Ring("lrs", [128, 512], F32, 2, ph)
            pss = Ring("lps", [128, 512], F32, 5, ph, psum=True)
            rr = mk_rope_rings(ph)
            evb = Ring("levb", [128, 512], BF16, 3, ph)
            for t0 in range(0, ntok, 512):
                tw = min(512, ntok - t0)
                L, bL = Ls.next(); Ln, bLn = Lns.next(); r_, br = rs.next()
                sp.dma(lambda e: e.dma_start(out=L[:, :, 0:tw], in_=lat_d[:, :, t0:t0 + tw].rearrange("k p n -> p k n")),
                       k.db(lat_name), bL, bL, partial=False)
                act.op(lambda e: e.activation(out=sq[:, :, 0:tw], in_=L[:, :, 0:tw], func=AF.Square), reads=[bL], writes=[bsq])
                ps, bps = pss.next()
                for kc in range(nch):
                    pe.op(lambda e: e.matmul(ps[:, 0:tw], lhsT=ones_f[:], rhs=sq[:, kc, 0:tw], start=(kc == 0), stop=(kc == nch - 1)),
                          reads=[b_ones, bsq], writes=[bps], partial=(kc > 0), signal=(kc == nch - 1))
                dve.op(lambda e: e.tensor_scalar(out=r_[:, 0:tw], in0=ps[:, 0:tw], scalar1=1.0 / (nch * 128), scalar2=1e-6, op0=ALU.mult, op1=ALU.add),
                       reads=[bps], writes=[br])
                act.op(lambda e: e.activation(out=r_[:, 0:tw], in_=r_[:, 0:tw], func=AF.Ln), reads=[br], writes=[br]); act.op(lambda e: e.activation(out=r_[:, 0:tw], in_=r_[:, 0:tw], func=AF.Exp, scale=-0.5), reads=[br], writes=[br])
                for kc in range(nch):
                    dve.op(lambda e: e.scalar_tensor_tensor(out=Ln[:, kc, 0:tw], in0=L[:, kc, 0:tw], scalar=gT[:, kc:kc + 1], in1=r_[:, 0:tw],
                                                            op0=ALU.mult, op1=ALU.mult),
                           reads=[bL, bg, br], writes=[bLn], partial=(kc > 0))
                emit(Ln, bLn, wsb, bw, t0, tw, nch, pss, rr, evb)
            sc.barrier()

    def fm_head(Ln, bLn, wsb, bw, nch, col0, M, tw, pss):
        ps, bps = pss.next()
        for kc in range(nch):
            pe.op(lambda e: e.matmul(ps[0:M, 0:tw], lhsT=wsb[:, kc, col0:col0 + M], rhs=Ln[:, kc, 0:tw], start=(kc == 0), stop=(kc == nch - 1)),
                  reads=[bw, bLn], writes=[bps], partial=(kc > 0), signal=(kc == nch - 1))
        return ps, bps

    def emit_q(Ln, bLn, wsb, bw, t0, tw, nch, pss, rr, evb):
        for h in range(MH):
            ps, bps = fm_head(Ln, bLn, wsb, bw, nch, h * 192, 128, tw, pss)
            ob, bob = evb.next()
            act.op(lambda e: e.copy(out=ob[:, 0:tw], in_=ps[:, 0:tw]), reads=[bps], writes=[bob])
            act.dma(lambda e: e.dma_start(out=QN_d[h, :, t0:t0 + tw], in_=ob[:, 0:tw]), bob, k.db("QN_d"), bob)
            ps, bps = fm_head(Ln, bLn, wsb, bw, nch, h * 192 + 128, 64, tw, pss)
            rope_evac(None, ps, bps, 64, t0, tw, cosm, sinm, "cosm", "sinm", QR_d[h, :, t0:t0 + tw], "QR_d", rr)

    def emit_kv(Ln, bLn, wsb, bw, t0, tw, nch, pss, rr, evb):
        for h in range(MH):
            ps, bps = fm_head(Ln, bLn, wsb, bw, nch, h * 256, 128, tw, pss)
            ob, bob = evb.next()
            act.op(lambda e: e.copy(out=ob[:, 0:tw], in_=ps[:, 0:tw]), reads=[bps], writes=[bob])
            act.dma(lambda e: e.dma_start(out=KN_d[h, :, t0:t0 + tw], in_=ob[:, 0:tw]), bob, k.db("KN_d"), bob)
        for tt in range(tw // 128):
            for h0 in range(0, MH, 4):
                nh = min(4, MH - h0)
                ps, bps = pss.next()
                for kc in range(nch):
                    pe.op(lambda e: e.matmul(ps[:, 0:nh * 128].rearrange("p (h e) -> p h e", e=128), lhsT=Ln[:, kc, tt * 128:(tt + 1) * 128],
                                             rhs=wsb[:, kc, :].rearrange("p (h e) -> p h e", e=256)[:, h0:h0 + nh, 128:256],
                                             start=(kc == 0), stop=(kc == nch - 1)),
                          reads=[bw, bLn], writes=[bps], partial=(kc > 0), signal=(kc == nch - 1))
                ob, bob = evb.next()
                dve.op(lambda e: e.tensor_copy(out=ob[:, 0:nh * 128], in_=ps[:, 0:nh * 128]), reads=[bps], writes=[bob])
                r0 = t0 + tt * 128
                act.dma(lambda e: e.dma_start(out=MV_d[r0:r0 + 128, h0 * 128:(h0 + nh) * 128], in_=ob[:, 0:nh * 128]), bob, k.db("MV_d"), bob)

    latent_up(CQ_d, "CQ_d", QL // 128, NQ, g_q, "g_q", w_uq, "w_uq", MH * 192, emit_q)
    latent_up(CKV_d, "CKV_d", KVL // 128, NK, g_kv, "g_kv", w_ukv, "w_ukv", MH * 256, emit_kv)
    if upto <= 4:
        return

    OA_d = scratch("OA_d", [c.DAW // 128, 128, NQ], BF16); OB_d = scratch("OB_d", [c.MW // 128, 128, NQ], BF16)
    g_subln = inp("g_subln", [1, 256])
    lam_in = [inp(n, [1, 128]) for n in ("lam_q1", "lam_k1", "lam_q2", "lam_k2")]
    NKC = NK // 128

    def attn_branch(H, ncomp, E, scale, k_parts, q_parts, v_src, finalize, extra=None):
        with ExitStack() as ph:
            np_ = len(k_parts(0, 0))
            kts = Ring("akt", [128, ncomp * np_, NK], BF16, 2, ph)
            vts = Ring("avt", [128, NKC, E + 1], BF16, 2, ph)
            for i in range(2):
                pool.op(lambda e: e.memset(vts.t[i][:, :, E:E + 1], 1.0), writes=[vts.b[i]])
            qts = Ring("aqt", [128, ncomp * np_, 512], BF16, 2, ph)
            pts = Ring("apt", [128, 1024], BF16, 3, ph)
            ops_ = Ring("aop", [128, 512], F32, 4, ph, psum=True)
            sps = Ring("asp", [128, 1024], F32, 2, ph, psum=True)
            ctx_ = extra(ph, sps) if extra else None
            for h in range(H):
                kt, bkt = kts.next(); vt, bvt = vts.next()
                for comp in range(ncomp):
                    for pi, (ap_, nm, Kr) in enumerate(k_parts(h, comp)):
                        sp.dma(lambda e: e.dma_start(out=kt[0:Kr, comp * np_ + pi, :], in_=ap_), k.db(nm), bkt, bkt, partial=(comp + pi > 0))
                vap, vnm = v_src(h)
                sp.dma(lambda e: e.dma_start(out=vt[:, :, 0:E], in_=vap.rearrange("(k p) e -> p k e", p=128)), k.db(vnm), bvt, bvt)
                for q0 in range(0, NQ, 512):
                    tw = min(512, NQ - q0); nqs = tw // 128
                    qt, bqt = qts.next()
                    for comp in range(ncomp):
                        for pi, (ap_, nm, Kr) in enumerate(q_parts(h, comp)):
                            sp.dma(lambda e: e.dma_start(out=qt[0:Kr, comp * np_ + pi, 0:tw], in_=ap_[:, q0:q0 + tw]), k.db(nm), bqt, bqt,
                                   partial=(comp + pi > 0))
                    for comp in range(ncomp):
                        Os = [ops_.next() for _ in range(nqs)]
                        parts = k_parts(h, comp)
                        for kc0 in range(0, NKC, 2):
                            n2 = min(2, NKC - kc0)
                            sp_, bsp = sps.next()
                            for j in range(n2):
                                for pi, (_, _, Kr) in enumerate(parts):
                                    pe.op(lambda e: e.matmul(sp_[:, j * 512:j * 512 + tw], lhsT=kt[0:Kr, comp * np_ + pi, (kc0 + j) * 128:(kc0 + j + 1) * 128],
                                                             rhs=qt[0:Kr, comp * np_ + pi, 0:tw], start=(pi == 0), stop=(pi == np_ - 1)),
                                          reads=[bkt, bqt], writes=[bsp], partial=(j + pi > 0), signal=(j == n2 - 1 and pi == np_ - 1))
                            pt, bpt = pts.next()
                            if tw == 512:
                                act.op(lambda e: e.activation(out=pt[:, 0:n2 * 512], in_=sp_[:, 0:n2 * 512], func=AF.Exp, scale=scale), reads=[bsp], writes=[bpt])
                            else:
                                for j in range(n2):
                                    act.op(lambda e: e.activation(out=pt[:, j * 512:j * 512 + tw], in_=sp_[:, j * 512:j * 512 + tw], func=AF.Exp, scale=scale),
                                           reads=[bsp], writes=[bpt], partial=(j > 0))
                            for j in range(n2):
                                kc = kc0 + j
                                for qs in range(nqs):
                                    O, bO = Os[qs]
                                    pe.op(lambda e: e.matmul(O[:, 0:E + 1], lhsT=pt[:, j * 512 + qs * 128:j * 512 + (qs + 1) * 128], rhs=vt[:, kc, 0:E + 1],
                                                             start=(kc == 0), stop=(kc == NKC - 1)),
                                          reads=[bpt, bvt], writes=[bO], partial=(kc > 0), signal=(kc == NKC - 1 or (j == n2 - 1 and qs == nqs - 1)))
                        finalize(ctx_, h, comp, q0, tw, Os, sps)
            sc.barrier()

    def store_T(ctx_, src_bf, bsrc, nE, dst_d, dst_name, ch0, q0, tw, sps):
        nqs = tw // 128
        for ec in range(nE):
            sp_, bsp = sps.next()
            pb = sp_[:].bitcast(BF16)
            for qs in range(nqs):
                pe.op(lambda e: e.transpose(out=pb[:, qs * 128:(qs + 1) * 128], in_=src_bf[:, qs, ec * 128:(ec + 1) * 128], identity=ident[:]),
                      reads=[bsrc, b_ident], writes=[bsp], partial=(qs > 0), signal=(qs == nqs - 1))
            ob, bob = ctx_["oT"].next()
            dve.op(lambda e: e.tensor_copy(out=ob[:, 0:tw], in_=pb[:, 0:tw]), reads=[bsp], writes=[bob])
            act.dma(lambda e: e.dma_start(out=dst_d[ch0 + ec, :, q0:q0 + tw], in_=ob[:, 0:tw]), bob, k.db(dst_name), bob)

    def da_extra(ph, sps):
        d = {}
        d["o0"] = S_("da_o0", [128, 4, 256], F32, ph); d["bo0"] = Buf("da_o0")
        d["o1"] = S_("da_o1", [128, 4, 256], F32, ph); d["bo1"] = Buf("da_o1")
        d["ob"] = S_("da_ob", [128, 4, 256], BF16, ph); d["bob"] = Buf("da_ob")
        d["jk"] = S_("da_jk", [128, 256], F32, ph); d["bjk"] = Buf("da_jk")
        d["rs"] = S_("da_rs", [128, 8], F32, ph); d["brs"] = Buf("da_rs")
        d["oT"] = Ring("da_oT", [128, 512], BF16, 3, ph)
        lv = S_("da_lv", [128, 4], F32, ph); blv = Buf("da_lv")
        for i in range(4):
            sp.dma(lambda e: e.dma_start(out=lv[:, i:i + 1], in_=lam_in[i].rearrange("o p -> p o"), allow_slow_non_contiguous=True),
                   k.db(("lam_q1", "lam_k1", "lam_q2", "lam_k2")[i]), blv, blv)
        lp = S_("da_lp", [128, 2], F32, ph); blp = Buf("da_lp")
        dve.op(lambda e: e.tensor_tensor(out=lp[:, 0:1], in0=lv[:, 0:1], in1=lv[:, 1:2], op=ALU.mult), reads=[blv], writes=[blp])
        dve.op(lambda e: e.tensor_tensor(out=lp[:, 1:2], in0=lv[:, 2:3], in1=lv[:, 3:4], op=ALU.mult), reads=[blv], writes=[blp], partial=True)
        with ExitStack() as tmp:
            ps, bps = sps.next()
            pe.op(lambda e: e.matmul(ps[:, 0:2], lhsT=ones_f[:], rhs=lp[:], start=True, stop=True), reads=[b_ones, blp], writes=[bps])
            le = S_("da_le", [128, 2], F32, ph); ble = Buf("da_le")
            act.op(lambda e: e.activation(out=le[:], in_=ps[:, 0:2], func=AF.Exp), reads=[bps], writes=[ble])
            nl = S_("da_nl", [128, 1], F32, ph); bnl = Buf("da_nl")
            dve.op(lambda e: e.tensor_tensor(out=nl[:], in0=le[:, 1:2], in1=le[:, 0:1], op=ALU.subtract), reads=[ble], writes=[bnl])
            dve.op(lambda e: e.tensor_scalar(out=nl[:], in0=nl[:], scalar1=-c.lambda_init, scalar2=None, op0=ALU.add), reads=[bnl], writes=[bnl])
            d["nl"] = nl; d["bnl"] = bnl
            gr = S_("da_gr", [1, 256], F32, ph); bgr = Buf("da_gr")
            sp.dma(lambda e: e.dma_start(out=gr[:], in_=g_subln), k.db("g_subln"), bgr, bgr)
            pe.op(lambda e: e.matmul(ps[:, 256:512], lhsT=ones_f[0:1, :], rhs=gr[0:1, :], start=True, stop=True), reads=[b_ones, bgr], writes=[bps])
            gb_ = S_("da_gb", [128, 256], F32, ph); bgb = Buf("da_gb")
            dve.op(lambda e: e.tensor_scalar(out=gb_[:], in0=ps[:, 256:512], scalar1=1.0 - c.lambda_init, scalar2=None, op0=ALU.mult),
                   reads=[bps], writes=[bgb])
            d["gb"] = gb_; d["bgb"] = bgb
            sc.barrier([pe, dve, act])
        return d

    def da_final(d, h, comp, q0, tw, Os, sps):
        nqs = tw // 128
        rs, brs = d["rs"], d["brs"]
        dst, bdst = (d["o0"], d["bo0"]) if comp == 0 else (d["o1"], d["bo1"])
        for qs in range(nqs):
            O, bO = Os[qs]
            dve.op(lambda e: e.reciprocal(out=rs[:, qs:qs + 1], in_=O[:, 256:257]), reads=[bO], writes=[brs], partial=(qs > 0))
            dve.op(lambda e: e.tensor_scalar(out=dst[:, qs, :], in0=O[:, 0:256], scalar1=rs[:, qs:qs + 1], scalar2=None, op0=ALU.mult),
                   reads=[bO, brs], writes=[bdst], partial=(qs > 0))
        if comp == 0:
            return
        o0, bo0, o1, bo1 = d["o0"], d["bo0"], d["o1"], d["bo1"]
        for qs in range(nqs):
            dve.op(lambda e: e.scalar_tensor_tensor(out=o0[:, qs, :], in0=o1[:, qs, :], scalar=d["nl"][:, 0:1], in1=o0[:, qs, :], op0=ALU.mult, op1=ALU.add),
                   reads=[bo1, bo0, d["bnl"]], writes=[bo0])
            act.op(lambda e: e.activation(out=d["jk"][:], in_=o0[:, qs, :], func=AF.Square, accum_out=rs[:, 4 + qs:5 + qs]),
                   reads=[bo0], writes=[d["bjk"], brs])
            dve.op(lambda e: e.tensor_scalar(out=rs[:, 4 + qs:5 + qs], in0=rs[:, 4 + qs:5 + qs], scalar1=1.0 / 256, scalar2=1e-6, op0=ALU.mult, op1=ALU.add),
                   reads=[brs], writes=[brs])
            act.op(lambda e: e.activation(out=rs[:, 4 + qs:5 + qs], in_=rs[:, 4 + qs:5 + qs], func=AF.Ln), reads=[brs], writes=[brs]); act.op(lambda e: e.activation(out=rs[:, 4 + qs:5 + qs], in_=rs[:, 4 + qs:5 + qs], func=AF.Exp, scale=-0.5), reads=[brs], writes=[brs])
            dve.op(lambda e: e.scalar_tensor_tensor(out=d["ob"][:, qs, :], in0=o0[:, qs, :], scalar=rs[:, 4 + qs:5 + qs], in1=d["gb"][:], op0=ALU.mult, op1=ALU.mult),
                   reads=[bo0, brs, d["bgb"]], writes=[d["bob"]], partial=(qs > 0))
        store_T(d, d["ob"], d["bob"], 2, OA_d, "OA_d", h * 2, q0, tw, sps)

    attn_branch(DAH, 2, 256, 128 ** -0.5,
                lambda h, comp: [(KT_d[comp * DAH + h], "KT_d", 128)],
                lambda h, comp: [(QT_d[comp * DAH + h], "QT_d", 128)],
                lambda h: (V_d[:, h * 256:(h + 1) * 256], "V_d"), da_final, da_extra)
    if upto <= 5:
        return

    def mla_extra(ph, sps):
        d = {}
        d["ob"] = S_("ml_ob", [128, 4, 128], BF16, ph); d["bob"] = Buf("ml_ob")
        d["rs"] = S_("ml_rs", [128, 4], F32, ph); d["brs"] = Buf("ml_rs")
        d["oT"] = Ring("ml_oT", [128, 512], BF16, 3, ph)
        return d

    def mla_final(d, h, comp, q0, tw, Os, sps):
        nqs = tw // 128
        rs, brs = d["rs"], d["brs"]
        for qs in range(nqs):
            O, bO = Os[qs]
            dve.op(lambda e: e.reciprocal(out=rs[:, qs:qs + 1], in_=O[:, 128:129]), reads=[bO], writes=[brs], partial=(qs > 0))
            dve.op(lambda e: e.tensor_scalar(out=d["ob"][:, qs, :], in0=O[:, 0:128], scalar1=rs[:, qs:qs + 1], scalar2=None, op0=ALU.mult),
                   reads=[bO, brs], writes=[d["bob"]], partial=(qs > 0))
        store_T(d, d["ob"], d["bob"], 1, OB_d, "OB_d", h, q0, tw, sps)

    attn_branch(MH, 1, 128, 192 ** -0.5,
                lambda h, comp: [(KN_d[h], "KN_d", 128), (KR_d, "KR_d", 64)],
                lambda h, comp: [(QN_d[h], "QN_d", 128), (QR_d[h], "QR_d", 64)],
                lambda h: (MV_d[:, h * 128:(h + 1) * 128], "MV_d"), mla_final, mla_extra)
    if upto <= 6:
        return

    w_br_a = weight("w_br_a", c.DAW, D); w_br_b = weight("w_br_b", c.MW, D); w_out = weight("w_out", D, D)
    NR = c.NG + c.NE; NE = c.NE; NG = c.NG; EPG = c.EPG; DE = c.DE; HC = DE // 128
    NT = NQ // 128; NBLK = 2 * NT + NE
    g_ffn = inp("g_ffn", [1, D]); w_r = inp("w_r", [D, NR]); b_r = inp("b_r", [1, NR]); g_final = inp("g_final", [1, D])
    NP = 4; EPP = NE // NP
    w1p = [weight("w1_%d" % q, EPP * D, DE) for q in range(NP)]; w3p = [weight("w3_%d" % q, EPP * D, DE) for q in range(NP)]
    w2p = [weight("w2_%d" % q, EPP * DE, D) for q in range(NP)]
    MG_d = scratch("MG_d", [KD, 128, NQ], BF16); X1_d = scratch("X1_d", [NQ, D], F32); H2_d = scratch("H2_d", [NQ, D], BF16)
    Xs_d = scratch("Xs_d", [NBLK * 128, D], BF16); Ys_d = scratch("Ys_d", [NBLK * 128, D], F32)
    KA = c.DAW // 128; KB = c.MW // 128

    with ExitStack() as ph:
        was = Ring("wa", [128, KA, 512], BF16, 2, ph); wbs = Ring("wb", [128, KB, 512], BF16, 2, ph)
        oas = Ring("oa", [128, KA, 512], BF16, 2, ph); obs = Ring("ob", [128, KB, 512], BF16, 2, ph)
        gas = Ring("gat", [128, 512], BF16, 2, ph); gbs = Ring("gbt", [128, 512], BF16, 2, ph)
        t1s = Ring("mt1", [128, 512], F32, 2, ph); t2s = Ring("mt2", [128, 512], F32, 2, ph); mgs = Ring("mgo", [128, 512], BF16, 3, ph)
        pss = Ring("mps", [128, 512], F32, 6, ph, psum=True)
        for g0 in range(0, D, 512):
            gw = min(512, D - g0)
            wa, bwa = was.next(); wb, bwb = wbs.next()
            pool.dma(lambda e: e.dma_start(out=wa[:, :, 0:gw], in_=w_br_a[:, g0:g0 + gw].rearrange("(k p) n -> p k n", p=128)), k.db("w_br_a"), bwa, bwa, partial=False)
            pool.dma(lambda e: e.dma_start(out=wb[:, :, 0:gw], in_=w_br_b[:, g0:g0 + gw].rearrange("(k p) n -> p k n", p=128)), k.db("w_br_b"), bwb, bwb, partial=False)
            for t0 in range(0, NQ, 512):
                tw = min(512, NQ - t0)
                oa, boa = oas.next(); ob, bob = obs.next()
                sp.dma(lambda e: e.dma_start(out=oa[:, :, 0:tw], in_=OA_d[:, :, t0:t0 + tw].rearrange("k p n -> p k n")), k.db("OA_d"), boa, boa, partial=False)
                sp.dma(lambda e: e.dma_start(out=ob[:, :, 0:tw], in_=OB_d[:, :, t0:t0 + tw].rearrange("k p n -> p k n")), k.db("OB_d"), bob, bob, partial=False)
                for j in range(gw // 128):
                    dc = (g0 + j * 128) // 128
                    psa, bpa = pss.next(); psb, bpb = pss.next()
                    for kc in range(KA):
                        pe.op(lambda e: e.matmul(psa[:, 0:tw], lhsT=wa[:, kc, j * 128:(j + 1) * 128], rhs=oa[:, kc, 0:tw], start=(kc == 0), stop=(kc == KA - 1)),
                              reads=[bwa, boa], writes=[bpa], partial=(kc > 0), signal=(kc == KA - 1))
                    for kc in range(KB):
                        pe.op(lambda e: e.matmul(psb[:, 0:tw], lhsT=wb[:, kc, j * 128:(j + 1) * 128], rhs=ob[:, kc, 0:tw], start=(kc == 0), stop=(kc == KB - 1)),
                              reads=[bwb, bob], writes=[bpb], partial=(kc > 0), signal=(kc == KB - 1))
                    ga, bga = gas.next(); gb, bgb = gbs.next()
                    sp.dma(lambda e: e.dma_start(out=ga[:, 0:tw], in_=GA_d[dc, :, t0:t0 + tw]), k.db("GA_d"), bga, bga, partial=False)
                    sp.dma(lambda e: e.dma_start(out=gb[:, 0:tw], in_=GB_d[dc, :, t0:t0 + tw]), k.db("GB_d"), bgb, bgb, partial=False)
                    t1, bt1 = t1s.next(); t2, bt2 = t2s.next(); mg, bmg = mgs.next()
                    dve.op(lambda e: e.tensor_tensor(out=t1[:, 0:tw], in0=psa[:, 0:tw], in1=ga[:, 0:tw], op=ALU.mult), reads=[bpa, bga], writes=[bt1])
                    dve.op(lambda e: e.tensor_tensor(out=t2[:, 0:tw], in0=psb[:, 0:tw], in1=gb[:, 0:tw], op=ALU.mult), reads=[bpb, bgb], writes=[bt2])
                    pool.op(lambda e: e.tensor_tensor(out=mg[:, 0:tw], in0=t1[:, 0:tw], in1=t2[:, 0:tw], op=ALU.add), reads=[bt1, bt2], writes=[bmg])
                    act.dma(lambda e: e.dma_start(out=MG_d[dc, :, t0:t0 + tw], in_=mg[:, 0:tw]), bmg, k.db("MG_d"), bmg)
        sc.barrier()
    if upto <= 7:
        return

    with ExitStack() as ph:
        wos = Ring("wo", [128, KD, 512], BF16, 2, ph); mgt = Ring("mgt", [128, KD, 128], BF16, 3, ph)
        gts = Ring("gt1", [128, 512], F32, 2, ph); xs = Ring("xr", [128, 512], F32, 3, ph)
        t1s = Ring("ot1", [128, 512], F32, 2, ph); x1s = Ring("x1o", [128, 512], F32, 3, ph)
        pss = Ring("ops", [128, 512], F32, 4, ph, psum=True)
        for g0 in range(0, D, 512):
            gw = min(512, D - g0)
            wo, bwo = wos.next(); gt, bgt = gts.next()
            pool.dma(lambda e: e.dma_start(out=wo[:, :, 0:gw], in_=w_out[:, g0:g0 + gw].rearrange("(k p) n -> p k n", p=128)), k.db("w_out"), bwo, bwo, partial=False)
            sp.dma(lambda e: e.dma_start(out=gt[:, 0:gw], in_=mod_bc[:, g0:g0 + gw]), k.db("mod_bc"), bgt, bgt, partial=False)
            for tt in range(NT):
                mg, bmg = mgt.next(); xr, bxr = xs.next()
                sp.dma(lambda e: e.dma_start(out=mg[:], in_=MG_d[:, :, tt * 128:(tt + 1) * 128].rearrange("k p n -> p k n")), k.db("MG_d"), bmg, bmg, partial=False)
                sp.dma(lambda e: e.dma_start(out=xr[:, 0:gw], in_=xk[tt * 128:(tt + 1) * 128, g0:g0 + gw]), k.db("xk"), bxr, bxr, partial=False)
                ps, bps = pss.next()
                for kc in range(KD):
                    pe.op(lambda e: e.matmul(ps[:, 0:gw], lhsT=mg[:, kc, :], rhs=wo[:, kc, 0:gw], start=(kc == 0), stop=(kc == KD - 1)),
                          reads=[bmg, bwo], writes=[bps], partial=(kc > 0), signal=(kc == KD - 1))
                t1, bt1 = t1s.next(); x1, bx1 = x1s.next()
                dve.op(lambda e: e.tensor_tensor(out=t1[:, 0:gw], in0=ps[:, 0:gw], in1=gt[:, 0:gw], op=ALU.mult), reads=[bps, bgt], writes=[bt1])
                pool.op(lambda e: e.tensor_tensor(out=x1[:, 0:gw], in0=t1[:, 0:gw], in1=xr[:, 0:gw], op=ALU.add), reads=[bt1, bxr], writes=[bx1])
                act.dma(lambda e: e.dma_start(out=X1_d[tt * 128:(tt + 1) * 128, g0:g0 + gw], in_=x1[:, 0:gw]), bx1, k.db("X1_d"), bx1)
        sc.barrier()
    if upto <= 8:
        return

    moe = ExitStack(); top.enter_context(moe)
    Ind_all = S_("Ind_all", [128, NT, NE], BF16, moe); bInd = Buf("Ind_all")
    I1_all = S_("I1_all", [128, NT, NE], F32, moe); bI1 = Buf("I1_all")
    I2_all = S_("I2_all", [128, NT, NE], F32, moe); bI2 = Buf("I2_all")
    wsel = S_("wsel", [128, NT, 2], F32, moe); bwsel = Buf("wsel")
    idx_all = S_("idx_all", [128, NT, 2], I32, moe); bidx = Buf("idx_all")
    NSP_ = max(1, (KD * DE * 4 + 32767) // 32768)
    widx_i = S_("widx_i", [128, 4 * NSP_, NBLK], I32, moe); bwidx = Buf("widx_i")
    Uf = S_("Uf", [128, 128], F32, moe); bUf = Buf("Uf")
    Ub = S_("Ub", [128, 128], BF16, moe); bUb = Buf("Ub")
    jji = S_("jji", [128, 128], I32, moe); bjji = Buf("jji")
    pool.op(lambda e: e.iota(out=jji[:], pattern=[[1, 128]], base=0, channel_multiplier=0), writes=[bjji])
    ppi = S_("ppi", [128, 1], I32, moe); bppi = Buf("ppi")
    pool.op(lambda e: e.iota(out=ppi[:], pattern=[[0, 1]], base=0, channel_multiplier=1), writes=[bppi])
    ppf = S_("ppf", [128, 1], F32, moe); bppf = Buf("ppf")
    dve.op(lambda e: e.tensor_copy(out=ppf[:], in_=ppi[:]), reads=[bppi], writes=[bppf])
    dve.op(lambda e: e.tensor_copy(out=Uf[:], in_=jji[:]), reads=[bjji], writes=[bUf])
    dve.op(lambda e: e.tensor_scalar(out=Uf[:], in0=Uf[:], scalar1=ppf[:, 0:1], scalar2=None, op0=ALU.is_gt), reads=[bUf, bppf], writes=[bUf])
    dve.op(lambda e: e.tensor_copy(out=Ub[:], in_=Uf[:]), reads=[bUf], writes=[bUb])

    def bcast_row(row_ap, row_name, dst, bdst, st, pss, post=None):
        rw = S_("bc_row", [1, D], F32, st); brw = Buf("bc_row")
        sp.dma(lambda e: e.dma_start(out=rw[:], in_=row_ap), k.db(row_name), brw, brw)
        for g0 in range(0, D, 512):
            gw = min(512, D - g0)
            ps, bps = pss.next()
            pe.op(lambda e: e.matmul(ps[:, 0:gw], lhsT=ones_f[0:1, :], rhs=rw[0:1, g0:g0 + gw], start=True, stop=True), reads=[b_ones, brw], writes=[bps])
            if post is None:
                dve.op(lambda e: e.tensor_copy(out=dst[:, g0:g0 + gw], in_=ps[:, 0:gw]), reads=[bps], writes=[bdst], partial=True)
            else:
                post(ps, bps, g0, gw)

    with ExitStack() as ph:
        A2 = S_("A2", [128, D], F32, ph); bA2 = Buf("A2"); B2 = S_("B2", [128, D], F32, ph); bB2 = Buf("B2")
        pss = Ring("rps", [128, 512], F32, 4, ph, psum=True)
        sp.dma(lambda e: e.dma_start(out=A2[:], in_=mod_bc[:, 2 * D:3 * D]), k.db("mod_bc"), bA2, bA2)
        sp.dma(lambda e: e.dma_start(out=B2[:], in_=mod_bc[:, D:2 * D]), k.db("mod_bc"), bB2, bB2)
        bcast_row(g_ffn, "g_ffn", A2, bA2, ph, pss,
                  post=lambda ps, bps, g0, gw: dve.op(lambda e: e.scalar_tensor_tensor(out=A2[:, g0:g0 + gw], in0=A2[:, g0:g0 + gw], scalar=1.0, in1=ps[:, 0:gw],
                                                                                       op0=ALU.add, op1=ALU.mult), reads=[bps, bA2], writes=[bA2]))
        wr = S_("wr", [128, KD, NR], F32, ph); bwr = Buf("wr")
        sp.dma(lambda e: e.dma_start(out=wr[:], in_=w_r.rearrange("(k p) n -> p k n", p=128)), k.db("w_r"), bwr, bwr)
        brr = S_("brr", [1, NR], F32, ph); bbrr = Buf("brr")
        sp.dma(lambda e: e.dma_start(out=brr[:], in_=b_r), k.db("b_r"), bbrr, bbrr)
        x1s = Ring("cx1", [128, D], F32, 2, ph)
        h2 = S_("h2", [128, D], F32, ph); bh2 = Buf("h2")
        h2bs = Ring("h2b", [128, D], BF16, 2, ph)
        h2T = S_("h2T", [128, KD, 128], F32, ph); bh2T = Buf("h2T")
        sm = S_("rsm", [128, 16], F32, ph); bsm = Buf("rsm")
        lg = S_("lg", [128, NR], F32, ph); blg = Buf("lg")
        rt = S_("rt", [128, 8, NE], F32, ph); brt = Buf("rt")
        jk = S_("cjk", [128, D], BF16, ph); bjk = Buf("cjk")
        for tt in range(NT):
            x1, bx1 = x1s.next(); h2b, bh2b = h2bs.next()
            sp.dma(lambda e: e.dma_start(out=x1[:], in_=X1_d[tt * 128:(tt + 1) * 128, :]), k.db("X1_d"), bx1, bx1, partial=False)
            act.op(lambda e: e.activation(out=jk[:], in_=x1[:], func=AF.Square, accum_out=sm[:, 0:1]), reads=[bx1], writes=[bjk, bsm])
            dve.op(lambda e: e.tensor_scalar(out=sm[:, 1:2], in0=sm[:, 0:1], scalar1=1.0 / D, scalar2=1e-6, op0=ALU.mult, op1=ALU.add), reads=[bsm], writes=[bsm])
            act.op(lambda e: e.activation(out=sm[:, 1:2], in_=sm[:, 1:2], func=AF.Ln), reads=[bsm], writes=[bsm])
            act.op(lambda e: e.activation(out=sm[:, 1:2], in_=sm[:, 1:2], func=AF.Exp, scale=-0.5), reads=[bsm], writes=[bsm])
            dve.op(lambda e: e.scalar_tensor_tensor(out=h2[:], in0=x1[:], scalar=sm[:, 1:2], in1=A2[:], op0=ALU.mult, op1=ALU.mult),
                   reads=[bx1, bsm, bA2], writes=[bh2])
            pool.op(lambda e: e.tensor_tensor(out=h2[:], in0=h2[:], in1=B2[:], op=ALU.add), reads=[bh2, bB2], writes=[bh2])
            act.op(lambda e: e.copy(out=h2b[:], in_=h2[:]), reads=[bh2], writes=[bh2b])
            act.dma(lambda e: e.dma_start(out=H2_d[tt * 128:(tt + 1) * 128, :], in_=h2b[:]), bh2b, k.db("H2_d"), bh2b)
            for g0 in range(0, KD, 4):
                n4 = min(4, KD - g0)
                ps, bps = pss.next()
                for j in range(n4):
                    kc = g0 + j
                    pe.op(lambda e: e.transpose(out=ps[:, j * 128:(j + 1) * 128], in_=h2[:, kc * 128:(kc + 1) * 128], identity=identf[:]),
                          reads=[bh2, b_identf], writes=[bps], partial=(j > 0), signal=(j == n4 - 1))
                dve.op(lambda e: e.tensor_copy(out=h2T[:, g0:g0 + n4, :], in_=ps[:, 0:n4 * 128].rearrange("p (k n) -> p k n", n=128)),
                       reads=[bps], writes=[bh2T], partial=True)
            ps, bps = pss.next()
            for kc in range(KD):
                pe.op(lambda e: e.matmul(ps[:, 0:NR], lhsT=h2T[:, kc, :], rhs=wr[:, kc, :], start=(kc == 0), stop=False),
                      reads=[bh2T, bwr], writes=[bps], partial=(kc > 0), signal=False)
            pe.op(lambda e: e.matmul(ps[:, 0:NR], lhsT=ones_f[0:1, :], rhs=brr[0:1, :], start=False, stop=True), reads=[b_ones, bbrr], writes=[bps], partial=True)
            dve.op(lambda e: e.tensor_copy(out=lg[:], in_=ps[:, 0:NR]), reads=[bps], writes=[blg])
            gl = lg[:, 0:NG]; el = lg[:, NG:NR].rearrange("p (g e) -> p g e", e=EPG)
            ohg = rt[:, 0, 0:NG]; eg = rt[:, 6, 0:NG]
            R = lambda fn, **kw: dve.op(fn, reads=[blg, brt, bsm], writes=[brt, bsm], **kw)
            R(lambda e: e.reduce_max(out=sm[:, 2:3], in_=gl, axis=AX.X))
            R(lambda e: e.tensor_scalar(out=ohg, in0=gl, scalar1=sm[:, 2:3], scalar2=None, op0=ALU.is_ge))
            R(lambda e: e.tensor_scalar(out=sm[:, 3:4], in0=sm[:, 2:3], scalar1=-1.0, scalar2=None, op0=ALU.mult))
            act.op(lambda e: e.activation(out=eg, in_=gl, func=AF.Exp, bias=sm[:, 3:4], scale=1.0, accum_out=sm[:, 4:5]), reads=[blg, bsm], writes=[brt, bsm])
            R(lambda e: e.reciprocal(out=sm[:, 5:6], in_=sm[:, 4:5]))
            tmp = rt[:, 1, :].rearrange("p (g e) -> p g e", e=EPG)
            R(lambda e: e.tensor_tensor(out=tmp, in0=el, in1=ohg.unsqueeze(2).to_broadcast([128, NG, EPG]), op=ALU.mult))
            sel = rt[:, 2, 0:EPG]; mk1 = rt[:, 3, 0:EPG]; sel2 = rt[:, 4, 0:EPG]; mk2 = rt[:, 5, 0:EPG]
            R(lambda e: e.tensor_reduce(out=sel, in_=tmp.rearrange("p g e -> p e g"), axis=AX.X, op=ALU.add))
            R(lambda e: e.reduce_max(out=sm[:, 6:7], in_=sel, axis=AX.X))
            R(lambda e: e.tensor_scalar(out=mk1, in0=sel, scalar1=sm[:, 6:7], scalar2=None, op0=ALU.is_ge))
            R(lambda e: e.scalar_tensor_tensor(out=sel2, in0=mk1, scalar=-1e30, in1=sel, op0=ALU.mult, op1=ALU.add))
            R(lambda e: e.reduce_max(out=sm[:, 7:8], in_=sel2, axis=AX.X))
            R(lambda e: e.tensor_scalar(out=mk2, in0=sel2, scalar1=sm[:, 7:8], scalar2=None, op0=ALU.is_ge))
            R(lambda e: e.tensor_tensor(out=sm[:, 8:9], in0=sm[:, 7:8], in1=sm[:, 6:7], op=ALU.subtract))
            act.op(lambda e: e.activation(out=sm[:, 9:10], in_=sm[:, 8:9], func=AF.Exp), reads=[bsm], writes=[bsm])
            R(lambda e: e.tensor_scalar(out=sm[:, 10:11], in0=sm[:, 9:10], scalar1=1.0, scalar2=None, op0=ALU.add))
            R(lambda e: e.reciprocal(out=sm[:, 10:11], in_=sm[:, 10:11]))
            dve.op(lambda e: e.tensor_tensor(out=wsel[:, tt, 0:1], in0=sm[:, 5:6], in1=sm[:, 10:11], op=ALU.mult), reads=[bsm], writes=[bwsel], partial=True)
            dve.op(lambda e: e.tensor_tensor(out=wsel[:, tt, 1:2], in0=wsel[:, tt, 0:1], in1=sm[:, 9:10], op=ALU.mult), reads=[bsm, bwsel], writes=[bwsel], partial=True)
            bo = lambda a: a.unsqueeze(2).to_broadcast([128, NG, EPG])
            be_ = lambda a: a.unsqueeze(1).to_broadcast([128, NG, EPG])
            v3 = lambda a: a.rearrange("p (g e) -> p g e", e=EPG)
            dve.op(lambda e: e.tensor_tensor(out=v3(I1_all[:, tt, :]), in0=bo(ohg), in1=be_(mk1), op=ALU.mult), reads=[brt], writes=[bI1], partial=True)
            dve.op(lambda e: e.tensor_tensor(out=v3(I2_all[:, tt, :]), in0=bo(ohg), in1=be_(mk2), op=ALU.mult), reads=[brt], writes=[bI2], partial=True)
            dve.op(lambda e: e.tensor_tensor(out=Ind_all[:, tt, :], in0=I1_all[:, tt, :], in1=I2_all[:, tt, :], op=ALU.add), reads=[bI1, bI2], writes=[bInd], partial=True)
        cnt = S_("cnt", [128, 4, NE], F32, ph); bcnt = Buf("cnt")
        ps, bps = pss.next()
        for tt in range(NT):
            pe.op(lambda e: e.matmul(ps[:, 0:NE], lhsT=ones_b[:], rhs=Ind_all[:, tt, :], start=(tt == 0), stop=(tt == NT - 1)),
                  reads=[b_onesb, bInd], writes=[bps], partial=(tt > 0), signal=(tt == NT - 1))
        C = lambda fn, **kw: dve.op(fn, reads=[bcnt], writes=[bcnt], **kw)
        dve.op(lambda e: e.tensor_copy(out=cnt[:, 0, :], in_=ps[:, 0:NE]), reads=[bps], writes=[bcnt])
        thri = S_("thri", [128, NT], I32, ph); bthri = Buf("thri")
        pool.op(lambda e: e.iota(out=thri[:], pattern=[[128, NT]], base=0, channel_multiplier=0), writes=[bthri])
        thrf = S_("thrf", [128, NT], F32, ph); bthrf = Buf("thrf")
        dve.op(lambda e: e.tensor_copy(out=thrf[:], in_=thri[:]), reads=[bthri], writes=[bthrf])
        cmpc = S_("cmpc", [128, NE, NT], F32, ph); bcmpc = Buf("cmpc")
        dve.op(lambda e: e.tensor_tensor(out=cmpc[:], in0=cnt[:, 0, :].unsqueeze(2).to_broadcast([128, NE, NT]),
                                         in1=thrf[:].unsqueeze(1).to_broadcast([128, NE, NT]), op=ALU.is_gt), reads=[bcnt, bthrf], writes=[bcmpc])
        dve.op(lambda e: e.reduce_sum(out=cnt[:, 1, :], in_=cmpc[:], axis=AX.X), reads=[bcmpc], writes=[bcnt])
        C(lambda e: e.tensor_scalar(out=cnt[:, 1, :], in0=cnt[:, 1, :], scalar1=128.0, scalar2=None, op0=ALU.mult))
        ps, bps = pss.next()
        pe.op(lambda e: e.transpose(out=ps[0:NE, 0:128], in_=cnt[:, 1, :], identity=identf[:]), reads=[bcnt, b_identf], writes=[bps])
        pcT = S_("pcT", [128, 128], F32, ph); bpcT = Buf("pcT")
        dve.op(lambda e: e.tensor_copy(out=pcT[0:NE, :], in_=ps[0:NE, 0:128]), reads=[bps], writes=[bpcT])
        ps, bps = pss.next()
        pe.op(lambda e: e.matmul(ps[:, 0:NE], lhsT=pcT[0:NE, :], rhs=Uf[0:NE, 0:NE], start=True, stop=True), reads=[bpcT, bUf], writes=[bps])
        dve.op(lambda e: e.tensor_copy(out=cnt[:, 2, :], in_=ps[:, 0:NE]), reads=[bps], writes=[bcnt])
        C(lambda e: e.tensor_tensor(out=cnt[:, 3, :], in0=cnt[:, 2, :], in1=cnt[:, 1, :], op=ALU.add))
        posf = S_("posf", [128, NT, 2], F32, ph); bposf = Buf("posf")
        pz = S_("pz", [128, 2, NE], F32, ph); bpz = Buf("pz")
        for tt in range(NT):
            ps, bps = pss.next()
            for t2 in range(tt):
                pe.op(lambda e: e.matmul(ps[:, 0:NE], lhsT=ones_b[:], rhs=Ind_all[:, t2, :], start=(t2 == 0), stop=False),
                      reads=[b_onesb, bInd], writes=[bps], partial=(t2 > 0), signal=False)
            pe.op(lambda e: e.matmul(ps[:, 0:NE], lhsT=Ub[:], rhs=Ind_all[:, tt, :], start=(tt == 0), stop=True), reads=[bUb, bInd], writes=[bps], partial=(tt > 0))
            dve.op(lambda e: e.tensor_tensor(out=pz[:, 0, :], in0=ps[:, 0:NE], in1=cnt[:, 2, :], op=ALU.add), reads=[bps, bcnt], writes=[bpz])
            dve.op(lambda e: e.tensor_tensor(out=pz[:, 1, :], in0=pz[:, 0, :], in1=I1_all[:, tt, :], op=ALU.mult), reads=[bpz, bI1], writes=[bpz])
            dve.op(lambda e: e.reduce_sum(out=posf[:, tt, 0:1], in_=pz[:, 1, :], axis=AX.X), reads=[bpz], writes=[bposf], partial=True)
            dve.op(lambda e: e.tensor_tensor(out=pz[:, 1, :], in0=pz[:, 0, :], in1=I2_all[:, tt, :], op=ALU.mult), reads=[bpz, bI2], writes=[bpz])
            dve.op(lambda e: e.reduce_sum(out=posf[:, tt, 1:2], in_=pz[:, 1, :], axis=AX.X), reads=[bpz], writes=[bposf], partial=True)
        dve.op(lambda e: e.tensor_copy(out=idx_all[:], in_=posf[:]), reads=[bposf], writes=[bidx])
        bsi = S_("bsi", [128, NBLK], I32, ph); bbsi = Buf("bsi")
        pool.op(lambda e: e.iota(out=bsi[:], pattern=[[128, NBLK]], base=0, channel_multiplier=0), writes=[bbsi])
        bsf = S_("bsf", [128, NBLK], F32, ph); bbsf = Buf("bsf")
        dve.op(lambda e: e.tensor_copy(out=bsf[:], in_=bsi[:]), reads=[bbsi], writes=[bbsf])
        pidi = S_("pidi", [128, 1], I32, ph); bpidi = Buf("pidi")
        pool.op(lambda e: e.iota(out=pidi[:], pattern=[[0, 1]], base=0, channel_multiplier=1), writes=[bpidi])
        pidf = S_("pidf", [128, 1], F32, ph); bpidf = Buf("pidf")
        dve.op(lambda e: e.tensor_copy(out=pidf[:], in_=pidi[:]), reads=[bpidi], writes=[bpidf])
        cmp_ = S_("cmp", [128, NBLK, NE], F32, ph); bcmp = Buf("cmp")
        dve.op(lambda e: e.tensor_tensor(out=cmp_[:], in0=cnt[:, 3, :].unsqueeze(1).to_broadcast([128, NBLK, NE]),
                                         in1=bsf[:].unsqueeze(2).to_broadcast([128, NBLK, NE]), op=ALU.is_le), reads=[bcnt, bbsf], writes=[bcmp])
        bef = S_("bef", [128, NBLK], F32, ph); bbef = Buf("bef")
        dve.op(lambda e: e.reduce_sum(out=bef[:], in_=cmp_[:], axis=AX.X), reads=[bcmp], writes=[bbef])
        dve.op(lambda e: e.tensor_scalar(out=bef[:], in0=bef[:], scalar1=float(NE - 1), scalar2=None, op0=ALU.min), reads=[bbef], writes=[bbef])
        wq = S_("wq", [128, 3, NBLK], F32, ph); bwq = Buf("wq")
        for q in range(NP):
            dve.op(lambda e: e.tensor_scalar(out=wq[:, 0, :], in0=bef[:], scalar1=float(q * EPP), scalar2=None, op0=ALU.is_ge), reads=[bbef], writes=[bwq])
            dve.op(lambda e: e.tensor_scalar(out=wq[:, 1, :], in0=bef[:], scalar1=float((q + 1) * EPP), scalar2=None, op0=ALU.is_lt), reads=[bbef], writes=[bwq], partial=True)
            dve.op(lambda e: e.tensor_tensor(out=wq[:, 0, :], in0=wq[:, 0, :], in1=wq[:, 1, :], op=ALU.mult), reads=[bwq], writes=[bwq])
            dve.op(lambda e: e.tensor_scalar(out=wq[:, 1, :], in0=bef[:], scalar1=128.0, scalar2=pidf[:, 0:1], op0=ALU.mult, op1=ALU.add),
                   reads=[bbef, bpidf, bwq], writes=[bwq])
            dve.op(lambda e: e.tensor_scalar(out=wq[:, 1, :], in0=wq[:, 1, :], scalar1=-float(q * EPP * 128) - 1.0e6, scalar2=None, op0=ALU.add), reads=[bwq], writes=[bwq])
            dve.op(lambda e: e.tensor_tensor(out=wq[:, 1, :], in0=wq[:, 1, :], in1=wq[:, 0, :], op=ALU.mult), reads=[bwq], writes=[bwq])
            dve.op(lambda e: e.tensor_scalar(out=wq[:, 1, :], in0=wq[:, 1, :], scalar1=1.0e6, scalar2=None, op0=ALU.add), reads=[bwq], writes=[bwq])
            for si in range(NSP_):
                dve.op(lambda e: e.tensor_scalar(out=wq[:, 2, :], in0=wq[:, 1, :], scalar1=float(NSP_), scalar2=float(si), op0=ALU.mult, op1=ALU.add),
                       reads=[bwq], writes=[bwq])
                dve.op(lambda e: e.tensor_copy(out=widx_i[:, q * NSP_ + si, :], in_=wq[:, 2, :]), reads=[bwq], writes=[bwidx], partial=(q + si > 0))
        if "dbg_route" in debug_outs:
            for nm, t_, b_, shp, dt_ in (("dbg_idx", idx_all, bidx, [128, NT * 2], I32), ("dbg_widx", widx_i, bwidx, [128, 4 * NSP_ * NBLK], I32),
                                         ("dbg_wsel", wsel, bwsel, [128, NT * 2], F32)):
                o_ = nc.dram_tensor(nm, shp, dt_, kind="ExternalOutput").ap(); k.dbufs[nm] = Buf(nm)
                src_ = t_[:].rearrange("p a b -> p (a b)") if len(t_.shape) == 3 else t_[:]
                sp.dma(lambda e: e.dma_start(out=o_, in_=src_), b_, k.db(nm), b_)
        sc.barrier()
    if upto <= 9:
        return

    NSP = max(1, (KD * DE * 4 + 32767) // 32768)
    assert NSP == max(1, (HC * D * 4 + 32767) // 32768) and KD % NSP == 0 and HC % NSP == 0
    bnd_reg = nc.gpsimd.to_reg(EPP * 128 * NSP - 1)
    w1v = [w.rearrange("(r c) f -> r (c f)", c=KD // NSP) for w in w1p]; w3v = [w.rearrange("(r c) f -> r (c f)", c=KD // NSP) for w in w3p]
    w2v = [w.rearrange("(r c) f -> r (c f)", c=HC // NSP) for w in w2p]
    with ExitStack() as ph:
        zt = S_("zt", [128, D], BF16, ph); bzt = Buf("zt")
        dve.op(lambda e: e.memset(zt[:], 0.0), writes=[bzt])
        for b in range(NBLK):
            sp.dma(lambda e: e.dma_start(out=Xs_d[b * 128:(b + 1) * 128, :], in_=zt[:]), bzt, k.db("Xs_d"), bzt)
        hbs = Ring("sh2", [128, D], BF16, 2, ph)
        for tt in range(NT):
            hb, bhb = hbs.next()
            sp.dma(lambda e: e.dma_start(out=hb[:], in_=H2_d[tt * 128:(tt + 1) * 128, :]), k.db("H2_d"), bhb, bhb, partial=False)
            for kk in range(2):
                pool.wait(dict(bidx.w));
                pool.dma(lambda e: e.indirect_dma_start(out=Xs_d[:, :], out_offset=bass.IndirectOffsetOnAxis(ap=idx_all[:, tt, kk:kk + 1], axis=0),
                                                        in_=hb[:, :], in_offset=None), bhb, k.db("Xs_d"), bhb, partial=False)
        w1s = S_("w1s", [128, KD * DE], BF16, ph); bw1 = Buf("w1s")
        w3s = S_("w3s", [128, KD * DE], BF16, ph); bw3 = Buf("w3s")
        w2s = S_("w2s", [128, HC * D], BF16, ph); bw2 = Buf("w2s")
        xbs = Ring("xb", [128, D], BF16, 2, ph); xts = Ring("xbT", [128, KD, 128], BF16, 2, ph)
        hid = S_("hidT", [128, HC, 128], BF16, ph); bhid = Buf("hidT")
        sl = Ring("sil", [128, 128], F32, 2, ph)
        yb = S_("yb", [128, D], F32, ph); byb = Buf("yb")
        pst = Ring("bpt", [128, 1024], BF16, 2, ph, psum=True)
        pss = Ring("bps", [128, 512], F32, 4, ph, psum=True)
        for b in range(NBLK):
            pool.wait(dict(bwidx.w))
            for (ws, bw, wv, nm, rowlen) in ((w1s, bw1, w1v, "w1", KD * DE), (w3s, bw3, w3v, "w3", KD * DE), (w2s, bw2, w2v, "w2", HC * D)):
                cw = rowlen // NSP
                for q in range(NP):
                    for si in range(NSP):
                        pool.dma(lambda e: e.indirect_dma_start(out=ws[:, si * cw:(si + 1) * cw], out_offset=None, in_=wv[q][:, :],
                                                                in_offset=bass.IndirectOffsetOnAxis(ap=widx_i[:, q * NSP + si, b:b + 1], axis=0),
                                                                bounds_check=bnd_reg, oob_is_err=False),
                                 k.db("%s_%d" % (nm, q)), bw, bw, partial=False)
            xb, bxb = xbs.next(); xT, bxT = xts.next()
            sp.dma(lambda e: e.dma_start(out=xb[:], in_=Xs_d[b * 128:(b + 1) * 128, :]), k.db("Xs_d"), bxb, bxb, partial=False)
            for g0 in range(0, KD, 8):
                n8 = min(8, KD - g0)
                ps, bps = pst.next()
                for j in range(n8):
                    cc = g0 + j
                    pe.op(lambda e: e.transpose(out=ps[:, j * 128:(j + 1) * 128], in_=xb[:, cc:D:KD], identity=ident[:]),
                          reads=[bxb, b_ident], writes=[bps], partial=(j > 0), signal=(j == n8 - 1))
                dve.op(lambda e: e.tensor_copy(out=xT[:, g0:g0 + n8, :], in_=ps[:, 0:n8 * 128].rearrange("p (k n) -> p k n", n=128)),
                       reads=[bps], writes=[bxT], partial=True)
            for c2 in range(HC):
                p1, bp1 = pss.next(); p3, bp3 = pss.next()
                for (pp, bpp, ws, bw) in ((p1, bp1, w1s, bw1), (p3, bp3, w3s, bw3)):
                    for cc in range(KD):
                        pe.op(lambda e: e.matmul(pp[:, 0:128], lhsT=ws[:, cc * DE + c2:(cc + 1) * DE:HC], rhs=xT[:, cc, :], start=(cc == 0), stop=(cc == KD - 1)),
                              reads=[bw, bxT], writes=[bpp], partial=(cc > 0), signal=(cc == KD - 1))
                s_, bs_ = sl.next()
                act.op(lambda e: e.activation(out=s_[:], in_=p1[:, 0:128], func=AF.Silu), reads=[bp1], writes=[bs_])
                dve.op(lambda e: e.tensor_tensor(out=hid[:, c2, :], in0=p3[:, 0:128], in1=s_[:], op=ALU.mult), reads=[bp3, bs_], writes=[bhid], partial=(c2 > 0))
            for gi, g0 in enumerate(range(0, D, 512)):
                gw = min(512, D - g0)
                ps, bps = pss.next()
                for c2 in range(HC):
                    pe.op(lambda e: e.matmul(ps[:, 0:gw], lhsT=hid[:, c2, :], rhs=w2s[:, c2 * D + g0:c2 * D + g0 + gw], start=(c2 == 0), stop=(c2 == HC - 1)),
                          reads=[bhid, bw2], writes=[bps], partial=(c2 > 0), signal=(c2 == HC - 1))
                if gi % 2 == 0:
                    dve.op(lambda e: e.tensor_copy(out=yb[:, g0:g0 + gw], in_=ps[:, 0:gw]), reads=[bps], writes=[byb], partial=(gi > 0))
                else:
                    act.op(lambda e: e.copy(out=yb[:, g0:g0 + gw], in_=ps[:, 0:gw]), reads=[bps], writes=[byb], partial=True)
            act.dma(lambda e: e.dma_start(out=Ys_d[b * 128:(b + 1) * 128, :], in_=yb[:]), byb, k.db("Ys_d"), byb)
        sc.barrier()
    if upto <= 10:
        return

    with ExitStack() as ph:
        pss = Ring("fps", [128, 512], F32, 2, ph, psum=True)
        gt2 = S_("gt2", [128, D], F32, ph); bgt2 = Buf("gt2"); gfb = S_("gfb", [128, D], F32, ph); bgfb = Buf("gfb")
        sp.dma(lambda e: e.dma_start(out=gt2[:], in_=mod_bc[:, 3 * D:4 * D]), k.db("mod_bc"), bgt2, bgt2)
        bcast_row(g_final, "g_final", gfb, bgfb, ph, pss)
        y0 = S_("y0", [128, D], F32, ph); by0 = Buf("y0"); y1 = S_("y1", [128, D], F32, ph); by1 = Buf("y1")
        x1s = Ring("fx1", [128, D], F32, 2, ph)
        fo = S_("fo", [128, D], F32, ph); bfo = Buf("fo")
        fjk = S_("fjk", [128, D], BF16, ph); bfjk = Buf("fjk")
        fs = S_("fs", [128, 2], F32, ph); bfs = Buf("fs")
        for tt in range(NT):
            x1, bx1 = x1s.next()
            sp.dma(lambda e: e.dma_start(out=x1[:], in_=X1_d[tt * 128:(tt + 1) * 128, :]), k.db("X1_d"), bx1, bx1, partial=False)
            pool.wait(dict(bidx.w))
            pool.dma(lambda e: e.indirect_dma_start(out=y0[:, :], out_offset=None, in_=Ys_d[:, :],
                                                    in_offset=bass.IndirectOffsetOnAxis(ap=idx_all[:, tt, 0:1], axis=0)), k.db("Ys_d"), by0, by0, partial=False)
            pool.dma(lambda e: e.indirect_dma_start(out=y1[:, :], out_offset=None, in_=Ys_d[:, :],
                                                    in_offset=bass.IndirectOffsetOnAxis(ap=idx_all[:, tt, 1:2], axis=0)), k.db("Ys_d"), by1, by1, partial=False)
            dve.op(lambda e: e.tensor_scalar(out=y0[:], in0=y0[:], scalar1=wsel[:, tt, 0:1], scalar2=None, op0=ALU.mult), reads=[by0, bwsel], writes=[by0])
            dve.op(lambda e: e.scalar_tensor_tensor(out=y0[:], in0=y1[:], scalar=wsel[:, tt, 1:2], in1=y0[:], op0=ALU.mult, op1=ALU.add),
                   reads=[by1, by0, bwsel], writes=[by0])
            pool.op(lambda e: e.tensor_tensor(out=y0[:], in0=y0[:], in1=gt2[:], op=ALU.mult), reads=[by0, bgt2], writes=[by0])
            dve.op(lambda e: e.tensor_tensor(out=fo[:], in0=y0[:], in1=x1[:], op=ALU.add), reads=[by0, bx1], writes=[bfo])
            act.op(lambda e: e.activation(out=fjk[:], in_=fo[:], func=AF.Square, accum_out=fs[:, 0:1]), reads=[bfo], writes=[bfjk, bfs])
            dve.op(lambda e: e.tensor_scalar(out=fs[:, 1:2], in0=fs[:, 0:1], scalar1=1.0 / D, scalar2=1e-6, op0=ALU.mult, op1=ALU.add), reads=[bfs], writes=[bfs])
            act.op(lambda e: e.activation(out=fs[:, 1:2], in_=fs[:, 1:2], func=AF.Ln), reads=[bfs], writes=[bfs])
            act.op(lambda e: e.activation(out=fs[:, 1:2], in_=fs[:, 1:2], func=AF.Exp, scale=-0.5), reads=[bfs], writes=[bfs])
            dve.op(lambda e: e.scalar_tensor_tensor(out=fo[:], in0=fo[:], scalar=fs[:, 1:2], in1=gfb[:], op0=ALU.mult, op1=ALU.mult),
                   reads=[bfo, bfs, bgfb], writes=[bfo])
            sp.dma(lambda e: e.dma_start(out=out[tt * 128:(tt + 1) * 128, :], in_=fo[:]), bfo, k.db("out"), bfo)
        sc.barrier()


def rope_tables(cfg, half):
    c = cfg
    pos = np.concatenate([np.arange(c.NQ) + half * c.NQ, np.arange(c.NQ) + (1 - half) * c.NQ])
    row = (pos // c.GW).astype(np.float64); col = (pos % c.GW).astype(np.float64)
    def tab(rot):
        q = rot // 4
        inv = 10000.0 ** (-np.arange(q, dtype=np.float64) / q)
        ang = np.concatenate([row[:, None] * inv, col[:, None] * inv], axis=-1)
        hd = rot // 2
        cs = np.ones((rot, c.NK), np.float32); sn = np.zeros((rot, c.NK), np.float32)
        cs[:hd, :c.S] = np.cos(ang).T; cs[hd:, :c.S] = np.cos(ang).T
        sn[:hd, :c.S] = -np.sin(ang).T; sn[hd:, :c.S] = np.sin(ang).T
        return cs, sn
    cd, sd = tab(128); cm, sm = tab(64)
    return dict(cosd=cd, sind=sd, cosm=cm, sinm=sm)


def weight_pieces(c, inp):
    g = lambda n: np.asarray(inp[n][0]).reshape(-1, np.asarray(inp[n]).shape[-1])
    out = {}
    wm = g("w_mod")
    out["w_mod_0"] = wm[:, :3 * c.D]; out["w_mod_1"] = wm[:, 3 * c.D:]
    for n in ("w_in", "w_uq", "w_ukv", "w_br_a", "w_br_b", "w_out"):
        out[n] = g(n)
    epp = c.NE // 4
    for n, rpe in (("w1", c.D), ("w3", c.D), ("w2", c.DE)):
        w = g(n)
        for q in range(4):
            out["%s_%d" % (n, q)] = w[q * epp * rpe:(q + 1) * epp * rpe]
    return out


SHARD_POS = {0: 0, 4: 1, 1: 2, 5: 3, 2: 4, 6: 5, 3: 6, 7: 7}


GATHER_CHUNK_BYTES = 512 * 1024


def gather_unit(rows, cols):
    r8 = rows // 8
    u = max(1, min(r8, (GATHER_CHUNK_BYTES) // (cols * 4)))
    while r8 % u:
        u -= 1
    return u


def prep_core_inputs(cfg, inp, core, gather):
    c = cfg
    b, half = (core // 2, core % 2) if c.NCORES > 1 else (0, 0)
    f = lambda a: np.ascontiguousarray(np.asarray(a, dtype=np.float32))
    x = np.asarray(inp["x"][b]); ctx = np.asarray(inp["ctx"][b])
    own = x[half * c.NQ:(half + 1) * c.NQ]; oth = x[(1 - half) * c.NQ:(2 - half) * c.NQ]
    m = dict(xk=f(np.concatenate([own, oth, ctx], axis=0)), c_b=f(inp["c"][b:b + 1]), c_ctx=f(np.asarray(inp["c_ctx"])[None, :]))
    m.update(rope_tables(c, half))
    for n in ("b_mod", "g_attn", "g_q", "g_kv", "g_subln", "g_ffn", "lam_q1", "lam_k1", "lam_q2", "lam_k2"):
        m[n] = f(np.asarray(inp[n]).reshape(1, -1))
    m["g_final"] = f(np.asarray(inp["g_final"]).reshape(1, -1))
    m["w_r"] = f(np.concatenate([np.asarray(inp["w_grp"][0]), np.asarray(inp["w_exp"][0])], axis=1))
    m["b_r"] = f(np.concatenate([np.asarray(inp["b_grp"][0]), np.asarray(inp["b_exp"][0])])[None, :])
    for n, w2 in weight_pieces(c, inp).items():
        if gather:
            rows, cols = w2.shape
            u = gather_unit(rows, cols); nch = rows // 8 // u
            t_ = SHARD_POS[core]
            m[n + "_sh"] = f(w2.reshape(nch, 8, u, cols)[:, t_].reshape(nch * u, cols))
        else:
            m[n] = f(w2)
    return m


_NC_CACHE = {}


def kernel(**inputs):
    cfg = Cfg()
    n = cfg.NCORES
    if "nc" not in _NC_CACHE:
        _NC_CACHE["nc"] = build_program(cfg, gather=True)
    nc = _NC_CACHE["nc"]
    need = set()
    for a in nc.allocations:
        try:
            if a.kind == "ExternalInput":
                need.add(a.memorylocations[0].name)
        except Exception:
            pass
    in_maps = []
    for core in range(n):
        m = prep_core_inputs(cfg, inputs, core, gather=True)
        in_maps.append({k_: v for k_, v in m.items() if k_ in need})
    res = run_bass_kernel_spmd(nc, in_maps, core_ids=list(range(n)))
    out = np.empty((cfg.B, cfg.S, cfg.D), np.float32)
    for core in range(n):
        b, half = core // 2, core % 2
        out[b, half * cfg.NQ:(half + 1) * cfg.NQ] = res.results[core]["out"]
    return out
```

```python
import math
from contextlib import ExitStack
import numpy as np
import concourse.bass as bass
import concourse.mybir as mybir
from concourse.bass_utils import run_bass_kernel_spmd

F32 = mybir.dt.float32
BF16 = mybir.dt.bfloat16
I32 = mybir.dt.int32
AF = mybir.ActivationFunctionType
ALU = mybir.AluOpType
AX = mybir.AxisListType


class Cfg:
    def __init__(self, **kw):
        self.D = 4096; self.B = 4; self.S = 4096; self.GW = 64; self.C = 256
        self.DAH = 8; self.MH = 16; self.QL = 1024; self.KVL = 512
        self.NG = 8; self.EPG = 8; self.DE = 512; self.NCORES = 8
        self.lambda_init = 0.8 - 0.6 * math.exp(-0.3 * 0)
        for k, v in kw.items():
            setattr(self, k, v)
        self.KD = self.D // 128
        self.NQ = self.S // 2
        self.NK = self.S + self.C
        self.NE = self.NG * self.EPG
        self.DAW = self.DAH * 256
        self.MW = self.MH * 128
        self.INC = 3 * self.DAW + self.QL + self.KVL + 64 + 2 * self.D
        self.o_dq = 0; self.o_dk = self.DAW; self.o_dv = 2 * self.DAW
        self.o_cq = 3 * self.DAW; self.o_ckv = self.o_cq + self.QL
        self.o_ga = self.o_ckv + self.KVL + 64; self.o_gb = self.o_ga + self.D


class Buf:
    def __init__(self, name, sem=None):
        self.name = name
        self.w = {}
        self.r = {}
        self.sem = sem
        self.ndma = 0


def _merge(d, s):
    for k, v in s.items():
        if d.get(k, 0) < v:
            d[k] = v


class Eng:
    def __init__(self, sched, name, e, sem):
        self.s = sched; self.name = name; self.e = e; self.sem = sem
        self.cnt = 0
        self.seen = {}
        self.pend_r = []; self.pend_w = []

    def wait(self, deps):
        for k, v in deps.items():
            if self.seen.get(k, 0) < v:
                self.e.wait_ge(self.s.sems[k], v)
                self.seen[k] = v

    def _deps(self, reads, writes, partial):
        deps = {}
        for t in reads:
            _merge(deps, t.w)
        for t in writes:
            _merge(deps, t.r)
            if not partial:
                _merge(deps, t.w)
        return deps

    def op(self, fn, reads=(), writes=(), partial=False, signal=True):
        self.wait(self._deps(reads, writes, partial))
        ins = fn(self.e)
        self.pend_r += list(reads); self.pend_w += list(writes)
        if signal:
            self.cnt += 1
            ins.then_inc(self.sem_h, 1)
            ev = {self.sem: self.cnt}
            self.s.last[self.sem] = self.cnt
            for t in self.pend_r:
                _merge(t.r, ev)
            for t in self.pend_w:
                _merge(t.w, ev)
            self.pend_r = []; self.pend_w = []
        return ins

    def dma(self, fn, src, dst, via, partial=True, inc=16):
        self.wait(self._deps([src], [dst], partial))
        ins = fn(self.e)
        k = self.s.sem_of(via, self.name == 'pool')
        self.s.semcnt[k] = self.s.semcnt.get(k, 0) + inc
        ins.then_inc(self.s.sems[k], inc)
        ev = {k: self.s.semcnt[k]}
        self.s.last[k] = self.s.semcnt[k]
        _merge(src.r, ev)
        _merge(dst.w, ev)
        return ins


class Sched:
    def __init__(self, nc, stack):
        self.nc = nc
        self.sems = {}
        self.last = {}
        self.stack = stack
        self.n = 0
        self.dpool = []
        self.dpool_sw = []
        self.n_sw = 0
        self.semcnt = {}
        self.pe = self._eng("pe", nc.tensor)
        self.act = self._eng("act", nc.scalar)
        self.dve = self._eng("dve", nc.vector)
        self.pool = self._eng("pool", nc.gpsimd)
        self.sp = self._eng("sp", nc.sync)
        self.engs = [self.pe, self.act, self.dve, self.pool, self.sp]
        self.ccsem = self._newsem('ccsem'); self.ccn = 0

    def _newsem(self, name):
        h = self.stack.enter_context(self.nc.semaphore(name))
        k = name
        self.sems[k] = h
        return k

    def _eng(self, name, e):
        k = self._newsem("e_" + name)
        en = Eng(self, name, e, k)
        en.sem_h = self.sems[k]
        return en

    def sem_of(self, buf, sw=False):
        key = "sem_sw" if sw else "sem"
        if getattr(buf, key, None) is None:
            pl = self.dpool_sw if sw else self.dpool
            if len(pl) < 40:
                pl.append(self._newsem(("s%d" if sw else "d%d") % len(pl)))
            cnt = self.n_sw if sw else self.n
            setattr(buf, key, pl[cnt % 40])
            if sw:
                self.n_sw += 1
            else:
                self.n += 1
        return getattr(buf, key)

    def barrier(self, engs=None):
        for en in (engs or self.engs):
            en.wait(dict(self.last))


class K:
    def __init__(self, cfg, gather):
        self.cfg = cfg
        self.gather = gather
        self.nc = bass.Bass("TRN2", target_bir_lowering=False)
        self.ins = {}
        self.dbufs = {}

    def dram(self, name, shape, dt, kind="Internal"):
        t = self.nc.dram_tensor(name, list(shape), dt, kind=kind).ap()
        self.dbufs[name] = Buf(name)
        return t

    def db(self, name):
        return self.dbufs[name]


def build_program(cfg, gather=True, upto=99, debug_outs=()):
    k = K(cfg, gather)
    nc = k.nc
    c = cfg
    D, KD, NQ, NK, S, C = c.D, c.KD, c.NQ, c.NK, c.S, c.C
    with ExitStack() as top:
        sc = Sched(nc, top)
        pe, act, dve, pool, sp = sc.pe, sc.act, sc.dve, sc.pool, sc.sp
        _build(k, sc, top, upto, debug_outs)
    return nc


def _build(k, sc, top, upto, debug_outs):
    nc = k.nc; c = k.cfg
    D, KD, NQ, NK, S, C = c.D, c.KD, c.NQ, c.NK, c.S, c.C
    pe, act, dve, pool, sp = sc.pe, sc.act, sc.dve, sc.pool, sc.sp
    dbg = lambda n: ("ExternalOutput" if n in debug_outs else "Internal")

    def inp(name, shape, dt=F32):
        t = nc.dram_tensor(name, list(shape), dt, kind="ExternalInput").ap()
        k.dbufs[name] = Buf(name)
        return t

    def scratch(name, shape, dt):
        return k.dram(name, shape, dt, kind=dbg(name))

    pending_gathers = []

    def gather_group():
        grp = list(pending_gathers); del pending_gathers[:]
        for (name, shi, pair, full, u, nch) in grp:
            pool.wait(dict(k.db(name + "_shi").w))
        for (name, shi, pair, full, u, nch) in grp:
            for i in range(nch):
                ins = pool.e.collective_compute("AllGather", ALU.bypass, replica_groups=[[0, 4], [1, 5], [2, 6], [3, 7]],
                                                ins=[shi[i * u:(i + 1) * u, :]], outs=[pair[i * 2 * u:(i + 1) * 2 * u, :]])
                sc.ccn += 1
                ins.then_inc(sc.sems[sc.ccsem], 1)
        for (name, shi, pair, full, u, nch) in grp:
            for i in range(nch):
                ins = pool.e.collective_compute("AllGather", ALU.bypass, replica_groups=[[0, 1, 2, 3], [4, 5, 6, 7]],
                                                ins=[pair[i * 2 * u:(i + 1) * 2 * u, :]], outs=[full[i * 8 * u:(i + 1) * 8 * u, :]])
                sc.ccn += 1
                ins.then_inc(sc.sems[sc.ccsem], 1)
            sc.last[sc.ccsem] = sc.ccn
            _merge(k.db(name).w, {sc.ccsem: sc.ccn})

    def weight(name, rows, cols):
        if not k.gather:
            return inp(name, [rows, cols])
        r8 = rows // 8
        u = gather_unit(rows, cols); nch = r8 // u
        sh = inp(name + "_sh", [r8, cols])
        shi = k.dram(name + "_shi", [r8, cols], F32)
        pair = k.dram(name + "_pr", [2 * r8, cols], F32)
        full = k.dram(name, [rows, cols], F32)
        hc = Buf("cp_" + name)
        sp.dma(lambda e: e.dma_start(out=shi, in_=sh), k.db(name + "_sh"), k.db(name + "_shi"), hc)
        pending_gathers.append((name, shi, pair, full, u, nch))
        return full

    xk = inp("xk", [NK, D])
    c_b = inp("c_b", [1, D]); c_ctx = inp("c_ctx", [1, D])
    b_mod = inp("b_mod", [1, 6 * D]); g_attn = inp("g_attn", [1, D])
    out = nc.dram_tensor("out", [NQ, D], F32, kind="ExternalOutput").ap(); k.dbufs["out"] = Buf("out")
    w_mod_p = [weight("w_mod_%d" % i, D, 3 * D) for i in range(2)]
    w_in = weight("w_in", D, c.INC)
    w_uq = weight("w_uq", c.QL, c.MH * 192); w_ukv = weight("w_ukv", c.KVL, c.MH * 256)
    w_br_a = weight("w_br_a", c.DAW, D); w_br_b = weight("w_br_b", c.MW, D); w_out = weight("w_out", D, D)
    NP = 4; EPP = c.NE // NP
    w1p = [weight("w1_%d" % q, EPP * D, c.DE) for q in range(NP)]; w3p = [weight("w3_%d" % q, EPP * D, c.DE) for q in range(NP)]
    w2p = [weight("w2_%d" % q, EPP * c.DE, D) for q in range(NP)]
    if k.gather:
        gather_group()

    uid = [0]
    def S_(name, shape, dt, st):
        uid[0] += 1
        return st.enter_context(nc.sbuf_tensor("%s_%d" % (name, uid[0]), list(shape), dt))
    def P_(name, shape, dt, st):
        uid[0] += 1
        return st.enter_context(nc.psum_tensor("%s_%d" % (name, uid[0]), list(shape), dt))

    mod_bc = scratch("mod_bc", [128, 4 * D], F32)
    A1T = S_("A1T", [128, KD, 2], F32, top); B1T = S_("B1T", [128, KD, 2], F32, top)
    bA1T = Buf("A1T"); bB1T = Buf("B1T")
    ones_f = S_("ones_f", [128, 128], F32, top); b_ones = Buf("ones_f")
    dve.op(lambda e: e.memset(ones_f[:], 1.0), writes=[b_ones])

    with ExitStack() as ph:
        GC = 256
        cT = S_("cT", [128, KD, 2], F32, ph); bcT = Buf("cT")
        sT = S_("sT", [128, KD, 2], F32, ph); bsT = Buf("sT")
        srep = S_("srep", [128, KD, 128], F32, ph); bsrep = Buf("srep")
        bmT = S_("bmT", [128, 2 * KD], F32, ph); bbmT = Buf("bmT")
        gaT = S_("gaT", [128, KD], F32, ph); bgaT = Buf("gaT")
        brow = S_("brow", [1, 4 * D], F32, ph); bbrow = Buf("brow")
        modT = S_("modT", [128, 2 * KD, 2], F32, ph); bmodT = Buf("modT")
        wts = [S_("wm%d" % i, [128, KD, GC], F32, ph) for i in range(2)]; bwts = [Buf("wm%d" % i) for i in range(2)]
        evs = [S_("mev%d" % i, [128, GC], F32, ph) for i in range(2)]; bevs = [Buf("mev%d" % i) for i in range(2)]
        pss = [P_("mps%d" % i, [128, 512], F32, ph) for i in range(2)]; bpss = [Buf("mps%d" % i) for i in range(2)]
        sp.dma(lambda e: e.dma_start(out=cT[:, :, 0], in_=c_b.rearrange("o (k p) -> p (o k)", p=128), allow_slow_non_contiguous=True),
               k.db("c_b"), bcT, bcT)
        sp.dma(lambda e: e.dma_start(out=cT[:, :, 1], in_=c_ctx.rearrange("o (k p) -> p (o k)", p=128), allow_slow_non_contiguous=True),
               k.db("c_ctx"), bcT, bcT)
        sp.dma(lambda e: e.dma_start(out=bmT[:], in_=b_mod[:, 0:2 * D].rearrange("o (k p) -> p (o k)", p=128), allow_slow_non_contiguous=True),
               k.db("b_mod"), bbmT, bbmT)
        sp.dma(lambda e: e.dma_start(out=gaT[:], in_=g_attn.rearrange("o (k p) -> p (o k)", p=128), allow_slow_non_contiguous=True),
               k.db("g_attn"), bgaT, bgaT)
        sp.dma(lambda e: e.dma_start(out=brow[:], in_=b_mod[:, 2 * D:6 * D]), k.db("b_mod"), bbrow, bbrow)
        act.op(lambda e: e.activation(out=sT[:], in_=cT[:], func=AF.Silu), reads=[bcT], writes=[bsT])
        for kc in range(KD):
            dve.op(lambda e: e.tensor_copy(out=srep[:, kc, :], in_=sT[:, kc, 0:1].to_broadcast([128, 128])),
                   reads=[bsT], writes=[bsrep], partial=True)
        ng = 6 * D // GC
        for g in range(ng):
            wt = wts[g % 2]; bwt = bwts[g % 2]; ps = pss[g % 2]; bps = bpss[g % 2]
            pi_ = (g * GC) // (3 * D); pc0 = g * GC - pi_ * 3 * D
            sp.dma(lambda e: e.dma_start(out=wt[:], in_=w_mod_p[pi_][:, pc0:pc0 + GC].rearrange("(k p) n -> p k n", p=128)),
                   k.db("w_mod_%d" % pi_), bwt, bwt, partial=False)
            if g * GC < 2 * D:
                for j in range(GC // 128):
                    for kc in range(KD):
                        pe.op(lambda e: e.matmul(ps[:, j * 2:j * 2 + 2], lhsT=wt[:, kc, j * 128:(j + 1) * 128], rhs=sT[:, kc, :],
                                                 start=(kc == 0), stop=(kc == KD - 1)),
                              reads=[bwt, bsT], writes=[bps], partial=(kc > 0 or j > 0), signal=(kc == KD - 1 and j == GC // 128 - 1))
                ch0 = g * GC // 128
                dve.op(lambda e: e.tensor_tensor(out=modT[:, ch0:ch0 + GC // 128, :],
                                                 in0=ps[:, 0:2 * (GC // 128)].rearrange("p (j t) -> p j t", t=2),
                                                 in1=bmT[:, ch0:ch0 + GC // 128].unsqueeze(2).to_broadcast([128, GC // 128, 2]), op=ALU.add),
                       reads=[bps, bbmT], writes=[bmodT], partial=True)
            else:
                ev = evs[g % 2]; bev = bevs[g % 2]
                c0 = g * GC - 2 * D
                for kc in range(KD):
                    pe.op(lambda e: e.matmul(ps[:, 0:GC], lhsT=srep[:, kc, :], rhs=wt[:, kc, :], start=(kc == 0), stop=False),
                          reads=[bwt, bsrep], writes=[bps], partial=(kc > 0), signal=False)
                pe.op(lambda e: e.matmul(ps[:, 0:GC], lhsT=ones_f[0:1, :], rhs=brow[0:1, c0:c0 + GC], start=False, stop=True),
                      reads=[b_ones, bbrow], writes=[bps], partial=True)
                dve.op(lambda e: e.tensor_copy(out=ev[:], in_=ps[:, 0:GC]), reads=[bps], writes=[bev])
                sp.dma(lambda e: e.dma_start(out=mod_bc[:, c0:c0 + GC], in_=ev[:]), bev, k.db("mod_bc"), bev)
        dve.op(lambda e: e.tensor_scalar(out=A1T[:], in0=modT[:, KD:2 * KD, :], scalar1=1.0, scalar2=None, op0=ALU.add),
               reads=[bmodT], writes=[bA1T])
        dve.op(lambda e: e.tensor_tensor(out=A1T[:], in0=A1T[:], in1=gaT[:].unsqueeze(2).to_broadcast([128, KD, 2]), op=ALU.mult),
               reads=[bA1T, bgaT], writes=[bA1T])
        dve.op(lambda e: e.tensor_copy(out=B1T[:], in_=modT[:, 0:KD, :]), reads=[bmodT], writes=[bB1T])
        if "A1T" in debug_outs:
            o1 = nc.dram_tensor("dbg_A1T", [128, KD * 2], F32, kind="ExternalOutput").ap(); k.dbufs["dbg_A1T"] = Buf("dbg_A1T")
            o2 = nc.dram_tensor("dbg_B1T", [128, KD * 2], F32, kind="ExternalOutput").ap(); k.dbufs["dbg_B1T"] = Buf("dbg_B1T")
            sp.dma(lambda e: e.dma_start(out=o1, in_=A1T[:].rearrange("p k t -> p (k t)")), bA1T, k.db("dbg_A1T"), bA1T)
            sp.dma(lambda e: e.dma_start(out=o2, in_=B1T[:].rearrange("p k t -> p (k t)")), bB1T, k.db("dbg_B1T"), bB1T)
        sc.barrier()
    if upto <= 1:
        return

    class Ring:
        def __init__(self, name, shape, dt, n, st, psum=False):
            mk = P_ if psum else S_
            self.t = [mk("%s%d" % (name, i), shape, dt, st) for i in range(n)]
            self.b = [Buf("%s%d" % (name, i)) for i in range(n)]
            self.i = -1
        def next(self):
            self.i = (self.i + 1) % len(self.t)
            return self.t[self.i], self.b[self.i]

    ident = S_("ident", [128, 128], BF16, top); b_ident = Buf("ident")
    identf = S_("identf", [128, 128], F32, top); b_identf = Buf("identf")
    pool.op(lambda e: e.memset(identf[:], 0.0), writes=[b_identf])
    pool.op(lambda e: e.affine_select(out=identf[:], in_=ones_f[:], pattern=[[-1, 128]], compare_op=ALU.is_equal, fill=0.0,
                                      base=0, channel_multiplier=1), reads=[b_ones], writes=[b_identf])
    dve.op(lambda e: e.tensor_copy(out=ident[:], in_=identf[:]), reads=[b_identf], writes=[b_ident])
    ones_b = S_("ones_b", [128, 128], BF16, top); b_onesb = Buf("ones_b")
    dve.op(lambda e: e.memset(ones_b[:], 1.0), writes=[b_onesb])

    hT_d = scratch("hT_d", [KD, 128, NK], BF16)
    with ExitStack() as ph:
        xts = Ring("xt", [128, D], F32, 2, ph)
        xns = Ring("xn", [128, D], BF16, 2, ph)
        junk = S_("junk", [128, D], BF16, ph); bjunk = Buf("junk")
        sss = Ring("ss", [128, 2], F32, 2, ph)
        hts = Ring("hts", [128, KD, 128], BF16, 2, ph)
        pst = Ring("pst", [128, 1024], BF16, 2, ph, psum=True)
        import os as _os
        for t in range(int(_os.environ.get('P2T', NK // 128))):
            col = 0 if t * 128 < S else 1
            xt, bxt = xts.next(); xn, bxn = xns.next(); ss, bss = sss.next(); ht, bht = hts.next()
            sp.dma(lambda e: e.dma_start(out=xt[:], in_=xk[t * 128:(t + 1) * 128, :]), k.db("xk"), bxt, bxt, partial=False)
            act.op(lambda e: e.activation(out=junk[:], in_=xt[:], func=AF.Square, accum_out=ss[:, 0:1]), reads=[bxt], writes=[bjunk, bss])
            dve.op(lambda e: e.tensor_scalar(out=ss[:, 1:2], in0=ss[:, 0:1], scalar1=1.0 / D, scalar2=1e-6, op0=ALU.mult, op1=ALU.add),
                   reads=[bss], writes=[bss])
            act.op(lambda e: e.activation(out=ss[:, 1:2], in_=ss[:, 1:2], func=AF.Ln), reads=[bss], writes=[bss]); act.op(lambda e: e.activation(out=ss[:, 1:2], in_=ss[:, 1:2], func=AF.Exp, scale=-0.5), reads=[bss], writes=[bss])
            dve.op(lambda e: e.tensor_scalar(out=xn[:], in0=xt[:], scalar1=ss[:, 1:2], scalar2=None, op0=ALU.mult), reads=[bxt, bss], writes=[bxn])
            _stg = int(_os.environ.get('P2STAGE', 5))
            for g0 in range(0, KD if _stg >= 3 else 0, 8):
                ps, bps = pst.next()
                ng_ = min(8, KD - g0)
                for j in range(ng_):
                    kc = g0 + j
                    pe.op(lambda e: e.transpose(out=ps[:, j * 128:(j + 1) * 128], in_=xn[:, kc * 128:(kc + 1) * 128], identity=ident[:]),
                          reads=[bxn, b_ident], writes=[bps], partial=(j > 0), signal=(j == ng_ - 1))
                for j in range(ng_ if _stg >= 4 else 0):
                    kc = g0 + j
                    if True:
                        dve.op(lambda e: e.tensor_scalar(out=ht[:, kc, :], in0=ps[:, j * 128:(j + 1) * 128], scalar1=A1T[:, kc, col:col + 1],
                                                         scalar2=B1T[:, kc, col:col + 1], op0=ALU.mult, op1=ALU.add),
                               reads=[bps, bA1T, bB1T], writes=[bht], partial=True)
                    else:
                        act.op(lambda e: e.activation(out=ht[:, kc, :], in_=ps[:, j * 128:(j + 1) * 128], func=AF.Identity,
                                                      scale=A1T[:, kc, col:col + 1], bias=B1T[:, kc, col:col + 1]),
                               reads=[bps, bA1T, bB1T], writes=[bht], partial=True)
            if _stg >= 5:
                sp.dma(lambda e: e.dma_start(out=hT_d[:, :, t * 128:(t + 1) * 128].rearrange("k p n -> p k n"), in_=ht[:]),
                       bht, k.db("hT_d"), bht)
        sc.barrier()
    if upto <= 2:
        return

    cosd = inp("cosd", [128, NK]); sind = inp("sind", [128, NK]); cosm = inp("cosm", [64, NK]); sinm = inp("sinm", [64, NK])
    DAH, MH, QL, KVL = c.DAH, c.MH, c.QL, c.KVL
    QT_d = scratch("QT_d", [2 * DAH, 128, NQ], BF16); KT_d = scratch("KT_d", [2 * DAH, 128, NK], BF16)
    V_d = scratch("V_d", [NK, DAH * 256], BF16)
    CQ_d = scratch("CQ_d", [QL // 128, 128, NQ], F32); CKV_d = scratch("CKV_d", [KVL // 128, 128, NK], F32)
    KR_d = scratch("KR_d", [64, NK], BF16)
    GA_d = scratch("GA_d", [KD, 128, NQ], BF16); GB_d = scratch("GB_d", [KD, 128, NQ], BF16)

    def rope_evac(st, ps, bps, M, t0, tw, cos_d, sin_d, cname, sname, dst_ap, dst_name, rings):
        ct, bct = rings["cos"].next(); sn, bsn = rings["sin"].next()
        t1, bt1 = rings["t1"].next(); t2, bt2 = rings["t2"].next(); ob, bob = rings["ob"].next()
        h = M // 2
        sp.dma(lambda e: e.dma_start(out=ct[0:M, 0:tw], in_=cos_d[0:M, t0:t0 + tw]), k.db(cname), bct, bct, partial=False)
        sp.dma(lambda e: e.dma_start(out=sn[0:M, 0:tw], in_=sin_d[0:M, t0:t0 + tw]), k.db(sname), bsn, bsn, partial=False)
        dve.op(lambda e: e.tensor_tensor(out=t1[0:M, 0:tw], in0=ps[0:M, 0:tw], in1=ct[0:M, 0:tw], op=ALU.mult), reads=[bps, bct], writes=[bt1])
        dve.op(lambda e: e.tensor_tensor(out=t2[0:h, 0:tw], in0=ps[h:M, 0:tw], in1=sn[0:h, 0:tw], op=ALU.mult), reads=[bps, bsn], writes=[bt2])
        dve.op(lambda e: e.tensor_tensor(out=t2[h:M, 0:tw], in0=ps[0:h, 0:tw], in1=sn[h:M, 0:tw], op=ALU.mult), reads=[bps, bsn], writes=[bt2], partial=True)
        pool.op(lambda e: e.tensor_tensor(out=ob[0:M, 0:tw], in0=t1[0:M, 0:tw], in1=t2[0:M, 0:tw], op=ALU.add), reads=[bt1, bt2], writes=[bob])
        act.dma(lambda e: e.dma_start(out=dst_ap, in_=ob[0:M, 0:tw]), bob, k.db(dst_name), bob)

    def mk_rope_rings(st):
        return dict(cos=Ring("rcos", [128, 512], F32, 2, st), sin=Ring("rsin", [128, 512], F32, 2, st),
                    t1=Ring("rt1", [128, 512], F32, 2, st), t2=Ring("rt2", [128, 512], F32, 2, st),
                    ob=Ring("rob", [128, 512], BF16, 3, st))

    with ExitStack() as ph:
        wgs = Ring("wg", [128, KD, 512], BF16, 2, ph)
        hxs = Ring("hx", [128, KD, 512], BF16, 2, ph)
        pss = Ring("pj", [128, 512], F32, 6, ph, psum=True)
        rr = mk_rope_rings(ph)
        evf = Ring("evf", [128, 512], F32, 3, ph)
        evb = Ring("evb", [128, 512], BF16, 3, ph)
        segs = [(c.o_dq, c.DAW, NQ, "dq"), (c.o_dk, c.DAW, NK, "dk"), (c.o_dv, c.DAW, NK, "dv"),
                (c.o_cq, QL, NQ, "cq"), (c.o_ckv, KVL + 64, NK, "ckv"), (c.o_ga, D, NQ, "ga"), (c.o_gb, D, NQ, "gb")]
        groups = []
        for (c0, ncol, ntok, kind) in segs:
            for g0 in range(0, ncol, 512):
                groups.append((c0, g0, min(512, ncol - g0), ntok, kind))
        wl = {}
        def loadW(gi):
            if gi < len(groups) and gi not in wl:
                c0, g0, gw, ntok, kind = groups[gi]
                wg, bwg = wgs.next()
                pool.dma(lambda e: e.dma_start(out=wg[:, :, 0:gw], in_=w_in[:, c0 + g0:c0 + g0 + gw].rearrange("(k p) n -> p k n", p=128)),
                         k.db("w_in"), bwg, bwg, partial=False)
                wl[gi] = (wg, bwg)
        items = [(gi, t0) for gi, g in enumerate(groups) for t0 in range(0, g[3], 512)]
        hl = {}
        def loadH(ii):
            if ii < len(items) and ii not in hl:
                gi, t0 = items[ii]
                tw = min(512, groups[gi][3] - t0)
                hx, bhx = hxs.next()
                sp.dma(lambda e: e.dma_start(out=hx[:, :, 0:tw], in_=hT_d[:, :, t0:t0 + tw].rearrange("k p n -> p k n")),
                       k.db("hT_d"), bhx, bhx, partial=False)
                hl[ii] = (hx, bhx)
        loadW(0); loadH(0)
        for ii, (gi, t0) in enumerate(items):
            c0, g0, gw, ntok, kind = groups[gi]
            tw = min(512, ntok - t0)
            if t0 == 0:
                loadW(gi + 1)
            loadH(ii + 1)
            wg, bwg = wl[gi]; hx, bhx = hl[ii]
            if kind == "dv":
                for tt in range(tw // 128):
                    ps, bps = pss.next()
                    for kc in range(KD):
                        pe.op(lambda e: e.matmul(ps[:, 0:gw], lhsT=hx[:, kc, tt * 128:(tt + 1) * 128], rhs=wg[:, kc, 0:gw],
                                                 start=(kc == 0), stop=(kc == KD - 1)),
                              reads=[bhx, bwg], writes=[bps], partial=(kc > 0), signal=(kc == KD - 1))
                    ob, bob = evb.next()
                    act.op(lambda e: e.copy(out=ob[:, 0:gw], in_=ps[:, 0:gw]), reads=[bps], writes=[bob])
                    r0 = t0 + tt * 128
                    act.dma(lambda e: e.dma_start(out=V_d[r0:r0 + 128, g0:g0 + gw], in_=ob[:, 0:gw]), bob, k.db("V_d"), bob)
                continue
            nch = (gw + 127) // 128
            for j in range(nch):
                M = min(128, gw - j * 128)
                ch = (g0 + j * 128) // 128
                ps, bps = pss.next()
                for kc in range(KD):
                    pe.op(lambda e: e.matmul(ps[0:M, 0:tw], lhsT=wg[:, kc, j * 128:j * 128 + M], rhs=hx[:, kc, 0:tw],
                                             start=(kc == 0), stop=(kc == KD - 1)),
                          reads=[bhx, bwg], writes=[bps], partial=(kc > 0), signal=(kc == KD - 1))
                if kind == "dq":
                    rope_evac(ph, ps, bps, 128, t0, tw, cosd, sind, "cosd", "sind", QT_d[ch, :, t0:t0 + tw], "QT_d", rr)
                elif kind == "dk":
                    rope_evac(ph, ps, bps, 128, t0, tw, cosd, sind, "cosd", "sind", KT_d[ch, :, t0:t0 + tw], "KT_d", rr)
                elif kind == "ckv" and M == 64:
                    rope_evac(ph, ps, bps, 64, t0, tw, cosm, sinm, "cosm", "sinm", KR_d[:, t0:t0 + tw], "KR_d", rr)
                elif kind in ("cq", "ckv"):
                    ob, bob = evf.next()
                    dve.op(lambda e: e.tensor_copy(out=ob[:, 0:tw], in_=ps[:, 0:tw]), reads=[bps], writes=[bob])
                    dst, dn = (CQ_d, "CQ_d") if kind == "cq" else (CKV_d, "CKV_d")
                    act.dma(lambda e: e.dma_start(out=dst[ch, :, t0:t0 + tw], in_=ob[:, 0:tw]), bob, k.db(dn), bob)
                else:
                    ob, bob = evb.next()
                    act.op(lambda e: e.activation(out=ob[:, 0:tw], in_=ps[:, 0:tw], func=AF.Sigmoid), reads=[bps], writes=[bob])
                    dst, dn = (GA_d, "GA_d") if kind == "ga" else (GB_d, "GB_d")
                    act.dma(lambda e: e.dma_start(out=dst[ch, :, t0:t0 + tw], in_=ob[:, 0:tw]), bob, k.db(dn), bob)
        sc.barrier()
    if upto <= 3:
        return

    g_q = inp("g_q", [1, QL]); g_kv = inp("g_kv", [1, KVL])
    QN_d = scratch("QN_d", [MH, 128, NQ], BF16); QR_d = scratch("QR_d", [MH, 64, NQ], BF16)
    KN_d = scratch("KN_d", [MH, 128, NK], BF16); MV_d = scratch("MV_d", [NK, MH * 128], BF16)

    def latent_up(lat_d, lat_name, nch, ntok, g_ap, g_name, W, Wname, wcols, emit):
        with ExitStack() as ph:
            wsb = S_("lw", [128, nch, wcols], BF16, ph); bw = Buf("lw")
            pool.dma(lambda e: e.dma_start(out=wsb[:], in_=W.rearrange("(k p) n -> p k n", p=128)), k.db(Wname), bw, bw, partial=False)
            gT = S_("lg", [128, nch], F32, ph); bg = Buf("lg")
            sp.dma(lambda e: e.dma_start(out=gT[:], in_=g_ap.rearrange("o (k p) -> p (o k)", p=128), allow_slow_non_contiguous=True),
                   k.db(g_name), bg, bg)
            Ls = Ring("lL", [128, nch, 512], F32, 2, ph)
            sq = S_("lsq", [128, nch, 512], F32, ph); bsq = Buf("lsq")
            Lns = Ring("lLn", [128, nch, 512], BF16, 2, ph)
            rs = Ring("lrs", [128, 512], F32, 2, ph)
            pss = Ring("lps", [128, 512], F32, 5, ph, psum=True)
            rr = mk_rope_rings(ph)
            evb = Ring("levb", [128, 512], BF16, 3, ph)
            for t0 in range(0, ntok, 512):
                tw = min(512, ntok - t0)
                L, bL = Ls.next(); Ln, bLn = Lns.next(); r_, br = rs.next()
                sp.dma(lambda e: e.dma_start(out=L[:, :, 0:tw], in_=lat_d[:, :, t0:t0 + tw].rearrange("k p n -> p k n")),
                       k.db(lat_name), bL, bL, partial=False)
                act.op(lambda e: e.activation(out=sq[:, :, 0:tw], in_=L[:, :, 0:tw], func=AF.Square), reads=[bL], writes=[bsq])
                ps, bps = pss.next()
                for kc in range(nch):
                    pe.op(lambda e: e.matmul(ps[:, 0:tw], lhsT=ones_f[:], rhs=sq[:, kc, 0:tw], start=(kc == 0), stop=(kc == nch - 1)),
                          reads=[b_ones, bsq], writes=[bps], partial=(kc > 0), signal=(kc == nch - 1))
                dve.op(lambda e: e.tensor_scalar(out=r_[:, 0:tw], in0=ps[:, 0:tw], scalar1=1.0 / (nch * 128), scalar2=1e-6, op0=ALU.mult, op1=ALU.add),
                       reads=[bps], writes=[br])
                act.op(lambda e: e.activation(out=r_[:, 0:tw], in_=r_[:, 0:tw], func=AF.Ln), reads=[br], writes=[br]); act.op(lambda e: e.activation(out=r_[:, 0:tw], in_=r_[:, 0:tw], func=AF.Exp, scale=-0.5), reads=[br], writes=[br])
                for kc in range(nch):
                    dve.op(lambda e: e.scalar_tensor_tensor(out=Ln[:, kc, 0:tw], in0=L[:, kc, 0:tw], scalar=gT[:, kc:kc + 1], in1=r_[:, 0:tw],
                                                            op0=ALU.mult, op1=ALU.mult),
                           reads=[bL, bg, br], writes=[bLn], partial=(kc > 0))
                emit(Ln, bLn, wsb, bw, t0, tw, nch, pss, rr, evb)
            sc.barrier()

    def fm_head(Ln, bLn, wsb, bw, nch, col0, M, tw, pss):
        ps, bps = pss.next()
        for kc in range(nch):
            pe.op(lambda e: e.matmul(ps[0:M, 0:tw], lhsT=wsb[:, kc, col0:col0 + M], rhs=Ln[:, kc, 0:tw], start=(kc == 0), stop=(kc == nch - 1)),
                  reads=[bw, bLn], writes=[bps], partial=(kc > 0), signal=(kc == nch - 1))
        return ps, bps

    def emit_q(Ln, bLn, wsb, bw, t0, tw, nch, pss, rr, evb):
        for h in range(MH):
            ps, bps = fm_head(Ln, bLn, wsb, bw, nch, h * 192, 128, tw, pss)
            ob, bob = evb.next()
            act.op(lambda e: e.copy(out=ob[:, 0:tw], in_=ps[:, 0:tw]), reads=[bps], writes=[bob])
            act.dma(lambda e: e.dma_start(out=QN_d[h, :, t0:t0 + tw], in_=ob[:, 0:tw]), bob, k.db("QN_d"), bob)
            ps, bps = fm_head(Ln, bLn, wsb, bw, nch, h * 192 + 128, 64, tw, pss)
            rope_evac(None, ps, bps, 64, t0, tw, cosm, sinm, "cosm", "sinm", QR_d[h, :, t0:t0 + tw], "QR_d", rr)

    def emit_kv(Ln, bLn, wsb, bw, t0, tw, nch, pss, rr, evb):
        for h in range(MH):
            ps, bps = fm_head(Ln, bLn, wsb, bw, nch, h * 256, 128, tw, pss)
            ob, bob = evb.next()
            act.op(lambda e: e.copy(out=ob[:, 0:tw], in_=ps[:, 0:tw]), reads=[bps], writes=[bob])
            act.dma(lambda e: e.dma_start(out=KN_d[h, :, t0:t0 + tw], in_=ob[:, 0:tw]), bob, k.db("KN_d"), bob)
        for tt in range(tw // 128):
            for h0 in range(0, MH, 4):
                nh = min(4, MH - h0)
                ps, bps = pss.next()
                for kc in range(nch):
                    pe.op(lambda e: e.matmul(ps[:, 0:nh * 128].rearrange("p (h e) -> p h e", e=128), lhsT=Ln[:, kc, tt * 128:(tt + 1) * 128],
                                             rhs=wsb[:, kc, :].rearrange("p (h e) -> p h e", e=256)[:, h0:h0 + nh, 128:256],
                                             start=(kc == 0), stop=(kc == nch - 1)),
                          reads=[bw, bLn], writes=[bps], partial=(kc > 0), signal=(kc == nch - 1))
                ob, bob = evb.next()
                dve.op(lambda e: e.tensor_copy(out=ob[:, 0:nh * 128], in_=ps[:, 0:nh * 128]), reads=[bps], writes=[bob])
                r0 = t0 + tt * 128
                act.dma(lambda e: e.dma_start(out=MV_d[r0:r0 + 128, h0 * 128:(h0 + nh) * 128], in_=ob[:, 0:nh * 128]), bob, k.db("MV_d"), bob)

    latent_up(CQ_d, "CQ_d", QL // 128, NQ, g_q, "g_q", w_uq, "w_uq", MH * 192, emit_q)
    latent_up(CKV_d, "CKV_d", KVL // 128, NK, g_kv, "g_kv", w_ukv, "w_ukv", MH * 256, emit_kv)
    if upto <= 4:
        return

    OA_d = scratch("OA_d", [c.DAW // 128, 128, NQ], BF16); OB_d = scratch("OB_d", [c.MW // 128, 128, NQ], BF16)
    g_subln = inp("g_subln", [1, 256])
    lam_in = [inp(n, [1, 128]) for n in ("lam_q1", "lam_k1", "lam_q2", "lam_k2")]
    NKC = NK // 128

    def attn_branch(H, ncomp, E, scale, k_parts, q_parts, v_src, finalize, extra=None):
        with ExitStack() as ph:
            np_ = len(k_parts(0, 0))
            kts = Ring("akt", [128, ncomp * np_, NK], BF16, 2, ph)
            vts = Ring("avt", [128, NKC, E + 1], BF16, 2, ph)
            for i in range(2):
                pool.op(lambda e: e.memset(vts.t[i][:, :, E:E + 1], 1.0), writes=[vts.b[i]])
            qts = Ring("aqt", [128, ncomp * np_, 512], BF16, 2, ph)
            pts = Ring("apt", [128, 1024], BF16, 3, ph)
            ops_ = Ring("aop", [128, 512], F32, 4, ph, psum=True)
            sps = Ring("asp", [128, 1024], F32, 2, ph, psum=True)
            ctx_ = extra(ph, sps) if extra else None
            for h in range(H):
                kt, bkt = kts.next(); vt, bvt = vts.next()
                for comp in range(ncomp):
                    for pi, (ap_, nm, Kr) in enumerate(k_parts(h, comp)):
                        sp.dma(lambda e: e.dma_start(out=kt[0:Kr, comp * np_ + pi, :], in_=ap_), k.db(nm), bkt, bkt, partial=(comp + pi > 0))
                vap, vnm = v_src(h)
                sp.dma(lambda e: e.dma_start(out=vt[:, :, 0:E], in_=vap.rearrange("(k p) e -> p k e", p=128)), k.db(vnm), bvt, bvt)
                for q0 in range(0, NQ, 512):
                    tw = min(512, NQ - q0); nqs = tw // 128
                    qt, bqt = qts.next()
                    for comp in range(ncomp):
                        for pi, (ap_, nm, Kr) in enumerate(q_parts(h, comp)):
                            sp.dma(lambda e: e.dma_start(out=qt[0:Kr, comp * np_ + pi, 0:tw], in_=ap_[:, q0:q0 + tw]), k.db(nm), bqt, bqt,
                                   partial=(comp + pi > 0))
                    for comp in range(ncomp):
                        Os = [ops_.next() for _ in range(nqs)]
                        parts = k_parts(h, comp)
                        for kc0 in range(0, NKC, 2):
                            n2 = min(2, NKC - kc0)
                            sp_, bsp = sps.next()
                            for j in range(n2):
                                for pi, (_, _, Kr) in enumerate(parts):
                                    pe.op(lambda e: e.matmul(sp_[:, j * 512:j * 512 + tw], lhsT=kt[0:Kr, comp * np_ + pi, (kc0 + j) * 128:(kc0 + j + 1) * 128],
                                                             rhs=qt[0:Kr, comp * np_ + pi, 0:tw], start=(pi == 0), stop=(pi == np_ - 1)),
                                          reads=[bkt, bqt], writes=[bsp], partial=(j + pi > 0), signal=(j == n2 - 1 and pi == np_ - 1))
                            pt, bpt = pts.next()
                            if tw == 512:
                                act.op(lambda e: e.activation(out=pt[:, 0:n2 * 512], in_=sp_[:, 0:n2 * 512], func=AF.Exp, scale=scale), reads=[bsp], writes=[bpt])
                            else:
                                for j in range(n2):
                                    act.op(lambda e: e.activation(out=pt[:, j * 512:j * 512 + tw], in_=sp_[:, j * 512:j * 512 + tw], func=AF.Exp, scale=scale),
                                           reads=[bsp], writes=[bpt], partial=(j > 0))
                            for j in range(n2):
                                kc = kc0 + j
                                for qs in range(nqs):
                                    O, bO = Os[qs]
                                    pe.op(lambda e: e.matmul(O[:, 0:E + 1], lhsT=pt[:, j * 512 + qs * 128:j * 512 + (qs + 1) * 128], rhs=vt[:, kc, 0:E + 1],
                                                             start=(kc == 0), stop=(kc == NKC - 1)),
                                          reads=[bpt, bvt], writes=[bO], partial=(kc > 0), signal=(kc == NKC - 1 or (j == n2 - 1 and qs == nqs - 1)))
                        finalize(ctx_, h, comp, q0, tw, Os, sps)
            sc.barrier()

    def store_T(ctx_, src_bf, bsrc, nE, dst_d, dst_name, ch0, q0, tw, sps):
        nqs = tw // 128
        for ec in range(nE):
            sp_, bsp = sps.next()
            pb = sp_[:].bitcast(BF16)
            for qs in range(nqs):
                pe.op(lambda e: e.transpose(out=pb[:, qs * 128:(qs + 1) * 128], in_=src_bf[:, qs, ec * 128:(ec + 1) * 128], identity=ident[:]),
                      reads=[bsrc, b_ident], writes=[bsp], partial=(qs > 0), signal=(qs == nqs - 1))
            ob, bob = ctx_["oT"].next()
            dve.op(lambda e: e.tensor_copy(out=ob[:, 0:tw], in_=pb[:, 0:tw]), reads=[bsp], writes=[bob])
            act.dma(lambda e: e.dma_start(out=dst_d[ch0 + ec, :, q0:q0 + tw], in_=ob[:, 0:tw]), bob, k.db(dst_name), bob)

    def da_extra(ph, sps):
        d = {}
        d["o0"] = S_("da_o0", [128, 4, 256], F32, ph); d["bo0"] = Buf("da_o0")
        d["o1"] = S_("da_o1", [128, 4, 256], F32, ph); d["bo1"] = Buf("da_o1")
        d["ob"] = S_("da_ob", [128, 4, 256], BF16, ph); d["bob"] = Buf("da_ob")
        d["jk"] = S_("da_jk", [128, 256], F32, ph); d["bjk"] = Buf("da_jk")
        d["rs"] = S_("da_rs", [128, 8], F32, ph); d["brs"] = Buf("da_rs")
        d["oT"] = Ring("da_oT", [128, 512], BF16, 3, ph)
        lv = S_("da_lv", [128, 4], F32, ph); blv = Buf("da_lv")
        for i in range(4):
            sp.dma(lambda e: e.dma_start(out=lv[:, i:i + 1], in_=lam_in[i].rearrange("o p -> p o"), allow_slow_non_contiguous=True),
                   k.db(("lam_q1", "lam_k1", "lam_q2", "lam_k2")[i]), blv, blv)
        lp = S_("da_lp", [128, 2], F32, ph); blp = Buf("da_lp")
        dve.op(lambda e: e.tensor_tensor(out=lp[:, 0:1], in0=lv[:, 0:1], in1=lv[:, 1:2], op=ALU.mult), reads=[blv], writes=[blp])
        dve.op(lambda e: e.tensor_tensor(out=lp[:, 1:2], in0=lv[:, 2:3], in1=lv[:, 3:4], op=ALU.mult), reads=[blv], writes=[blp], partial=True)
        with ExitStack() as tmp:
            ps, bps = sps.next()
            pe.op(lambda e: e.matmul(ps[:, 0:2], lhsT=ones_f[:], rhs=lp[:], start=True, stop=True), reads=[b_ones, blp], writes=[bps])
            le = S_("da_le", [128, 2], F32, ph); ble = Buf("da_le")
            act.op(lambda e: e.activation(out=le[:], in_=ps[:, 0:2], func=AF.Exp), reads=[bps], writes=[ble])
            nl = S_("da_nl", [128, 1], F32, ph); bnl = Buf("da_nl")
            dve.op(lambda e: e.tensor_tensor(out=nl[:], in0=le[:, 1:2], in1=le[:, 0:1], op=ALU.subtract), reads=[ble], writes=[bnl])
            dve.op(lambda e: e.tensor_scalar(out=nl[:], in0=nl[:], scalar1=-c.lambda_init, scalar2=None, op0=ALU.add), reads=[bnl], writes=[bnl])
            d["nl"] = nl; d["bnl"] = bnl
            gr = S_("da_gr", [1, 256], F32, ph); bgr = Buf("da_gr")
            sp.dma(lambda e: e.dma_start(out=gr[:], in_=g_subln), k.db("g_subln"), bgr, bgr)
            pe.op(lambda e: e.matmul(ps[:, 256:512], lhsT=ones_f[0:1, :], rhs=gr[0:1, :], start=True, stop=True), reads=[b_ones, bgr], writes=[bps])
            gb_ = S_("da_gb", [128, 256], F32, ph); bgb = Buf("da_gb")
            dve.op(lambda e: e.tensor_scalar(out=gb_[:], in0=ps[:, 256:512], scalar1=1.0 - c.lambda_init, scalar2=None, op0=ALU.mult),
                   reads=[bps], writes=[bgb])
            d["gb"] = gb_; d["bgb"] = bgb
            sc.barrier([pe, dve, act])
        return d

    def da_final(d, h, comp, q0, tw, Os, sps):
        nqs = tw // 128
        rs, brs = d["rs"], d["brs"]
        dst, bdst = (d["o0"], d["bo0"]) if comp == 0 else (d["o1"], d["bo1"])
        for qs in range(nqs):
            O, bO = Os[qs]
            dve.op(lambda e: e.reciprocal(out=rs[:, qs:qs + 1], in_=O[:, 256:257]), reads=[bO], writes=[brs], partial=(qs > 0))
            dve.op(lambda e: e.tensor_scalar(out=dst[:, qs, :], in0=O[:, 0:256], scalar1=rs[:, qs:qs + 1], scalar2=None, op0=ALU.mult),
                   reads=[bO, brs], writes=[bdst], partial=(qs > 0))
        if comp == 0:
            return
        o0, bo0, o1, bo1 = d["o0"], d["bo0"], d["o1"], d["bo1"]
        for qs in range(nqs):
            dve.op(lambda e: e.scalar_tensor_tensor(out=o0[:, qs, :], in0=o1[:, qs, :], scalar=d["nl"][:, 0:1], in1=o0[:, qs, :], op0=ALU.mult, op1=ALU.add),
                   reads=[bo1, bo0, d["bnl"]], writes=[bo0])
            act.op(lambda e: e.activation(out=d["jk"][:], in_=o0[:, qs, :], func=AF.Square, accum_out=rs[:, 4 + qs:5 + qs]),
                   reads=[bo0], writes=[d["bjk"], brs])
            dve.op(lambda e: e.tensor_scalar(out=rs[:, 4 + qs:5 + qs], in0=rs[:, 4 + qs:5 + qs], scalar1=1.0 / 256, scalar2=1e-6, op0=ALU.mult, op1=ALU.add),
                   reads=[brs], writes=[brs])
            act.op(lambda e: e.activation(out=rs[:, 4 + qs:5 + qs], in_=rs[:, 4 + qs:5 + qs], func=AF.Ln), reads=[brs], writes=[brs]); act.op(lambda e: e.activation(out=rs[:, 4 + qs:5 + qs], in_=rs[:, 4 + qs:5 + qs], func=AF.Exp, scale=-0.5), reads=[brs], writes=[brs])
            dve.op(lambda e: e.scalar_tensor_tensor(out=d["ob"][:, qs, :], in0=o0[:, qs, :], scalar=rs[:, 4 + qs:5 + qs], in1=d["gb"][:], op0=ALU.mult, op1=ALU.mult),
                   reads=[bo0, brs, d["bgb"]], writes=[d["bob"]], partial=(qs > 0))
        store_T(d, d["ob"], d["bob"], 2, OA_d, "OA_d", h * 2, q0, tw, sps)

    attn_branch(DAH, 2, 256, 128 ** -0.5,
                lambda h, comp: [(KT_d[comp * DAH + h], "KT_d", 128)],
                lambda h, comp: [(QT_d[comp * DAH + h], "QT_d", 128)],
                lambda h: (V_d[:, h * 256:(h + 1) * 256], "V_d"), da_final, da_extra)
    if upto <= 5:
        return

    def mla_extra(ph, sps):
        d = {}
        d["ob"] = S_("ml_ob", [128, 4, 128], BF16, ph); d["bob"] = Buf("ml_ob")
        d["rs"] = S_("ml_rs", [128, 4], F32, ph); d["brs"] = Buf("ml_rs")
        d["oT"] = Ring("ml_oT", [128, 512], BF16, 3, ph)
        return d

    def mla_final(d, h, comp, q0, tw, Os, sps):
        nqs = tw // 128
        rs, brs = d["rs"], d["brs"]
        for qs in range(nqs):
            O, bO = Os[qs]
            dve.op(lambda e: e.reciprocal(out=rs[:, qs:qs + 1], in_=O[:, 128:129]), reads=[bO], writes=[brs], partial=(qs > 0))
            dve.op(lambda e: e.tensor_scalar(out=d["ob"][:, qs, :], in0=O[:, 0:128], scalar1=rs[:, qs:qs + 1], scalar2=None, op0=ALU.mult),
                   reads=[bO, brs], writes=[d["bob"]], partial=(qs > 0))
        store_T(d, d["ob"], d["bob"], 1, OB_d, "OB_d", h, q0, tw, sps)

    attn_branch(MH, 1, 128, 192 ** -0.5,
                lambda h, comp: [(KN_d[h], "KN_d", 128), (KR_d, "KR_d", 64)],
                lambda h, comp: [(QN_d[h], "QN_d", 128), (QR_d[h], "QR_d", 64)],
                lambda h: (MV_d[:, h * 128:(h + 1) * 128], "MV_d"), mla_final, mla_extra)
    if upto <= 6:
        return

    NR = c.NG + c.NE; NE = c.NE; NG = c.NG; EPG = c.EPG; DE = c.DE; HC = DE // 128
    NT = NQ // 128; NBLK = 2 * NT + NE
    g_ffn = inp("g_ffn", [1, D]); w_r = inp("w_r", [D, NR]); b_r = inp("b_r", [1, NR]); g_final = inp("g_final", [1, D])
    MG_d = scratch("MG_d", [KD, 128, NQ], BF16); X1_d = scratch("X1_d", [NQ, D], F32); H2_d = scratch("H2_d", [NQ, D], BF16)
    Xs_d = scratch("Xs_d", [NBLK * 128, D], BF16); Ys_d = scratch("Ys_d", [NBLK * 128, D], F32)
    KA = c.DAW // 128; KB = c.MW // 128

    with ExitStack() as ph:
        was = Ring("wa", [128, KA, 512], BF16, 2, ph); wbs = Ring("wb", [128, KB, 512], BF16, 2, ph)
        oas = Ring("oa", [128, KA, 512], BF16, 2, ph); obs = Ring("ob", [128, KB, 512], BF16, 2, ph)
        gas = Ring("gat", [128, 512], BF16, 2, ph); gbs = Ring("gbt", [128, 512], BF16, 2, ph)
        t1s = Ring("mt1", [128, 512], F32, 2, ph); t2s = Ring("mt2", [128, 512], F32, 2, ph); mgs = Ring("mgo", [128, 512], BF16, 3, ph)
        pss = Ring("mps", [128, 512], F32, 6, ph, psum=True)
        for g0 in range(0, D, 512):
            gw = min(512, D - g0)
            wa, bwa = was.next(); wb, bwb = wbs.next()
            pool.dma(lambda e: e.dma_start(out=wa[:, :, 0:gw], in_=w_br_a[:, g0:g0 + gw].rearrange("(k p) n -> p k n", p=128)), k.db("w_br_a"), bwa, bwa, partial=False)
            pool.dma(lambda e: e.dma_start(out=wb[:, :, 0:gw], in_=w_br_b[:, g0:g0 + gw].rearrange("(k p) n -> p k n", p=128)), k.db("w_br_b"), bwb, bwb, partial=False)
            for t0 in range(0, NQ, 512):
                tw = min(512, NQ - t0)
                oa, boa = oas.next(); ob, bob = obs.next()
                sp.dma(lambda e: e.dma_start(out=oa[:, :, 0:tw], in_=OA_d[:, :, t0:t0 + tw].rearrange("k p n -> p k n")), k.db("OA_d"), boa, boa, partial=False)
                sp.dma(lambda e: e.dma_start(out=ob[:, :, 0:tw], in_=OB_d[:, :, t0:t0 + tw].rearrange("k p n -> p k n")), k.db("OB_d"), bob, bob, partial=False)
                for j in range(gw // 128):
                    dc = (g0 + j * 128) // 128
                    psa, bpa = pss.next(); psb, bpb = pss.next()
                    for kc in range(KA):
                        pe.op(lambda e: e.matmul(psa[:, 0:tw], lhsT=wa[:, kc, j * 128:(j + 1) * 128], rhs=oa[:, kc, 0:tw], start=(kc == 0), stop=(kc == KA - 1)),
                              reads=[bwa, boa], writes=[bpa], partial=(kc > 0), signal=(kc == KA - 1))
                    for kc in range(KB):
                        pe.op(lambda e: e.matmul(psb[:, 0:tw], lhsT=wb[:, kc, j * 128:(j + 1) * 128], rhs=ob[:, kc, 0:tw], start=(kc == 0), stop=(kc == KB - 1)),
                              reads=[bwb, bob], writes=[bpb], partial=(kc > 0), signal=(kc == KB - 1))
                    ga, bga = gas.next(); gb, bgb = gbs.next()
                    sp.dma(lambda e: e.dma_start(out=ga[:, 0:tw], in_=GA_d[dc, :, t0:t0 + tw]), k.db("GA_d"), bga, bga, partial=False)
                    sp.dma(lambda e: e.dma_start(out=gb[:, 0:tw], in_=GB_d[dc, :, t0:t0 + tw]), k.db("GB_d"), bgb, bgb, partial=False)
                    t1, bt1 = t1s.next(); t2, bt2 = t2s.next(); mg, bmg = mgs.next()
                    dve.op(lambda e: e.tensor_tensor(out=t1[:, 0:tw], in0=psa[:, 0:tw], in1=ga[:, 0:tw], op=ALU.mult), reads=[bpa, bga], writes=[bt1])
                    dve.op(lambda e: e.tensor_tensor(out=t2[:, 0:tw], in0=psb[:, 0:tw], in1=gb[:, 0:tw], op=ALU.mult), reads=[bpb, bgb], writes=[bt2])
                    pool.op(lambda e: e.tensor_tensor(out=mg[:, 0:tw], in0=t1[:, 0:tw], in1=t2[:, 0:tw], op=ALU.add), reads=[bt1, bt2], writes=[bmg])
                    act.dma(lambda e: e.dma_start(out=MG_d[dc, :, t0:t0 + tw], in_=mg[:, 0:tw]), bmg, k.db("MG_d"), bmg)
        sc.barrier()
    if upto <= 7:
        return

    with ExitStack() as ph:
        wos = Ring("wo", [128, KD, 512], BF16, 2, ph); mgt = Ring("mgt", [128, KD, 128], BF16, 3, ph)
        gts = Ring("gt1", [128, 512], F32, 2, ph); xs = Ring("xr", [128, 512], F32, 3, ph)
        t1s = Ring("ot1", [128, 512], F32, 2, ph); x1s = Ring("x1o", [128, 512], F32, 3, ph)
        pss = Ring("ops", [128, 512], F32, 4, ph, psum=True)
        for g0 in range(0, D, 512):
            gw = min(512, D - g0)
            wo, bwo = wos.next(); gt, bgt = gts.next()
            pool.dma(lambda e: e.dma_start(out=wo[:, :, 0:gw], in_=w_out[:, g0:g0 + gw].rearrange("(k p) n -> p k n", p=128)), k.db("w_out"), bwo, bwo, partial=False)
            sp.dma(lambda e: e.dma_start(out=gt[:, 0:gw], in_=mod_bc[:, g0:g0 + gw]), k.db("mod_bc"), bgt, bgt, partial=False)
            for tt in range(NT):
                mg, bmg = mgt.next(); xr, bxr = xs.next()
                sp.dma(lambda e: e.dma_start(out=mg[:], in_=MG_d[:, :, tt * 128:(tt + 1) * 128].rearrange("k p n -> p k n")), k.db("MG_d"), bmg, bmg, partial=False)
                sp.dma(lambda e: e.dma_start(out=xr[:, 0:gw], in_=xk[tt * 128:(tt + 1) * 128, g0:g0 + gw]), k.db("xk"), bxr, bxr, partial=False)
                ps, bps = pss.next()
                for kc in range(KD):
                    pe.op(lambda e: e.matmul(ps[:, 0:gw], lhsT=mg[:, kc, :], rhs=wo[:, kc, 0:gw], start=(kc == 0), stop=(kc == KD - 1)),
                          reads=[bmg, bwo], writes=[bps], partial=(kc > 0), signal=(kc == KD - 1))
                t1, bt1 = t1s.next(); x1, bx1 = x1s.next()
                dve.op(lambda e: e.tensor_tensor(out=t1[:, 0:gw], in0=ps[:, 0:gw], in1=gt[:, 0:gw], op=ALU.mult), reads=[bps, bgt], writes=[bt1])
                pool.op(lambda e: e.tensor_tensor(out=x1[:, 0:gw], in0=t1[:, 0:gw], in1=xr[:, 0:gw], op=ALU.add), reads=[bt1, bxr], writes=[bx1])
                act.dma(lambda e: e.dma_start(out=X1_d[tt * 128:(tt + 1) * 128, g0:g0 + gw], in_=x1[:, 0:gw]), bx1, k.db("X1_d"), bx1)
        sc.barrier()
    if upto <= 8:
        return

    moe = ExitStack(); top.enter_context(moe)
    Ind_all = S_("Ind_all", [128, NT, NE], BF16, moe); bInd = Buf("Ind_all")
    I1_all = S_("I1_all", [128, NT, NE], F32, moe); bI1 = Buf("I1_all")
    I2_all = S_("I2_all", [128, NT, NE], F32, moe); bI2 = Buf("I2_all")
    wsel = S_("wsel", [128, NT, 2], F32, moe); bwsel = Buf("wsel")
    idx_all = S_("idx_all", [128, NT, 2], I32, moe); bidx = Buf("idx_all")
    NSP_ = max(1, (KD * DE * 4 + 32767) // 32768)
    widx_i = S_("widx_i", [128, 4 * NSP_, NBLK], I32, moe); bwidx = Buf("widx_i")
    Uf = S_("Uf", [128, 128], F32, moe); bUf = Buf("Uf")
    Ub = S_("Ub", [128, 128], BF16, moe); bUb = Buf("Ub")
    jji = S_("jji", [128, 128], I32, moe); bjji = Buf("jji")
    pool.op(lambda e: e.iota(out=jji[:], pattern=[[1, 128]], base=0, channel_multiplier=0), writes=[bjji])
    ppi = S_("ppi", [128, 1], I32, moe); bppi = Buf("ppi")
    pool.op(lambda e: e.iota(out=ppi[:], pattern=[[0, 1]], base=0, channel_multiplier=1), writes=[bppi])
    ppf = S_("ppf", [128, 1], F32, moe); bppf = Buf("ppf")
    dve.op(lambda e: e.tensor_copy(out=ppf[:], in_=ppi[:]), reads=[bppi], writes=[bppf])
    dve.op(lambda e: e.tensor_copy(out=Uf[:], in_=jji[:]), reads=[bjji], writes=[bUf])
    dve.op(lambda e: e.tensor_scalar(out=Uf[:], in0=Uf[:], scalar1=ppf[:, 0:1], scalar2=None, op0=ALU.is_gt), reads=[bUf, bppf], writes=[bUf])
    dve.op(lambda e: e.tensor_copy(out=Ub[:], in_=Uf[:]), reads=[bUf], writes=[bUb])

    def bcast_row(row_ap, row_name, dst, bdst, st, pss, post=None):
        rw = S_("bc_row", [1, D], F32, st); brw = Buf("bc_row")
        sp.dma(lambda e: e.dma_start(out=rw[:], in_=row_ap), k.db(row_name), brw, brw)
        for g0 in range(0, D, 512):
            gw = min(512, D - g0)
            ps, bps = pss.next()
            pe.op(lambda e: e.matmul(ps[:, 0:gw], lhsT=ones_f[0:1, :], rhs=rw[0:1, g0:g0 + gw], start=True, stop=True), reads=[b_ones, brw], writes=[bps])
            if post is None:
                dve.op(lambda e: e.tensor_copy(out=dst[:, g0:g0 + gw], in_=ps[:, 0:gw]), reads=[bps], writes=[bdst], partial=True)
            else:
                post(ps, bps, g0, gw)

    with ExitStack() as ph:
        A2 = S_("A2", [128, D], F32, ph); bA2 = Buf("A2"); B2 = S_("B2", [128, D], F32, ph); bB2 = Buf("B2")
        pss = Ring("rps", [128, 512], F32, 4, ph, psum=True)
        sp.dma(lambda e: e.dma_start(out=A2[:], in_=mod_bc[:, 2 * D:3 * D]), k.db("mod_bc"), bA2, bA2)
        sp.dma(lambda e: e.dma_start(out=B2[:], in_=mod_bc[:, D:2 * D]), k.db("mod_bc"), bB2, bB2)
        bcast_row(g_ffn, "g_ffn", A2, bA2, ph, pss,
                  post=lambda ps, bps, g0, gw: dve.op(lambda e: e.scalar_tensor_tensor(out=A2[:, g0:g0 + gw], in0=A2[:, g0:g0 + gw], scalar=1.0, in1=ps[:, 0:gw],
                                                                                       op0=ALU.add, op1=ALU.mult), reads=[bps, bA2], writes=[bA2]))
        wr = S_("wr", [128, KD, NR], F32, ph); bwr = Buf("wr")
        sp.dma(lambda e: e.dma_start(out=wr[:], in_=w_r.rearrange("(k p) n -> p k n", p=128)), k.db("w_r"), bwr, bwr)
        brr = S_("brr", [1, NR], F32, ph); bbrr = Buf("brr")
        sp.dma(lambda e: e.dma_start(out=brr[:], in_=b_r), k.db("b_r"), bbrr, bbrr)
        x1s = Ring("cx1", [128, D], F32, 2, ph)
        h2 = S_("h2", [128, D], F32, ph); bh2 = Buf("h2")
        h2bs = Ring("h2b", [128, D], BF16, 2, ph)
        h2T = S_("h2T", [128, KD, 128], F32, ph); bh2T = Buf("h2T")
        sm = S_("rsm", [128, 16], F32, ph); bsm = Buf("rsm")
        lg = S_("lg", [128, NR], F32, ph); blg = Buf("lg")
        rt = S_("rt", [128, 8, NE], F32, ph); brt = Buf("rt")
        jk = S_("cjk", [128, D], BF16, ph); bjk = Buf("cjk")
        for tt in range(NT):
            x1, bx1 = x1s.next(); h2b, bh2b = h2bs.next()
            sp.dma(lambda e: e.dma_start(out=x1[:], in_=X1_d[tt * 128:(tt + 1) * 128, :]), k.db("X1_d"), bx1, bx1, partial=False)
            act.op(lambda e: e.activation(out=jk[:], in_=x1[:], func=AF.Square, accum_out=sm[:, 0:1]), reads=[bx1], writes=[bjk, bsm])
            dve.op(lambda e: e.tensor_scalar(out=sm[:, 1:2], in0=sm[:, 0:1], scalar1=1.0 / D, scalar2=1e-6, op0=ALU.mult, op1=ALU.add), reads=[bsm], writes=[bsm])
            act.op(lambda e: e.activation(out=sm[:, 1:2], in_=sm[:, 1:2], func=AF.Ln), reads=[bsm], writes=[bsm])
            act.op(lambda e: e.activation(out=sm[:, 1:2], in_=sm[:, 1:2], func=AF.Exp, scale=-0.5), reads=[bsm], writes=[bsm])
            dve.op(lambda e: e.scalar_tensor_tensor(out=h2[:], in0=x1[:], scalar=sm[:, 1:2], in1=A2[:], op0=ALU.mult, op1=ALU.mult),
                   reads=[bx1, bsm, bA2], writes=[bh2])
            pool.op(lambda e: e.tensor_tensor(out=h2[:], in0=h2[:], in1=B2[:], op=ALU.add), reads=[bh2, bB2], writes=[bh2])
            act.op(lambda e: e.copy(out=h2b[:], in_=h2[:]), reads=[bh2], writes=[bh2b])
            act.dma(lambda e: e.dma_start(out=H2_d[tt * 128:(tt + 1) * 128, :], in_=h2b[:]), bh2b, k.db("H2_d"), bh2b)
            for g0 in range(0, KD, 4):
                n4 = min(4, KD - g0)
                ps, bps = pss.next()
                for j in range(n4):
                    kc = g0 + j
                    pe.op(lambda e: e.transpose(out=ps[:, j * 128:(j + 1) * 128], in_=h2[:, kc * 128:(kc + 1) * 128], identity=identf[:]),
                          reads=[bh2, b_identf], writes=[bps], partial=(j > 0), signal=(j == n4 - 1))
                dve.op(lambda e: e.tensor_copy(out=h2T[:, g0:g0 + n4, :], in_=ps[:, 0:n4 * 128].rearrange("p (k n) -> p k n", n=128)),
                       reads=[bps], writes=[bh2T], partial=True)
            ps, bps = pss.next()
            for kc in range(KD):
                pe.op(lambda e: e.matmul(ps[:, 0:NR], lhsT=h2T[:, kc, :], rhs=wr[:, kc, :], start=(kc == 0), stop=False),
                      reads=[bh2T, bwr], writes=[bps], partial=(kc > 0), signal=False)
            pe.op(lambda e: e.matmul(ps[:, 0:NR], lhsT=ones_f[0:1, :], rhs=brr[0:1, :], start=False, stop=True), reads=[b_ones, bbrr], writes=[bps], partial=True)
            dve.op(lambda e: e.tensor_copy(out=lg[:], in_=ps[:, 0:NR]), reads=[bps], writes=[blg])
            gl = lg[:, 0:NG]; el = lg[:, NG:NR].rearrange("p (g e) -> p g e", e=EPG)
            ohg = rt[:, 0, 0:NG]; eg = rt[:, 6, 0:NG]
            R = lambda fn, **kw: dve.op(fn, reads=[blg, brt, bsm], writes=[brt, bsm], **kw)
            R(lambda e: e.reduce_max(out=sm[:, 2:3], in_=gl, axis=AX.X))
            R(lambda e: e.tensor_scalar(out=ohg, in0=gl, scalar1=sm[:, 2:3], scalar2=None, op0=ALU.is_ge))
            R(lambda e: e.tensor_scalar(out=sm[:, 3:4], in0=sm[:, 2:3], scalar1=-1.0, scalar2=None, op0=ALU.mult))
            act.op(lambda e: e.activation(out=eg, in_=gl, func=AF.Exp, bias=sm[:, 3:4], scale=1.0, accum_out=sm[:, 4:5]), reads=[blg, bsm], writes=[brt, bsm])
            R(lambda e: e.reciprocal(out=sm[:, 5:6], in_=sm[:, 4:5]))
            tmp = rt[:, 1, :].rearrange("p (g e) -> p g e", e=EPG)
            R(lambda e: e.tensor_tensor(out=tmp, in0=el, in1=ohg.unsqueeze(2).to_broadcast([128, NG, EPG]), op=ALU.mult))
            sel = rt[:, 2, 0:EPG]; mk1 = rt[:, 3, 0:EPG]; sel2 = rt[:, 4, 0:EPG]; mk2 = rt[:, 5, 0:EPG]
            R(lambda e: e.tensor_reduce(out=sel, in_=tmp.rearrange("p g e -> p e g"), axis=AX.X, op=ALU.add))
            R(lambda e: e.reduce_max(out=sm[:, 6:7], in_=sel, axis=AX.X))
            R(lambda e: e.tensor_scalar(out=mk1, in0=sel, scalar1=sm[:, 6:7], scalar2=None, op0=ALU.is_ge))
            R(lambda e: e.scalar_tensor_tensor(out=sel2, in0=mk1, scalar=-1e30, in1=sel, op0=ALU.mult, op1=ALU.add))
            R(lambda e: e.reduce_max(out=sm[:, 7:8], in_=sel2, axis=AX.X))
            R(lambda e: e.tensor_scalar(out=mk2, in0=sel2, scalar1=sm[:, 7:8], scalar2=None, op0=ALU.is_ge))
            R(lambda e: e.tensor_tensor(out=sm[:, 8:9], in0=sm[:, 7:8], in1=sm[:, 6:7], op=ALU.subtract))
            act.op(lambda e: e.activation(out=sm[:, 9:10], in_=sm[:, 8:9], func=AF.Exp), reads=[bsm], writes=[bsm])
            R(lambda e: e.tensor_scalar(out=sm[:, 10:11], in0=sm[:, 9:10], scalar1=1.0, scalar2=None, op0=ALU.add))
            R(lambda e: e.reciprocal(out=sm[:, 10:11], in_=sm[:, 10:11]))
            dve.op(lambda e: e.tensor_tensor(out=wsel[:, tt, 0:1], in0=sm[:, 5:6], in1=sm[:, 10:11], op=ALU.mult), reads=[bsm], writes=[bwsel], partial=True)
            dve.op(lambda e: e.tensor_tensor(out=wsel[:, tt, 1:2], in0=wsel[:, tt, 0:1], in1=sm[:, 9:10], op=ALU.mult), reads=[bsm, bwsel], writes=[bwsel], partial=True)
            bo = lambda a: a.unsqueeze(2).to_broadcast([128, NG, EPG])
            be_ = lambda a: a.unsqueeze(1).to_broadcast([128, NG, EPG])
            v3 = lambda a: a.rearrange("p (g e) -> p g e", e=EPG)
            dve.op(lambda e: e.tensor_tensor(out=v3(I1_all[:, tt, :]), in0=bo(ohg), in1=be_(mk1), op=ALU.mult), reads=[brt], writes=[bI1], partial=True)
            dve.op(lambda e: e.tensor_tensor(out=v3(I2_all[:, tt, :]), in0=bo(ohg), in1=be_(mk2), op=ALU.mult), reads=[brt], writes=[bI2], partial=True)
            dve.op(lambda e: e.tensor_tensor(out=Ind_all[:, tt, :], in0=I1_all[:, tt, :], in1=I2_all[:, tt, :], op=ALU.add), reads=[bI1, bI2], writes=[bInd], partial=True)
        cnt = S_("cnt", [128, 4, NE], F32, ph); bcnt = Buf("cnt")
        ps, bps = pss.next()
        for tt in range(NT):
            pe.op(lambda e: e.matmul(ps[:, 0:NE], lhsT=ones_b[:], rhs=Ind_all[:, tt, :], start=(tt == 0), stop=(tt == NT - 1)),
                  reads=[b_onesb, bInd], writes=[bps], partial=(tt > 0), signal=(tt == NT - 1))
        C = lambda fn, **kw: dve.op(fn, reads=[bcnt], writes=[bcnt], **kw)
        dve.op(lambda e: e.tensor_copy(out=cnt[:, 0, :], in_=ps[:, 0:NE]), reads=[bps], writes=[bcnt])
        thri = S_("thri", [128, NT], I32, ph); bthri = Buf("thri")
        pool.op(lambda e: e.iota(out=thri[:], pattern=[[128, NT]], base=0, channel_multiplier=0), writes=[bthri])
        thrf = S_("thrf", [128, NT], F32, ph); bthrf = Buf("thrf")
        dve.op(lambda e: e.tensor_copy(out=thrf[:], in_=thri[:]), reads=[bthri], writes=[bthrf])
        cmpc = S_("cmpc", [128, NE, NT], F32, ph); bcmpc = Buf("cmpc")
        dve.op(lambda e: e.tensor_tensor(out=cmpc[:], in0=cnt[:, 0, :].unsqueeze(2).to_broadcast([128, NE, NT]),
                                         in1=thrf[:].unsqueeze(1).to_broadcast([128, NE, NT]), op=ALU.is_gt), reads=[bcnt, bthrf], writes=[bcmpc])
        dve.op(lambda e: e.reduce_sum(out=cnt[:, 1, :], in_=cmpc[:], axis=AX.X), reads=[bcmpc], writes=[bcnt])
        C(lambda e: e.tensor_scalar(out=cnt[:, 1, :], in0=cnt[:, 1, :], scalar1=128.0, scalar2=None, op0=ALU.mult))
        ps, bps = pss.next()
        pe.op(lambda e: e.transpose(out=ps[0:NE, 0:128], in_=cnt[:, 1, :], identity=identf[:]), reads=[bcnt, b_identf], writes=[bps])
        pcT = S_("pcT", [128, 128], F32, ph); bpcT = Buf("pcT")
        dve.op(lambda e: e.tensor_copy(out=pcT[0:NE, :], in_=ps[0:NE, 0:128]), reads=[bps], writes=[bpcT])
        ps, bps = pss.next()
        pe.op(lambda e: e.matmul(ps[:, 0:NE], lhsT=pcT[0:NE, :], rhs=Uf[0:NE, 0:NE], start=True, stop=True), reads=[bpcT, bUf], writes=[bps])
        dve.op(lambda e: e.tensor_copy(out=cnt[:, 2, :], in_=ps[:, 0:NE]), reads=[bps], writes=[bcnt])
        C(lambda e: e.tensor_tensor(out=cnt[:, 3, :], in0=cnt[:, 2, :], in1=cnt[:, 1, :], op=ALU.add))
        posf = S_("posf", [128, NT, 2], F32, ph); bposf = Buf("posf")
        pz = S_("pz", [128, 2, NE], F32, ph); bpz = Buf("pz")
        for tt in range(NT):
            ps, bps = pss.next()
            for t2 in range(tt):
                pe.op(lambda e: e.matmul(ps[:, 0:NE], lhsT=ones_b[:], rhs=Ind_all[:, t2, :], start=(t2 == 0), stop=False),
                      reads=[b_onesb, bInd], writes=[bps], partial=(t2 > 0), signal=False)
            pe.op(lambda e: e.matmul(ps[:, 0:NE], lhsT=Ub[:], rhs=Ind_all[:, tt, :], start=(tt == 0), stop=True), reads=[bUb, bInd], writes=[bps], partial=(tt > 0))
            dve.op(lambda e: e.tensor_tensor(out=pz[:, 0, :], in0=ps[:, 0:NE], in1=cnt[:, 2, :], op=ALU.add), reads=[bps, bcnt], writes=[bpz])
            dve.op(lambda e: e.tensor_tensor(out=pz[:, 1, :], in0=pz[:, 0, :], in1=I1_all[:, tt, :], op=ALU.mult), reads=[bpz, bI1], writes=[bpz])
            dve.op(lambda e: e.reduce_sum(out=posf[:, tt, 0:1], in_=pz[:, 1, :], axis=AX.X), reads=[bpz], writes=[bposf], partial=True)
            dve.op(lambda e: e.tensor_tensor(out=pz[:, 1, :], in0=pz[:, 0, :], in1=I2_all[:, tt, :], op=ALU.mult), reads=[bpz, bI2], writes=[bpz])
            dve.op(lambda e: e.reduce_sum(out=posf[:, tt, 1:2], in_=pz[:, 1, :], axis=AX.X), reads=[bpz], writes=[bposf], partial=True)
        dve.op(lambda e: e.tensor_copy(out=idx_all[:], in_=posf[:]), reads=[bposf], writes=[bidx])
        bsi = S_("bsi", [128, NBLK], I32, ph); bbsi = Buf("bsi")
        pool.op(lambda e: e.iota(out=bsi[:], pattern=[[128, NBLK]], base=0, channel_multiplier=0), writes=[bbsi])
        bsf = S_("bsf", [128, NBLK], F32, ph); bbsf = Buf("bsf")
        dve.op(lambda e: e.tensor_copy(out=bsf[:], in_=bsi[:]), reads=[bbsi], writes=[bbsf])
        pidi = S_("pidi", [128, 1], I32, ph); bpidi = Buf("pidi")
        pool.op(lambda e: e.iota(out=pidi[:], pattern=[[0, 1]], base=0, channel_multiplier=1), writes=[bpidi])
        pidf = S_("pidf", [128, 1], F32, ph); bpidf = Buf("pidf")
        dve.op(lambda e: e.tensor_copy(out=pidf[:], in_=pidi[:]), reads=[bpidi], writes=[bpidf])
        cmp_ = S_("cmp", [128, NBLK, NE], F32, ph); bcmp = Buf("cmp")
        dve.op(lambda e: e.tensor_tensor(out=cmp_[:], in0=cnt[:, 3, :].unsqueeze(1).to_broadcast([128, NBLK, NE]),
                                         in1=bsf[:].unsqueeze(2).to_broadcast([128, NBLK, NE]), op=ALU.is_le), reads=[bcnt, bbsf], writes=[bcmp])
        bef = S_("bef", [128, NBLK], F32, ph); bbef = Buf("bef")
        dve.op(lambda e: e.reduce_sum(out=bef[:], in_=cmp_[:], axis=AX.X), reads=[bcmp], writes=[bbef])
        dve.op(lambda e: e.tensor_scalar(out=bef[:], in0=bef[:], scalar1=float(NE - 1), scalar2=None, op0=ALU.min), reads=[bbef], writes=[bbef])
        wq = S_("wq", [128, 3, NBLK], F32, ph); bwq = Buf("wq")
        nsame = S_("nsame", [128, NBLK], F32, ph); bns = Buf("nsame")
        dve.op(lambda e: e.memset(nsame[:], 1.0), writes=[bns])
        dve.op(lambda e: e.tensor_tensor(out=nsame[:, 1:NBLK], in0=bef[:, 1:NBLK], in1=bef[:, 0:NBLK - 1], op=ALU.not_equal), reads=[bbef], writes=[bns])
        for q in range(NP):
            dve.op(lambda e: e.tensor_scalar(out=wq[:, 0, :], in0=bef[:], scalar1=float(q * EPP), scalar2=None, op0=ALU.is_ge), reads=[bbef], writes=[bwq])
            dve.op(lambda e: e.tensor_scalar(out=wq[:, 1, :], in0=bef[:], scalar1=float((q + 1) * EPP), scalar2=None, op0=ALU.is_lt), reads=[bbef], writes=[bwq], partial=True)
            dve.op(lambda e: e.tensor_tensor(out=wq[:, 0, :], in0=wq[:, 0, :], in1=wq[:, 1, :], op=ALU.mult), reads=[bwq], writes=[bwq])
            dve.op(lambda e: e.tensor_tensor(out=wq[:, 0, :], in0=wq[:, 0, :], in1=nsame[:], op=ALU.mult), reads=[bwq, bns], writes=[bwq])
            dve.op(lambda e: e.tensor_scalar(out=wq[:, 1, :], in0=bef[:], scalar1=128.0, scalar2=pidf[:, 0:1], op0=ALU.mult, op1=ALU.add),
                   reads=[bbef, bpidf, bwq], writes=[bwq])
            dve.op(lambda e: e.tensor_scalar(out=wq[:, 1, :], in0=wq[:, 1, :], scalar1=-float(q * EPP * 128) - 1.0e6, scalar2=None, op0=ALU.add), reads=[bwq], writes=[bwq])
            dve.op(lambda e: e.tensor_tensor(out=wq[:, 1, :], in0=wq[:, 1, :], in1=wq[:, 0, :], op=ALU.mult), reads=[bwq], writes=[bwq])
            dve.op(lambda e: e.tensor_scalar(out=wq[:, 1, :], in0=wq[:, 1, :], scalar1=1.0e6, scalar2=None, op0=ALU.add), reads=[bwq], writes=[bwq])
            for si in range(NSP_):
                dve.op(lambda e: e.tensor_scalar(out=wq[:, 2, :], in0=wq[:, 1, :], scalar1=float(NSP_), scalar2=float(si), op0=ALU.mult, op1=ALU.add),
                       reads=[bwq], writes=[bwq])
                dve.op(lambda e: e.tensor_copy(out=widx_i[:, q * NSP_ + si, :], in_=wq[:, 2, :]), reads=[bwq], writes=[bwidx], partial=(q + si > 0))
        if "dbg_route" in debug_outs:
            for nm, t_, b_, shp, dt_ in (("dbg_idx", idx_all, bidx, [128, NT * 2], I32), ("dbg_widx", widx_i, bwidx, [128, 4 * NSP_ * NBLK], I32),
                                         ("dbg_wsel", wsel, bwsel, [128, NT * 2], F32)):
                o_ = nc.dram_tensor(nm, shp, dt_, kind="ExternalOutput").ap(); k.dbufs[nm] = Buf(nm)
                src_ = t_[:].rearrange("p a b -> p (a b)") if len(t_.shape) == 3 else t_[:]
                sp.dma(lambda e: e.dma_start(out=o_, in_=src_), b_, k.db(nm), b_)
        sc.barrier()
    if upto <= 9:
        return

    NSP = max(1, (KD * DE * 4 + 32767) // 32768)
    assert NSP == max(1, (HC * D * 4 + 32767) // 32768) and KD % NSP == 0 and HC % NSP == 0
    bnd_reg = nc.gpsimd.to_reg(EPP * 128 * NSP - 1)
    w1v = [w.rearrange("(r c) f -> r (c f)", c=KD // NSP) for w in w1p]; w3v = [w.rearrange("(r c) f -> r (c f)", c=KD // NSP) for w in w3p]
    w2v = [w.rearrange("(r c) f -> r (c f)", c=HC // NSP) for w in w2p]
    with ExitStack() as ph:
        zt = S_("zt", [128, D], BF16, ph); bzt = Buf("zt")
        dve.op(lambda e: e.memset(zt[:], 0.0), writes=[bzt])
        for b in range(NBLK):
            sp.dma(lambda e: e.dma_start(out=Xs_d[b * 128:(b + 1) * 128, :], in_=zt[:]), bzt, k.db("Xs_d"), bzt)
        hbs = Ring("sh2", [128, D], BF16, 2, ph)
        for tt in range(NT):
            hb, bhb = hbs.next()
            sp.dma(lambda e: e.dma_start(out=hb[:], in_=H2_d[tt * 128:(tt + 1) * 128, :]), k.db("H2_d"), bhb, bhb, partial=False)
            for kk in range(2):
                pool.wait(dict(bidx.w));
                pool.dma(lambda e: e.indirect_dma_start(out=Xs_d[:, :], out_offset=bass.IndirectOffsetOnAxis(ap=idx_all[:, tt, kk:kk + 1], axis=0),
                                                        in_=hb[:, :], in_offset=None), bhb, k.db("Xs_d"), bhb, partial=False)
        w1s = S_("w1s", [128, KD * DE], BF16, ph); bw1 = Buf("w1s")
        w3s = S_("w3s", [128, KD * DE], BF16, ph); bw3 = Buf("w3s")
        w2s = S_("w2s", [128, HC * D], BF16, ph); bw2 = Buf("w2s")
        xbs = Ring("xb", [128, D], BF16, 2, ph); xts = Ring("xbT", [128, KD, 128], BF16, 2, ph)
        hid = S_("hidT", [128, HC, 128], BF16, ph); bhid = Buf("hidT")
        sl = Ring("sil", [128, 128], F32, 2, ph)
        yb = S_("yb", [128, D], F32, ph); byb = Buf("yb")
        pst = Ring("bpt", [128, 1024], BF16, 2, ph, psum=True)
        pss = Ring("bps", [128, 512], F32, 4, ph, psum=True)
        for b in range(NBLK):
            pool.wait(dict(bwidx.w))
            for (ws, bw, wv, nm, rowlen) in ((w1s, bw1, w1v, "w1", KD * DE), (w3s, bw3, w3v, "w3", KD * DE), (w2s, bw2, w2v, "w2", HC * D)):
                cw = rowlen // NSP
                for q in range(NP):
                    for si in range(NSP):
                        pool.dma(lambda e: e.indirect_dma_start(out=ws[:, si * cw:(si + 1) * cw], out_offset=None, in_=wv[q][:, :],
                                                                in_offset=bass.IndirectOffsetOnAxis(ap=widx_i[:, q * NSP + si, b:b + 1], axis=0),
                                                                bounds_check=bnd_reg, oob_is_err=False),
                                 k.db("%s_%d" % (nm, q)), bw, bw, partial=(q + si > 0))
            xb, bxb = xbs.next(); xT, bxT = xts.next()
            sp.dma(lambda e: e.dma_start(out=xb[:], in_=Xs_d[b * 128:(b + 1) * 128, :]), k.db("Xs_d"), bxb, bxb, partial=False)
            for g0 in range(0, KD, 8):
                n8 = min(8, KD - g0)
                ps, bps = pst.next()
                for j in range(n8):
                    cc = g0 + j
                    pe.op(lambda e: e.transpose(out=ps[:, j * 128:(j + 1) * 128], in_=xb[:, cc:D:KD], identity=ident[:]),
                          reads=[bxb, b_ident], writes=[bps], partial=(j > 0), signal=(j == n8 - 1))
                dve.op(lambda e: e.tensor_copy(out=xT[:, g0:g0 + n8, :], in_=ps[:, 0:n8 * 128].rearrange("p (k n) -> p k n", n=128)),
                       reads=[bps], writes=[bxT], partial=True)
            for c2 in range(HC):
                p1, bp1 = pss.next(); p3, bp3 = pss.next()
                for (pp, bpp, ws, bw) in ((p1, bp1, w1s, bw1), (p3, bp3, w3s, bw3)):
                    for cc in range(KD):
                        pe.op(lambda e: e.matmul(pp[:, 0:128], lhsT=ws[:, cc * DE + c2:(cc + 1) * DE:HC], rhs=xT[:, cc, :], start=(cc == 0), stop=(cc == KD - 1)),
                              reads=[bw, bxT], writes=[bpp], partial=(cc > 0), signal=(cc == KD - 1))
                s_, bs_ = sl.next()
                act.op(lambda e: e.activation(out=s_[:], in_=p1[:, 0:128], func=AF.Silu), reads=[bp1], writes=[bs_])
                dve.op(lambda e: e.tensor_tensor(out=hid[:, c2, :], in0=p3[:, 0:128], in1=s_[:], op=ALU.mult), reads=[bp3, bs_], writes=[bhid], partial=(c2 > 0))
            for gi, g0 in enumerate(range(0, D, 512)):
                gw = min(512, D - g0)
                ps, bps = pss.next()
                for c2 in range(HC):
                    pe.op(lambda e: e.matmul(ps[:, 0:gw], lhsT=hid[:, c2, :], rhs=w2s[:, c2 * D + g0:c2 * D + g0 + gw], start=(c2 == 0), stop=(c2 == HC - 1)),
                          reads=[bhid, bw2], writes=[bps], partial=(c2 > 0), signal=(c2 == HC - 1))
                if gi % 2 == 0:
                    dve.op(lambda e: e.tensor_copy(out=yb[:, g0:g0 + gw], in_=ps[:, 0:gw]), reads=[bps], writes=[byb], partial=(gi > 0))
                else:
                    act.op(lambda e: e.copy(out=yb[:, g0:g0 + gw], in_=ps[:, 0:gw]), reads=[bps], writes=[byb], partial=True)
            act.dma(lambda e: e.dma_start(out=Ys_d[b * 128:(b + 1) * 128, :], in_=yb[:]), byb, k.db("Ys_d"), byb)
        sc.barrier()
    if upto <= 10:
        return

    with ExitStack() as ph:
        pss = Ring("fps", [128, 512], F32, 2, ph, psum=True)
        gt2 = S_("gt2", [128, D], F32, ph); bgt2 = Buf("gt2"); gfb = S_("gfb", [128, D], F32, ph); bgfb = Buf("gfb")
        sp.dma(lambda e: e.dma_start(out=gt2[:], in_=mod_bc[:, 3 * D:4 * D]), k.db("mod_bc"), bgt2, bgt2)
        bcast_row(g_final, "g_final", gfb, bgfb, ph, pss)
        y0 = S_("y0", [128, D], F32, ph); by0 = Buf("y0"); y1 = S_("y1", [128, D], F32, ph); by1 = Buf("y1")
        x1s = Ring("fx1", [128, D], F32, 2, ph)
        fo = S_("fo", [128, D], F32, ph); bfo = Buf("fo")
        fjk = S_("fjk", [128, D], BF16, ph); bfjk = Buf("fjk")
        fs = S_("fs", [128, 2], F32, ph); bfs = Buf("fs")
        for tt in range(NT):
            x1, bx1 = x1s.next()
            sp.dma(lambda e: e.dma_start(out=x1[:], in_=X1_d[tt * 128:(tt + 1) * 128, :]), k.db("X1_d"), bx1, bx1, partial=False)
            pool.wait(dict(bidx.w))
            pool.dma(lambda e: e.indirect_dma_start(out=y0[:, :], out_offset=None, in_=Ys_d[:, :],
                                                    in_offset=bass.IndirectOffsetOnAxis(ap=idx_all[:, tt, 0:1], axis=0)), k.db("Ys_d"), by0, by0, partial=False)
            pool.dma(lambda e: e.indirect_dma_start(out=y1[:, :], out_offset=None, in_=Ys_d[:, :],
                                                    in_offset=bass.IndirectOffsetOnAxis(ap=idx_all[:, tt, 1:2], axis=0)), k.db("Ys_d"), by1, by1, partial=False)
            dve.op(lambda e: e.tensor_scalar(out=y0[:], in0=y0[:], scalar1=wsel[:, tt, 0:1], scalar2=None, op0=ALU.mult), reads=[by0, bwsel], writes=[by0])
            dve.op(lambda e: e.scalar_tensor_tensor(out=y0[:], in0=y1[:], scalar=wsel[:, tt, 1:2], in1=y0[:], op0=ALU.mult, op1=ALU.add),
                   reads=[by1, by0, bwsel], writes=[by0])
            pool.op(lambda e: e.tensor_tensor(out=y0[:], in0=y0[:], in1=gt2[:], op=ALU.mult), reads=[by0, bgt2], writes=[by0])
            dve.op(lambda e: e.tensor_tensor(out=fo[:], in0=y0[:], in1=x1[:], op=ALU.add), reads=[by0, bx1], writes=[bfo])
            act.op(lambda e: e.activation(out=fjk[:], in_=fo[:], func=AF.Square, accum_out=fs[:, 0:1]), reads=[bfo], writes=[bfjk, bfs])
            dve.op(lambda e: e.tensor_scalar(out=fs[:, 1:2], in0=fs[:, 0:1], scalar1=1.0 / D, scalar2=1e-6, op0=ALU.mult, op1=ALU.add), reads=[bfs], writes=[bfs])
            act.op(lambda e: e.activation(out=fs[:, 1:2], in_=fs[:, 1:2], func=AF.Ln), reads=[bfs], writes=[bfs])
            act.op(lambda e: e.activation(out=fs[:, 1:2], in_=fs[:, 1:2], func=AF.Exp, scale=-0.5), reads=[bfs], writes=[bfs])
            dve.op(lambda e: e.scalar_tensor_tensor(out=fo[:], in0=fo[:], scalar=fs[:, 1:2], in1=gfb[:], op0=ALU.mult, op1=ALU.mult),
                   reads=[bfo, bfs, bgfb], writes=[bfo])
            sp.dma(lambda e: e.dma_start(out=out[tt * 128:(tt + 1) * 128, :], in_=fo[:]), bfo, k.db("out"), bfo)
        sc.barrier()


def rope_tables(cfg, half):
    c = cfg
    pos = np.concatenate([np.arange(c.NQ) + half * c.NQ, np.arange(c.NQ) + (1 - half) * c.NQ])
    row = (pos // c.GW).astype(np.float64); col = (pos % c.GW).astype(np.float64)
    def tab(rot):
        q = rot // 4
        inv = 10000.0 ** (-np.arange(q, dtype=np.float64) / q)
        ang = np.concatenate([row[:, None] * inv, col[:, None] * inv], axis=-1)
        hd = rot // 2
        cs = np.ones((rot, c.NK), np.float32); sn = np.zeros((rot, c.NK), np.float32)
        cs[:hd, :c.S] = np.cos(ang).T; cs[hd:, :c.S] = np.cos(ang).T
        sn[:hd, :c.S] = -np.sin(ang).T; sn[hd:, :c.S] = np.sin(ang).T
        return cs, sn
    cd, sd = tab(128); cm, sm = tab(64)
    return dict(cosd=cd, sind=sd, cosm=cm, sinm=sm)


def weight_pieces(c, inp):
    g = lambda n: np.asarray(inp[n][0]).reshape(-1, np.asarray(inp[n]).shape[-1])
    out = {}
    wm = g("w_mod")
    out["w_mod_0"] = wm[:, :3 * c.D]; out["w_mod_1"] = wm[:, 3 * c.D:]
    for n in ("w_in", "w_uq", "w_ukv", "w_br_a", "w_br_b", "w_out"):
        out[n] = g(n)
    epp = c.NE // 4
    for n, rpe in (("w1", c.D), ("w3", c.D), ("w2", c.DE)):
        w = g(n)
        for q in range(4):
            out["%s_%d" % (n, q)] = w[q * epp * rpe:(q + 1) * epp * rpe]
    return out


SHARD_POS = {0: 0, 4: 1, 1: 2, 5: 3, 2: 4, 6: 5, 3: 6, 7: 7}


GATHER_CHUNK_BYTES = 512 * 1024


def gather_unit(rows, cols):
    r8 = rows // 8
    u = max(1, min(r8, (GATHER_CHUNK_BYTES) // (cols * 4)))
    while r8 % u:
        u -= 1
    return u


def prep_core_inputs(cfg, inp, core, gather):
    c = cfg
    b, half = (core // 2, core % 2) if c.NCORES > 1 else (0, 0)
    f = lambda a: np.ascontiguousarray(np.asarray(a, dtype=np.float32))
    x = np.asarray(inp["x"][b]); ctx = np.asarray(inp["ctx"][b])
    own = x[half * c.NQ:(half + 1) * c.NQ]; oth = x[(1 - half) * c.NQ:(2 - half) * c.NQ]
    m = dict(xk=f(np.concatenate([own, oth, ctx], axis=0)), c_b=f(inp["c"][b:b + 1]), c_ctx=f(np.asarray(inp["c_ctx"])[None, :]))
    m.update(rope_tables(c, half))
    for n in ("b_mod", "g_attn", "g_q", "g_kv", "g_subln", "g_ffn", "lam_q1", "lam_k1", "lam_q2", "lam_k2"):
        m[n] = f(np.asarray(inp[n]).reshape(1, -1))
    m["g_final"] = f(np.asarray(inp["g_final"]).reshape(1, -1))
    m["w_r"] = f(np.concatenate([np.asarray(inp["w_grp"][0]), np.asarray(inp["w_exp"][0])], axis=1))
    m["b_r"] = f(np.concatenate([np.asarray(inp["b_grp"][0]), np.asarray(inp["b_exp"][0])])[None, :])
    for n, w2 in weight_pieces(c, inp).items():
        if gather:
            rows, cols = w2.shape
            u = gather_unit(rows, cols); nch = rows // 8 // u
            t_ = SHARD_POS[core]
            m[n + "_sh"] = f(w2.reshape(nch, 8, u, cols)[:, t_].reshape(nch * u, cols))
        else:
            m[n] = f(w2)
    return m


_NC_CACHE = {}


def kernel(**inputs):
    cfg = Cfg()
    n = cfg.NCORES
    if "nc" not in _NC_CACHE:
        _NC_CACHE["nc"] = build_program(cfg, gather=True)
    nc = _NC_CACHE["nc"]
    need = set()
    for a in nc.allocations:
        try:
            if a.kind == "ExternalInput":
                need.add(a.memorylocations[0].name)
        except Exception:
            pass
    in_maps = []
    for core in range(n):
        m = prep_core_inputs(cfg, inputs, core, gather=True)
        in_maps.append({k_: v for k_, v in m.items() if k_ in need})
    res = run_bass_kernel_spmd(nc, in_maps, core_ids=list(range(n)))
    out = np.empty((cfg.B, cfg.S, cfg.D), np.float32)
    for core in range(n):
        b, half = core // 2, core % 2
        out[b, half * cfg.NQ:(half + 1) * cfg.NQ] = res.results[core]["out"]
    return out
```

```python
import math
from contextlib import ExitStack
import numpy as np
import concourse.bass as bass
import concourse.mybir as mybir
from concourse.bass_utils import run_bass_kernel_spmd

F32 = mybir.dt.float32
BF16 = mybir.dt.bfloat16
I32 = mybir.dt.int32
AF = mybir.ActivationFunctionType
ALU = mybir.AluOpType
AX = mybir.AxisListType


class Cfg:
    def __init__(self, **kw):
        self.D = 4096; self.B = 4; self.S = 4096; self.GW = 64; self.C = 256
        self.DAH = 8; self.MH = 16; self.QL = 1024; self.KVL = 512
        self.NG = 8; self.EPG = 8; self.DE = 512; self.NCORES = 8
        self.lambda_init = 0.8 - 0.6 * math.exp(-0.3 * 0)
        for k, v in kw.items():
            setattr(self, k, v)
        self.KD = self.D // 128
        self.NQ = self.S // 2
        self.NK = self.S + self.C
        self.NE = self.NG * self.EPG
        self.DAW = self.DAH * 256
        self.MW = self.MH * 128
        self.INC = 3 * self.DAW + self.QL + self.KVL + 64 + 2 * self.D
        self.o_dq = 0; self.o_dk = self.DAW; self.o_dv = 2 * self.DAW
        self.o_cq = 3 * self.DAW; self.o_ckv = self.o_cq + self.QL
        self.o_ga = self.o_ckv + self.KVL + 64; self.o_gb = self.o_ga + self.D


class Buf:
    def __init__(self, name, sem=None):
        self.name = name
        self.w = {}
        self.r = {}
        self.sem = sem
        self.ndma = 0


def _merge(d, s):
    for k, v in s.items():
        if d.get(k, 0) < v:
            d[k] = v


class Eng:
    def __init__(self, sched, name, e, sem):
        self.s = sched; self.name = name; self.e = e; self.sem = sem
        self.cnt = 0
        self.seen = {}
        self.pend_r = []; self.pend_w = []

    def wait(self, deps):
        for k, v in deps.items():
            if self.seen.get(k, 0) < v:
                self.e.wait_ge(self.s.sems[k], v)
                self.seen[k] = v

    def _deps(self, reads, writes, partial):
        deps = {}
        for t in reads:
            _merge(deps, t.w)
        for t in writes:
            _merge(deps, t.r)
            if not partial:
                _merge(deps, t.w)
        return deps

    def op(self, fn, reads=(), writes=(), partial=False, signal=True):
        self.wait(self._deps(reads, writes, partial))
        ins = fn(self.e)
        self.pend_r += list(reads); self.pend_w += list(writes)
        if signal:
            self.cnt += 1
            ins.then_inc(self.sem_h, 1)
            ev = {self.sem: self.cnt}
            self.s.last[self.sem] = self.cnt
            for t in self.pend_r:
                _merge(t.r, ev)
            for t in self.pend_w:
                _merge(t.w, ev)
            self.pend_r = []; self.pend_w = []
        return ins

    def dma(self, fn, src, dst, via, partial=True, inc=16):
        self.wait(self._deps([src], [dst], partial))
        ins = fn(self.e)
        k = self.s.sem_of(via, self.name == 'pool')
        self.s.semcnt[k] = self.s.semcnt.get(k, 0) + inc
        ins.then_inc(self.s.sems[k], inc)
        ev = {k: self.s.semcnt[k]}
        self.s.last[k] = self.s.semcnt[k]
        _merge(src.r, ev)
        _merge(dst.w, ev)
        return ins


class Sched:
    def __init__(self, nc, stack):
        self.nc = nc
        self.sems = {}
        self.last = {}
        self.stack = stack
        self.n = 0
        self.dpool = []
        self.dpool_sw = []
        self.n_sw = 0
        self.semcnt = {}
        self.pe = self._eng("pe", nc.tensor)
        self.act = self._eng("act", nc.scalar)
        self.dve = self._eng("dve", nc.vector)
        self.pool = self._eng("pool", nc.gpsimd)
        self.sp = self._eng("sp", nc.sync)
        self.engs = [self.pe, self.act, self.dve, self.pool, self.sp]
        self.ccsem = self._newsem('ccsem'); self.ccn = 0

    def _newsem(self, name):
        h = self.stack.enter_context(self.nc.semaphore(name))
        k = name
        self.sems[k] = h
        return k

    def _eng(self, name, e):
        k = self._newsem("e_" + name)
        en = Eng(self, name, e, k)
        en.sem_h = self.sems[k]
        return en

    def sem_of(self, buf, sw=False):
        key = "sem_sw" if sw else "sem"
        if getattr(buf, key, None) is None:
            pl = self.dpool_sw if sw else self.dpool
            if len(pl) < 40:
                pl.append(self._newsem(("s%d" if sw else "d%d") % len(pl)))
            cnt = self.n_sw if sw else self.n
            setattr(buf, key, pl[cnt % 40])
            if sw:
                self.n_sw += 1
            else:
                self.n += 1
        return getattr(buf, key)

    def barrier(self, engs=None):
        d = dict(self.last)
        d.pop(self.ccsem, None)
        for en in (engs or self.engs):
            en.wait(d)


class K:
    def __init__(self, cfg, gather):
        self.cfg = cfg
        self.gather = gather
        self.nc = bass.Bass("TRN2", target_bir_lowering=False)
        self.ins = {}
        self.dbufs = {}

    def dram(self, name, shape, dt, kind="Internal"):
        t = self.nc.dram_tensor(name, list(shape), dt, kind=kind).ap()
        self.dbufs[name] = Buf(name)
        return t

    def db(self, name):
        return self.dbufs[name]


def build_program(cfg, gather=True, upto=99, debug_outs=()):
    k = K(cfg, gather)
    nc = k.nc
    c = cfg
    D, KD, NQ, NK, S, C = c.D, c.KD, c.NQ, c.NK, c.S, c.C
    with ExitStack() as top:
        sc = Sched(nc, top)
        pe, act, dve, pool, sp = sc.pe, sc.act, sc.dve, sc.pool, sc.sp
        _build(k, sc, top, upto, debug_outs)
    return nc


def _build(k, sc, top, upto, debug_outs):
    nc = k.nc; c = k.cfg
    D, KD, NQ, NK, S, C = c.D, c.KD, c.NQ, c.NK, c.S, c.C
    pe, act, dve, pool, sp = sc.pe, sc.act, sc.dve, sc.pool, sc.sp
    dbg = lambda n: ("ExternalOutput" if n in debug_outs else "Internal")

    def inp(name, shape, dt=F32):
        t = nc.dram_tensor(name, list(shape), dt, kind="ExternalInput").ap()
        k.dbufs[name] = Buf(name)
        return t

    def scratch(name, shape, dt):
        return k.dram(name, shape, dt, kind=dbg(name))

    pending_gathers = []

    def gather_group():
        grp = list(pending_gathers); del pending_gathers[:]
        for (name, shi, pair, full, u, nch) in grp:
            pool.wait(dict(k.db(name + "_shi").w))
        for (name, shi, pair, full, u, nch) in grp:
            for i in range(nch):
                ins = pool.e.collective_compute("AllGather", ALU.bypass, replica_groups=[[0, 4], [1, 5], [2, 6], [3, 7]],
                                                ins=[shi[i * u:(i + 1) * u, :]], outs=[pair[i * 2 * u:(i + 1) * 2 * u, :]])
                sc.ccn += 1
                ins.then_inc(sc.sems[sc.ccsem], 1)
        for (name, shi, pair, full, u, nch) in grp:
            for i in range(nch):
                ins = pool.e.collective_compute("AllGather", ALU.bypass, replica_groups=[[0, 1, 2, 3], [4, 5, 6, 7]],
                                                ins=[pair[i * 2 * u:(i + 1) * 2 * u, :]], outs=[full[i * 8 * u:(i + 1) * 8 * u, :]])
                sc.ccn += 1
                ins.then_inc(sc.sems[sc.ccsem], 1)
            sc.last[sc.ccsem] = sc.ccn
            _merge(k.db(name).w, {sc.ccsem: sc.ccn})

    def weight(name, rows, cols):
        if not k.gather:
            return inp(name, [rows, cols])
        r8 = rows // 8
        u = gather_unit(rows, cols); nch = r8 // u
        sh = inp(name + "_sh", [r8, cols])
        shi = k.dram(name + "_shi", [r8, cols], F32)
        pair = k.dram(name + "_pr", [2 * r8, cols], F32)
        full = k.dram(name, [rows, cols], F32)
        hc = Buf("cp_" + name)
        sp.dma(lambda e: e.dma_start(out=shi, in_=sh), k.db(name + "_sh"), k.db(name + "_shi"), hc)
        pending_gathers.append((name, shi, pair, full, u, nch))
        return full

    xk = inp("xk", [NK, D])
    c_b = inp("c_b", [1, D]); c_ctx = inp("c_ctx", [1, D])
    b_mod = inp("b_mod", [1, 6 * D]); g_attn = inp("g_attn", [1, D])
    out = nc.dram_tensor("out", [NQ, D], F32, kind="ExternalOutput").ap(); k.dbufs["out"] = Buf("out")
    w_mod_p = [weight("w_mod_%d" % i, D, 3 * D) for i in range(2)]
    w_in = weight("w_in", D, c.INC)
    w_uq = weight("w_uq", c.QL, c.MH * 192); w_ukv = weight("w_ukv", c.KVL, c.MH * 256)
    w_br_a = weight("w_br_a", c.DAW, D); w_br_b = weight("w_br_b", c.MW, D); w_out = weight("w_out", D, D)
    NP = 4; EPP = c.NE // NP
    w1p = [weight("w1_%d" % q, EPP * D, c.DE) for q in range(NP)]; w3p = [weight("w3_%d" % q, EPP * D, c.DE) for q in range(NP)]
    w2p = [weight("w2_%d" % q, EPP * c.DE, D) for q in range(NP)]
    if k.gather:
        gather_group()

    uid = [0]
    def S_(name, shape, dt, st):
        uid[0] += 1
        return st.enter_context(nc.sbuf_tensor("%s_%d" % (name, uid[0]), list(shape), dt))
    def P_(name, shape, dt, st):
        uid[0] += 1
        return st.enter_context(nc.psum_tensor("%s_%d" % (name, uid[0]), list(shape), dt))

    mod_bc = scratch("mod_bc", [128, 4 * D], F32)
    A1T = S_("A1T", [128, KD, 2], F32, top); B1T = S_("B1T", [128, KD, 2], F32, top)
    bA1T = Buf("A1T"); bB1T = Buf("B1T")
    ones_f = S_("ones_f", [128, 128], F32, top); b_ones = Buf("ones_f")
    dve.op(lambda e: e.memset(ones_f[:], 1.0), writes=[b_ones])

    with ExitStack() as ph:
        GC = 256
        cT = S_("cT", [128, KD, 2], F32, ph); bcT = Buf("cT")
        sT = S_("sT", [128, KD, 2], F32, ph); bsT = Buf("sT")
        srep = S_("srep", [128, KD, 128], F32, ph); bsrep = Buf("srep")
        bmT = S_("bmT", [128, 2 * KD], F32, ph); bbmT = Buf("bmT")
        gaT = S_("gaT", [128, KD], F32, ph); bgaT = Buf("gaT")
        brow = S_("brow", [1, 4 * D], F32, ph); bbrow = Buf("brow")
        modT = S_("modT", [128, 2 * KD, 2], F32, ph); bmodT = Buf("modT")
        wts = [S_("wm%d" % i, [128, KD, GC], F32, ph) for i in range(2)]; bwts = [Buf("wm%d" % i) for i in range(2)]
        evs = [S_("mev%d" % i, [128, GC], F32, ph) for i in range(2)]; bevs = [Buf("mev%d" % i) for i in range(2)]
        pss = [P_("mps%d" % i, [128, 512], F32, ph) for i in range(2)]; bpss = [Buf("mps%d" % i) for i in range(2)]
        sp.dma(lambda e: e.dma_start(out=cT[:, :, 0], in_=c_b.rearrange("o (k p) -> p (o k)", p=128), allow_slow_non_contiguous=True),
               k.db("c_b"), bcT, bcT)
        sp.dma(lambda e: e.dma_start(out=cT[:, :, 1], in_=c_ctx.rearrange("o (k p) -> p (o k)", p=128), allow_slow_non_contiguous=True),
               k.db("c_ctx"), bcT, bcT)
        sp.dma(lambda e: e.dma_start(out=bmT[:], in_=b_mod[:, 0:2 * D].rearrange("o (k p) -> p (o k)", p=128), allow_slow_non_contiguous=True),
               k.db("b_mod"), bbmT, bbmT)
        sp.dma(lambda e: e.dma_start(out=gaT[:], in_=g_attn.rearrange("o (k p) -> p (o k)", p=128), allow_slow_non_contiguous=True),
               k.db("g_attn"), bgaT, bgaT)
        sp.dma(lambda e: e.dma_start(out=brow[:], in_=b_mod[:, 2 * D:6 * D]), k.db("b_mod"), bbrow, bbrow)
        act.op(lambda e: e.activation(out=sT[:], in_=cT[:], func=AF.Silu), reads=[bcT], writes=[bsT])
        for kc in range(KD):
            dve.op(lambda e: e.tensor_copy(out=srep[:, kc, :], in_=sT[:, kc, 0:1].to_broadcast([128, 128])),
                   reads=[bsT], writes=[bsrep], partial=True)
        ng = 6 * D // GC
        for g in range(ng):
            wt = wts[g % 2]; bwt = bwts[g % 2]; ps = pss[g % 2]; bps = bpss[g % 2]
            pi_ = (g * GC) // (3 * D); pc0 = g * GC - pi_ * 3 * D
            sp.dma(lambda e: e.dma_start(out=wt[:], in_=w_mod_p[pi_][:, pc0:pc0 + GC].rearrange("(k p) n -> p k n", p=128)),
                   k.db("w_mod_%d" % pi_), bwt, bwt, partial=False)
            if g * GC < 2 * D:
                for j in range(GC // 128):
                    for kc in range(KD):
                        pe.op(lambda e: e.matmul(ps[:, j * 2:j * 2 + 2], lhsT=wt[:, kc, j * 128:(j + 1) * 128], rhs=sT[:, kc, :],
                                                 start=(kc == 0), stop=(kc == KD - 1)),
                              reads=[bwt, bsT], writes=[bps], partial=(kc > 0 or j > 0), signal=(kc == KD - 1 and j == GC // 128 - 1))
                ch0 = g * GC // 128
                dve.op(lambda e: e.tensor_tensor(out=modT[:, ch0:ch0 + GC // 128, :],
                                                 in0=ps[:, 0:2 * (GC // 128)].rearrange("p (j t) -> p j t", t=2),
                                                 in1=bmT[:, ch0:ch0 + GC // 128].unsqueeze(2).to_broadcast([128, GC // 128, 2]), op=ALU.add),
                       reads=[bps, bbmT], writes=[bmodT], partial=True)
            else:
                ev = evs[g % 2]; bev = bevs[g % 2]
                c0 = g * GC - 2 * D
                for kc in range(KD):
                    pe.op(lambda e: e.matmul(ps[:, 0:GC], lhsT=srep[:, kc, :], rhs=wt[:, kc, :], start=(kc == 0), stop=False),
                          reads=[bwt, bsrep], writes=[bps], partial=(kc > 0), signal=False)
                pe.op(lambda e: e.matmul(ps[:, 0:GC], lhsT=ones_f[0:1, :], rhs=brow[0:1, c0:c0 + GC], start=False, stop=True),
                      reads=[b_ones, bbrow], writes=[bps], partial=True)
                dve.op(lambda e: e.tensor_copy(out=ev[:], in_=ps[:, 0:GC]), reads=[bps], writes=[bev])
                sp.dma(lambda e: e.dma_start(out=mod_bc[:, c0:c0 + GC], in_=ev[:]), bev, k.db("mod_bc"), bev)
        dve.op(lambda e: e.tensor_scalar(out=A1T[:], in0=modT[:, KD:2 * KD, :], scalar1=1.0, scalar2=None, op0=ALU.add),
               reads=[bmodT], writes=[bA1T])
        dve.op(lambda e: e.tensor_tensor(out=A1T[:], in0=A1T[:], in1=gaT[:].unsqueeze(2).to_broadcast([128, KD, 2]), op=ALU.mult),
               reads=[bA1T, bgaT], writes=[bA1T])
        dve.op(lambda e: e.tensor_copy(out=B1T[:], in_=modT[:, 0:KD, :]), reads=[bmodT], writes=[bB1T])
        if "A1T" in debug_outs:
            o1 = nc.dram_tensor("dbg_A1T", [128, KD * 2], F32, kind="ExternalOutput").ap(); k.dbufs["dbg_A1T"] = Buf("dbg_A1T")
            o2 = nc.dram_tensor("dbg_B1T", [128, KD * 2], F32, kind="ExternalOutput").ap(); k.dbufs["dbg_B1T"] = Buf("dbg_B1T")
            sp.dma(lambda e: e.dma_start(out=o1, in_=A1T[:].rearrange("p k t -> p (k t)")), bA1T, k.db("dbg_A1T"), bA1T)
            sp.dma(lambda e: e.dma_start(out=o2, in_=B1T[:].rearrange("p k t -> p (k t)")), bB1T, k.db("dbg_B1T"), bB1T)
        sc.barrier()
    if upto <= 1:
        return

    class Ring:
        def __init__(self, name, shape, dt, n, st, psum=False):
            mk = P_ if psum else S_
            self.t = [mk("%s%d" % (name, i), shape, dt, st) for i in range(n)]
            self.b = [Buf("%s%d" % (name, i)) for i in range(n)]
            self.i = -1
        def next(self):
            self.i = (self.i + 1) % len(self.t)
            return self.t[self.i], self.b[self.i]

    ident = S_("ident", [128, 128], BF16, top); b_ident = Buf("ident")
    identf = S_("identf", [128, 128], F32, top); b_identf = Buf("identf")
    pool.op(lambda e: e.memset(identf[:], 0.0), writes=[b_identf])
    pool.op(lambda e: e.affine_select(out=identf[:], in_=ones_f[:], pattern=[[-1, 128]], compare_op=ALU.is_equal, fill=0.0,
                                      base=0, channel_multiplier=1), reads=[b_ones], writes=[b_identf])
    dve.op(lambda e: e.tensor_copy(out=ident[:], in_=identf[:]), reads=[b_identf], writes=[b_ident])
    ones_b = S_("ones_b", [128, 128], BF16, top); b_onesb = Buf("ones_b")
    dve.op(lambda e: e.memset(ones_b[:], 1.0), writes=[b_onesb])

    hT_d = scratch("hT_d", [KD, 128, NK], BF16)
    with ExitStack() as ph:
        xts = Ring("xt", [128, D], F32, 2, ph)
        xns = Ring("xn", [128, D], BF16, 2, ph)
        junk = S_("junk", [128, D], BF16, ph); bjunk = Buf("junk")
        sss = Ring("ss", [128, 2], F32, 2, ph)
        hts = Ring("hts", [128, KD, 128], BF16, 2, ph)
        pst = Ring("pst", [128, 1024], BF16, 2, ph, psum=True)
        import os as _os
        for t in range(int(_os.environ.get('P2T', NK // 128))):
            col = 0 if t * 128 < S else 1
            xt, bxt = xts.next(); xn, bxn = xns.next(); ss, bss = sss.next(); ht, bht = hts.next()
            sp.dma(lambda e: e.dma_start(out=xt[:], in_=xk[t * 128:(t + 1) * 128, :]), k.db("xk"), bxt, bxt, partial=False)
            act.op(lambda e: e.activation(out=junk[:], in_=xt[:], func=AF.Square, accum_out=ss[:, 0:1]), reads=[bxt], writes=[bjunk, bss])
            dve.op(lambda e: e.tensor_scalar(out=ss[:, 1:2], in0=ss[:, 0:1], scalar1=1.0 / D, scalar2=1e-6, op0=ALU.mult, op1=ALU.add),
                   reads=[bss], writes=[bss])
            act.op(lambda e: e.activation(out=ss[:, 1:2], in_=ss[:, 1:2], func=AF.Ln), reads=[bss], writes=[bss]); act.op(lambda e: e.activation(out=ss[:, 1:2], in_=ss[:, 1:2], func=AF.Exp, scale=-0.5), reads=[bss], writes=[bss])
            dve.op(lambda e: e.tensor_scalar(out=xn[:], in0=xt[:], scalar1=ss[:, 1:2], scalar2=None, op0=ALU.mult), reads=[bxt, bss], writes=[bxn])
            _stg = int(_os.environ.get('P2STAGE', 5))
            for g0 in range(0, KD if _stg >= 3 else 0, 8):
                ps, bps = pst.next()
                ng_ = min(8, KD - g0)
                for j in range(ng_):
                    kc = g0 + j
                    pe.op(lambda e: e.transpose(out=ps[:, j * 128:(j + 1) * 128], in_=xn[:, kc * 128:(kc + 1) * 128], identity=ident[:]),
                          reads=[bxn, b_ident], writes=[bps], partial=(j > 0), signal=(j == ng_ - 1))
                for j in range(ng_ if _stg >= 4 else 0):
                    kc = g0 + j
                    if True:
                        dve.op(lambda e: e.tensor_scalar(out=ht[:, kc, :], in0=ps[:, j * 128:(j + 1) * 128], scalar1=A1T[:, kc, col:col + 1],
                                                         scalar2=B1T[:, kc, col:col + 1], op0=ALU.mult, op1=ALU.add),
                               reads=[bps, bA1T, bB1T], writes=[bht], partial=True)
                    else:
                        act.op(lambda e: e.activation(out=ht[:, kc, :], in_=ps[:, j * 128:(j + 1) * 128], func=AF.Identity,
                                                      scale=A1T[:, kc, col:col + 1], bias=B1T[:, kc, col:col + 1]),
                               reads=[bps, bA1T, bB1T], writes=[bht], partial=True)
            if _stg >= 5:
                sp.dma(lambda e: e.dma_start(out=hT_d[:, :, t * 128:(t + 1) * 128].rearrange("k p n -> p k n"), in_=ht[:]),
                       bht, k.db("hT_d"), bht)
        sc.barrier()
    if upto <= 2:
        return

    cosd = inp("cosd", [128, NK]); sind = inp("sind", [128, NK]); cosm = inp("cosm", [64, NK]); sinm = inp("sinm", [64, NK])
    DAH, MH, QL, KVL = c.DAH, c.MH, c.QL, c.KVL
    QT_d = scratch("QT_d", [2 * DAH, 128, NQ], BF16); KT_d = scratch("KT_d", [2 * DAH, 128, NK], BF16)
    V_d = scratch("V_d", [NK, DAH * 256], BF16)
    CQ_d = scratch("CQ_d", [QL // 128, 128, NQ], F32); CKV_d = scratch("CKV_d", [KVL // 128, 128, NK], F32)
    KR_d = scratch("KR_d", [64, NK], BF16)
    GA_d = scratch("GA_d", [KD, 128, NQ], BF16); GB_d = scratch("GB_d", [KD, 128, NQ], BF16)

    def rope_evac(st, ps, bps, M, t0, tw, cos_d, sin_d, cname, sname, dst_ap, dst_name, rings):
        ct, bct = rings["cos"].next(); sn, bsn = rings["sin"].next()
        t1, bt1 = rings["t1"].next(); t2, bt2 = rings["t2"].next(); ob, bob = rings["ob"].next()
        h = M // 2
        sp.dma(lambda e: e.dma_start(out=ct[0:M, 0:tw], in_=cos_d[0:M, t0:t0 + tw]), k.db(cname), bct, bct, partial=False)
        sp.dma(lambda e: e.dma_start(out=sn[0:M, 0:tw], in_=sin_d[0:M, t0:t0 + tw]), k.db(sname), bsn, bsn, partial=False)
        dve.op(lambda e: e.tensor_tensor(out=t1[0:M, 0:tw], in0=ps[0:M, 0:tw], in1=ct[0:M, 0:tw], op=ALU.mult), reads=[bps, bct], writes=[bt1])
        dve.op(lambda e: e.tensor_tensor(out=t2[0:h, 0:tw], in0=ps[h:M, 0:tw], in1=sn[0:h, 0:tw], op=ALU.mult), reads=[bps, bsn], writes=[bt2])
        dve.op(lambda e: e.tensor_tensor(out=t2[h:M, 0:tw], in0=ps[0:h, 0:tw], in1=sn[h:M, 0:tw], op=ALU.mult), reads=[bps, bsn], writes=[bt2], partial=True)
        pool.op(lambda e: e.tensor_tensor(out=ob[0:M, 0:tw], in0=t1[0:M, 0:tw], in1=t2[0:M, 0:tw], op=ALU.add), reads=[bt1, bt2], writes=[bob])
        act.dma(lambda e: e.dma_start(out=dst_ap, in_=ob[0:M, 0:tw]), bob, k.db(dst_name), bob)

    def mk_rope_rings(st):
        return dict(cos=Ring("rcos", [128, 512], F32, 2, st), sin=Ring("rsin", [128, 512], F32, 2, st),
                    t1=Ring("rt1", [128, 512], F32, 2, st), t2=Ring("rt2", [128, 512], F32, 2, st),
                    ob=Ring("rob", [128, 512], BF16, 3, st))

    with ExitStack() as ph:
        wgs = Ring("wg", [128, KD, 512], BF16, 2, ph)
        hxs = Ring("hx", [128, KD, 512], BF16, 2, ph)
        pss = Ring("pj", [128, 512], F32, 6, ph, psum=True)
        rr = mk_rope_rings(ph)
        evf = Ring("evf", [128, 512], F32, 3, ph)
        evb = Ring("evb", [128, 512], BF16, 3, ph)
        segs = [(c.o_dq, c.DAW, NQ, "dq"), (c.o_dk, c.DAW, NK, "dk"), (c.o_dv, c.DAW, NK, "dv"),
                (c.o_cq, QL, NQ, "cq"), (c.o_ckv, KVL + 64, NK, "ckv"), (c.o_ga, D, NQ, "ga"), (c.o_gb, D, NQ, "gb")]
        groups = []
        for (c0, ncol, ntok, kind) in segs:
            for g0 in range(0, ncol, 512):
                groups.append((c0, g0, min(512, ncol - g0), ntok, kind))
        wl = {}
        def loadW(gi):
            if gi < len(groups) and gi not in wl:
                c0, g0, gw, ntok, kind = groups[gi]
                wg, bwg = wgs.next()
                pool.dma(lambda e: e.dma_start(out=wg[:, :, 0:gw], in_=w_in[:, c0 + g0:c0 + g0 + gw].rearrange("(k p) n -> p k n", p=128)),
                         k.db("w_in"), bwg, bwg, partial=False)
                wl[gi] = (wg, bwg)
        items = [(gi, t0) for gi, g in enumerate(groups) for t0 in range(0, g[3], 512)]
        hl = {}
        def loadH(ii):
            if ii < len(items) and ii not in hl:
                gi, t0 = items[ii]
                tw = min(512, groups[gi][3] - t0)
                hx, bhx = hxs.next()
                sp.dma(lambda e: e.dma_start(out=hx[:, :, 0:tw], in_=hT_d[:, :, t0:t0 + tw].rearrange("k p n -> p k n")),
                       k.db("hT_d"), bhx, bhx, partial=False)
                hl[ii] = (hx, bhx)
        loadW(0); loadH(0)
        for ii, (gi, t0) in enumerate(items):
            c0, g0, gw, ntok, kind = groups[gi]
            tw = min(512, ntok - t0)
            if t0 == 0:
                loadW(gi + 1)
            loadH(ii + 1)
            wg, bwg = wl[gi]; hx, bhx = hl[ii]
            if kind == "dv":
                for tt in range(tw // 128):
                    ps, bps = pss.next()
                    for kc in range(KD):
                        pe.op(lambda e: e.matmul(ps[:, 0:gw], lhsT=hx[:, kc, tt * 128:(tt + 1) * 128], rhs=wg[:, kc, 0:gw],
                                                 start=(kc == 0), stop=(kc == KD - 1)),
                              reads=[bhx, bwg], writes=[bps], partial=(kc > 0), signal=(kc == KD - 1))
                    ob, bob = evb.next()
                    act.op(lambda e: e.copy(out=ob[:, 0:gw], in_=ps[:, 0:gw]), reads=[bps], writes=[bob])
                    r0 = t0 + tt * 128
                    act.dma(lambda e: e.dma_start(out=V_d[r0:r0 + 128, g0:g0 + gw], in_=ob[:, 0:gw]), bob, k.db("V_d"), bob)
                continue
            nch = (gw + 127) // 128
            for j in range(nch):
                M = min(128, gw - j * 128)
                ch = (g0 + j * 128) // 128
                ps, bps = pss.next()
                for kc in range(KD):
                    pe.op(lambda e: e.matmul(ps[0:M, 0:tw], lhsT=wg[:, kc, j * 128:j * 128 + M], rhs=hx[:, kc, 0:tw],
                                             start=(kc == 0), stop=(kc == KD - 1)),
                          reads=[bhx, bwg], writes=[bps], partial=(kc > 0), signal=(kc == KD - 1))
                if kind == "dq":
                    rope_evac(ph, ps, bps, 128, t0, tw, cosd, sind, "cosd", "sind", QT_d[ch, :, t0:t0 + tw], "QT_d", rr)
                elif kind == "dk":
                    rope_evac(ph, ps, bps, 128, t0, tw, cosd, sind, "cosd", "sind", KT_d[ch, :, t0:t0 + tw], "KT_d", rr)
                elif kind == "ckv" and M == 64:
                    rope_evac(ph, ps, bps, 64, t0, tw, cosm, sinm, "cosm", "sinm", KR_d[:, t0:t0 + tw], "KR_d", rr)
                elif kind in ("cq", "ckv"):
                    ob, bob = evf.next()
                    dve.op(lambda e: e.tensor_copy(out=ob[:, 0:tw], in_=ps[:, 0:tw]), reads=[bps], writes=[bob])
                    dst, dn = (CQ_d, "CQ_d") if kind == "cq" else (CKV_d, "CKV_d")
                    act.dma(lambda e: e.dma_start(out=dst[ch, :, t0:t0 + tw], in_=ob[:, 0:tw]), bob, k.db(dn), bob)
                else:
                    ob, bob = evb.next()
                    act.op(lambda e: e.activation(out=ob[:, 0:tw], in_=ps[:, 0:tw], func=AF.Sigmoid), reads=[bps], writes=[bob])
                    dst, dn = (GA_d, "GA_d") if kind == "ga" else (GB_d, "GB_d")
                    act.dma(lambda e: e.dma_start(out=dst[ch, :, t0:t0 + tw], in_=ob[:, 0:tw]), bob, k.db(dn), bob)
        sc.barrier()
    if upto <= 3:
        return

    g_q = inp("g_q", [1, QL]); g_kv = inp("g_kv", [1, KVL])
    QN_d = scratch("QN_d", [MH, 128, NQ], BF16); QR_d = scratch("QR_d", [MH, 64, NQ], BF16)
    KN_d = scratch("KN_d", [MH, 128, NK], BF16); MV_d = scratch("MV_d", [NK, MH * 128], BF16)

    def latent_up(lat_d, lat_name, nch, ntok, g_ap, g_name, W, Wname, wcols, emit):
        with ExitStack() as ph:
            wsb = S_("lw", [128, nch, wcols], BF16, ph); bw = Buf("lw")
            pool.dma(lambda e: e.dma_start(out=wsb[:], in_=W.rearrange("(k p) n -> p k n", p=128)), k.db(Wname), bw, bw, partial=False)
            gT = S_("lg", [128, nch], F32, ph); bg = Buf("lg")
            sp.dma(lambda e: e.dma_start(out=gT[:], in_=g_ap.rearrange("o (k p) -> p (o k)", p=128), allow_slow_non_contiguous=True),
                   k.db(g_name), bg, bg)
            Ls = Ring("lL", [128, nch, 512], F32, 2, ph)
            sq = S_("lsq", [128, nch, 512], F32, ph); bsq = Buf("lsq")
            Lns = Ring("lLn", [128, nch, 512], BF16, 2, ph)
            rs = Ring("lrs", [128, 512], F32, 2, ph)
            pss = Ring("lps", [128, 512], F32, 5, ph, psum=True)
            rr = mk_rope_rings(ph)
            evb = Ring("levb", [128, 512], BF16, 3, ph)
            for t0 in range(0, ntok, 512):
                tw = min(512, ntok - t0)
                L, bL = Ls.next(); Ln, bLn = Lns.next(); r_, br = rs.next()
                sp.dma(lambda e: e.dma_start(out=L[:, :, 0:tw], in_=lat_d[:, :, t0:t0 + tw].rearrange("k p n -> p k n")),
                       k.db(lat_name), bL, bL, partial=False)
                act.op(lambda e: e.activation(out=sq[:, :, 0:tw], in_=L[:, :, 0:tw], func=AF.Square), reads=[bL], writes=[bsq])
                ps, bps = pss.next()
                for kc in range(nch):
                    pe.op(lambda e: e.matmul(ps[:, 0:tw], lhsT=ones_f[:], rhs=sq[:, kc, 0:tw], start=(kc == 0), stop=(kc == nch - 1)),
                          reads=[b_ones, bsq], writes=[bps], partial=(kc > 0), signal=(kc == nch - 1))
                dve.op(lambda e: e.tensor_scalar(out=r_[:, 0:tw], in0=ps[:, 0:tw], scalar1=1.0 / (nch * 128), scalar2=1e-6, op0=ALU.mult, op1=ALU.add),
                       reads=[bps], writes=[br])
                act.op(lambda e: e.activation(out=r_[:, 0:tw], in_=r_[:, 0:tw], func=AF.Ln), reads=[br], writes=[br]); act.op(lambda e: e.activation(out=r_[:, 0:tw], in_=r_[:, 0:tw], func=AF.Exp, scale=-0.5), reads=[br], writes=[br])
                for kc in range(nch):
                    dve.op(lambda e: e.scalar_tensor_tensor(out=Ln[:, kc, 0:tw], in0=L[:, kc, 0:tw], scalar=gT[:, kc:kc + 1], in1=r_[:, 0:tw],
                                                            op0=ALU.mult, op1=ALU.mult),
                           reads=[bL, bg, br], writes=[bLn], partial=(kc > 0))
                emit(Ln, bLn, wsb, bw, t0, tw, nch, pss, rr, evb)
            sc.barrier()

    def fm_head(Ln, bLn, wsb, bw, nch, col0, M, tw, pss):
        ps, bps = pss.next()
        for kc in range(nch):
            pe.op(lambda e: e.matmul(ps[0:M, 0:tw], lhsT=wsb[:, kc, col0:col0 + M], rhs=Ln[:, kc, 0:tw], start=(kc == 0), stop=(kc == nch - 1)),
                  reads=[bw, bLn], writes=[bps], partial=(kc > 0), signal=(kc == nch - 1))
        return ps, bps

    def emit_q(Ln, bLn, wsb, bw, t0, tw, nch, pss, rr, evb):
        for h in range(MH):
            ps, bps = fm_head(Ln, bLn, wsb, bw, nch, h * 192, 128, tw, pss)
            ob, bob = evb.next()
            act.op(lambda e: e.copy(out=ob[:, 0:tw], in_=ps[:, 0:tw]), reads=[bps], writes=[bob])
            act.dma(lambda e: e.dma_start(out=QN_d[h, :, t0:t0 + tw], in_=ob[:, 0:tw]), bob, k.db("QN_d"), bob)
            ps, bps = fm_head(Ln, bLn, wsb, bw, nch, h * 192 + 128, 64, tw, pss)
            rope_evac(None, ps, bps, 64, t0, tw, cosm, sinm, "cosm", "sinm", QR_d[h, :, t0:t0 + tw], "QR_d", rr)

    def emit_kv(Ln, bLn, wsb, bw, t0, tw, nch, pss, rr, evb):
        for h in range(MH):
            ps, bps = fm_head(Ln, bLn, wsb, bw, nch, h * 256, 128, tw, pss)
            ob, bob = evb.next()
            act.op(lambda e: e.copy(out=ob[:, 0:tw], in_=ps[:, 0:tw]), reads=[bps], writes=[bob])
            act.dma(lambda e: e.dma_start(out=KN_d[h, :, t0:t0 + tw], in_=ob[:, 0:tw]), bob, k.db("KN_d"), bob)
        for tt in range(tw // 128):
            for h0 in range(0, MH, 4):
                nh = min(4, MH - h0)
                ps, bps = pss.next()
                for kc in range(nch):
                    pe.op(lambda e: e.matmul(ps[:, 0:nh * 128].rearrange("p (h e) -> p h e", e=128), lhsT=Ln[:, kc, tt * 128:(tt + 1) * 128],
                                             rhs=wsb[:, kc, :].rearrange("p (h e) -> p h e", e=256)[:, h0:h0 + nh, 128:256],
                                             start=(kc == 0), stop=(kc == nch - 1)),
                          reads=[bw, bLn], writes=[bps], partial=(kc > 0), signal=(kc == nch - 1))
                ob, bob = evb.next()
                dve.op(lambda e: e.tensor_copy(out=ob[:, 0:nh * 128], in_=ps[:, 0:nh * 128]), reads=[bps], writes=[bob])
                r0 = t0 + tt * 128
                act.dma(lambda e: e.dma_start(out=MV_d[r0:r0 + 128, h0 * 128:(h0 + nh) * 128], in_=ob[:, 0:nh * 128]), bob, k.db("MV_d"), bob)

    latent_up(CQ_d, "CQ_d", QL // 128, NQ, g_q, "g_q", w_uq, "w_uq", MH * 192, emit_q)
    latent_up(CKV_d, "CKV_d", KVL // 128, NK, g_kv, "g_kv", w_ukv, "w_ukv", MH * 256, emit_kv)
    if upto <= 4:
        return

    OA_d = scratch("OA_d", [c.DAW // 128, 128, NQ], BF16); OB_d = scratch("OB_d", [c.MW // 128, 128, NQ], BF16)
    g_subln = inp("g_subln", [1, 256])
    lam_in = [inp(n, [1, 128]) for n in ("lam_q1", "lam_k1", "lam_q2", "lam_k2")]
    NKC = NK // 128

    def attn_branch(H, ncomp, E, scale, k_parts, q_parts, v_src, finalize, extra=None):
        with ExitStack() as ph:
            np_ = len(k_parts(0, 0))
            kts = Ring("akt", [128, ncomp * np_, NK], BF16, 2, ph)
            vts = Ring("avt", [128, NKC, E + 1], BF16, 2, ph)
            for i in range(2):
                pool.op(lambda e: e.memset(vts.t[i][:, :, E:E + 1], 1.0), writes=[vts.b[i]])
            qts = Ring("aqt", [128, ncomp * np_, 512], BF16, 2, ph)
            pts = Ring("apt", [128, 1024], BF16, 3, ph)
            ops_ = Ring("aop", [128, 512], F32, 4, ph, psum=True)
            sps = Ring("asp", [128, 1024], F32, 2, ph, psum=True)
            ctx_ = extra(ph, sps) if extra else None
            for h in range(H):
                kt, bkt = kts.next(); vt, bvt = vts.next()
                for comp in range(ncomp):
                    for pi, (ap_, nm, Kr) in enumerate(k_parts(h, comp)):
                        sp.dma(lambda e: e.dma_start(out=kt[0:Kr, comp * np_ + pi, :], in_=ap_), k.db(nm), bkt, bkt, partial=(comp + pi > 0))
                vap, vnm = v_src(h)
                sp.dma(lambda e: e.dma_start(out=vt[:, :, 0:E], in_=vap.rearrange("(k p) e -> p k e", p=128)), k.db(vnm), bvt, bvt)
                for q0 in range(0, NQ, 512):
                    tw = min(512, NQ - q0); nqs = tw // 128
                    qt, bqt = qts.next()
                    for comp in range(ncomp):
                        for pi, (ap_, nm, Kr) in enumerate(q_parts(h, comp)):
                            sp.dma(lambda e: e.dma_start(out=qt[0:Kr, comp * np_ + pi, 0:tw], in_=ap_[:, q0:q0 + tw]), k.db(nm), bqt, bqt,
                                   partial=(comp + pi > 0))
                    for comp in range(ncomp):
                        Os = [ops_.next() for _ in range(nqs)]
                        parts = k_parts(h, comp)
                        for kc0 in range(0, NKC, 2):
                            n2 = min(2, NKC - kc0)
                            sp_, bsp = sps.next()
                            for j in range(n2):
                                for pi, (_, _, Kr) in enumerate(parts):
                                    pe.op(lambda e: e.matmul(sp_[:, j * 512:j * 512 + tw], lhsT=kt[0:Kr, comp * np_ + pi, (kc0 + j) * 128:(kc0 + j + 1) * 128],
                                                             rhs=qt[0:Kr, comp * np_ + pi, 0:tw], start=(pi == 0), stop=(pi == np_ - 1)),
                                          reads=[bkt, bqt], writes=[bsp], partial=(j + pi > 0), signal=(j == n2 - 1 and pi == np_ - 1))
                            pt, bpt = pts.next()
                            if tw == 512:
                                act.op(lambda e: e.activation(out=pt[:, 0:n2 * 512], in_=sp_[:, 0:n2 * 512], func=AF.Exp, scale=scale), reads=[bsp], writes=[bpt])
                            else:
                                for j in range(n2):
                                    act.op(lambda e: e.activation(out=pt[:, j * 512:j * 512 + tw], in_=sp_[:, j * 512:j * 512 + tw], func=AF.Exp, scale=scale),
                                           reads=[bsp], writes=[bpt], partial=(j > 0))
                            for j in range(n2):
                                kc = kc0 + j
                                for qs in range(nqs):
                                    O, bO = Os[qs]
                                    pe.op(lambda e: e.matmul(O[:, 0:E + 1], lhsT=pt[:, j * 512 + qs * 128:j * 512 + (qs + 1) * 128], rhs=vt[:, kc, 0:E + 1],
                                                             start=(kc == 0), stop=(kc == NKC - 1)),
                                          reads=[bpt, bvt], writes=[bO], partial=(kc > 0), signal=(kc == NKC - 1 or (j == n2 - 1 and qs == nqs - 1)))
                        finalize(ctx_, h, comp, q0, tw, Os, sps)
            sc.barrier()

    def store_T(ctx_, src_bf, bsrc, nE, dst_d, dst_name, ch0, q0, tw, sps):
        nqs = tw // 128
        for ec in range(nE):
            sp_, bsp = sps.next()
            pb = sp_[:].bitcast(BF16)
            for qs in range(nqs):
                pe.op(lambda e: e.transpose(out=pb[:, qs * 128:(qs + 1) * 128], in_=src_bf[:, qs, ec * 128:(ec + 1) * 128], identity=ident[:]),
                      reads=[bsrc, b_ident], writes=[bsp], partial=(qs > 0), signal=(qs == nqs - 1))
            ob, bob = ctx_["oT"].next()
            dve.op(lambda e: e.tensor_copy(out=ob[:, 0:tw], in_=pb[:, 0:tw]), reads=[bsp], writes=[bob])
            act.dma(lambda e: e.dma_start(out=dst_d[ch0 + ec, :, q0:q0 + tw], in_=ob[:, 0:tw]), bob, k.db(dst_name), bob)

    def da_extra(ph, sps):
        d = {}
        d["o0"] = S_("da_o0", [128, 4, 256], F32, ph); d["bo0"] = Buf("da_o0")
        d["o1"] = S_("da_o1", [128, 4, 256], F32, ph); d["bo1"] = Buf("da_o1")
        d["ob"] = S_("da_ob", [128, 4, 256], BF16, ph); d["bob"] = Buf("da_ob")
        d["jk"] = S_("da_jk", [128, 256], F32, ph); d["bjk"] = Buf("da_jk")
        d["rs"] = S_("da_rs", [128, 8], F32, ph); d["brs"] = Buf("da_rs")
        d["oT"] = Ring("da_oT", [128, 512], BF16, 3, ph)
        lv = S_("da_lv", [128, 4], F32, ph); blv = Buf("da_lv")
        for i in range(4):
            sp.dma(lambda e: e.dma_start(out=lv[:, i:i + 1], in_=lam_in[i].rearrange("o p -> p o"), allow_slow_non_contiguous=True),
                   k.db(("lam_q1", "lam_k1", "lam_q2", "lam_k2")[i]), blv, blv)
        lp = S_("da_lp", [128, 2], F32, ph); blp = Buf("da_lp")
        dve.op(lambda e: e.tensor_tensor(out=lp[:, 0:1], in0=lv[:, 0:1], in1=lv[:, 1:2], op=ALU.mult), reads=[blv], writes=[blp])
        dve.op(lambda e: e.tensor_tensor(out=lp[:, 1:2], in0=lv[:, 2:3], in1=lv[:, 3:4], op=ALU.mult), reads=[blv], writes=[blp], partial=True)
        with ExitStack() as tmp:
            ps, bps = sps.next()
            pe.op(lambda e: e.matmul(ps[:, 0:2], lhsT=ones_f[:], rhs=lp[:], start=True, stop=True), reads=[b_ones, blp], writes=[bps])
            le = S_("da_le", [128, 2], F32, ph); ble = Buf("da_le")
            act.op(lambda e: e.activation(out=le[:], in_=ps[:, 0:2], func=AF.Exp), reads=[bps], writes=[ble])
            nl = S_("da_nl", [128, 1], F32, ph); bnl = Buf("da_nl")
            dve.op(lambda e: e.tensor_tensor(out=nl[:], in0=le[:, 1:2], in1=le[:, 0:1], op=ALU.subtract), reads=[ble], writes=[bnl])
            dve.op(lambda e: e.tensor_scalar(out=nl[:], in0=nl[:], scalar1=-c.lambda_init, scalar2=None, op0=ALU.add), reads=[bnl], writes=[bnl])
            d["nl"] = nl; d["bnl"] = bnl
            gr = S_("da_gr", [1, 256], F32, ph); bgr = Buf("da_gr")
            sp.dma(lambda e: e.dma_start(out=gr[:], in_=g_subln), k.db("g_subln"), bgr, bgr)
            pe.op(lambda e: e.matmul(ps[:, 256:512], lhsT=ones_f[0:1, :], rhs=gr[0:1, :], start=True, stop=True), reads=[b_ones, bgr], writes=[bps])
            gb_ = S_("da_gb", [128, 256], F32, ph); bgb = Buf("da_gb")
            dve.op(lambda e: e.tensor_scalar(out=gb_[:], in0=ps[:, 256:512], scalar1=1.0 - c.lambda_init, scalar2=None, op0=ALU.mult),
                   reads=[bps], writes=[bgb])
            d["gb"] = gb_; d["bgb"] = bgb
            sc.barrier([pe, dve, act])
        return d

    def da_final(d, h, comp, q0, tw, Os, sps):
        nqs = tw // 128
        rs, brs = d["rs"], d["brs"]
        dst, bdst = (d["o0"], d["bo0"]) if comp == 0 else (d["o1"], d["bo1"])
        for qs in range(nqs):
            O, bO = Os[qs]
            dve.op(lambda e: e.reciprocal(out=rs[:, qs:qs + 1], in_=O[:, 256:257]), reads=[bO], writes=[brs], partial=(qs > 0))
            dve.op(lambda e: e.tensor_scalar(out=dst[:, qs, :], in0=O[:, 0:256], scalar1=rs[:, qs:qs + 1], scalar2=None, op0=ALU.mult),
                   reads=[bO, brs], writes=[bdst], partial=(qs > 0))
        if comp == 0:
            return
        o0, bo0, o1, bo1 = d["o0"], d["bo0"], d["o1"], d["bo1"]
        for qs in range(nqs):
            dve.op(lambda e: e.scalar_tensor_tensor(out=o0[:, qs, :], in0=o1[:, qs, :], scalar=d["nl"][:, 0:1], in1=o0[:, qs, :], op0=ALU.mult, op1=ALU.add),
                   reads=[bo1, bo0, d["bnl"]], writes=[bo0])
            act.op(lambda e: e.activation(out=d["jk"][:], in_=o0[:, qs, :], func=AF.Square, accum_out=rs[:, 4 + qs:5 + qs]),
                   reads=[bo0], writes=[d["bjk"], brs])
            dve.op(lambda e: e.tensor_scalar(out=rs[:, 4 + qs:5 + qs], in0=rs[:, 4 + qs:5 + qs], scalar1=1.0 / 256, scalar2=1e-6, op0=ALU.mult, op1=ALU.add),
                   reads=[brs], writes=[brs])
            act.op(lambda e: e.activation(out=rs[:, 4 + qs:5 + qs], in_=rs[:, 4 + qs:5 + qs], func=AF.Ln), reads=[brs], writes=[brs]); act.op(lambda e: e.activation(out=rs[:, 4 + qs:5 + qs], in_=rs[:, 4 + qs:5 + qs], func=AF.Exp, scale=-0.5), reads=[brs], writes=[brs])
            dve.op(lambda e: e.scalar_tensor_tensor(out=d["ob"][:, qs, :], in0=o0[:, qs, :], scalar=rs[:, 4 + qs:5 + qs], in1=d["gb"][:], op0=ALU.mult, op1=ALU.mult),
                   reads=[bo0, brs, d["bgb"]], writes=[d["bob"]], partial=(qs > 0))
        store_T(d, d["ob"], d["bob"], 2, OA_d, "OA_d", h * 2, q0, tw, sps)

    attn_branch(DAH, 2, 256, 128 ** -0.5,
                lambda h, comp: [(KT_d[comp * DAH + h], "KT_d", 128)],
                lambda h, comp: [(QT_d[comp * DAH + h], "QT_d", 128)],
                lambda h: (V_d[:, h * 256:(h + 1) * 256], "V_d"), da_final, da_extra)
    if upto <= 5:
        return

    def mla_extra(ph, sps):
        d = {}
        d["ob"] = S_("ml_ob", [128, 4, 128], BF16, ph); d["bob"] = Buf("ml_ob")
        d["rs"] = S_("ml_rs", [128, 4], F32, ph); d["brs"] = Buf("ml_rs")
        d["oT"] = Ring("ml_oT", [128, 512], BF16, 3, ph)
        return d

    def mla_final(d, h, comp, q0, tw, Os, sps):
        nqs = tw // 128
        rs, brs = d["rs"], d["brs"]
        for qs in range(nqs):
            O, bO = Os[qs]
            dve.op(lambda e: e.reciprocal(out=rs[:, qs:qs + 1], in_=O[:, 128:129]), reads=[bO], writes=[brs], partial=(qs > 0))
            dve.op(lambda e: e.tensor_scalar(out=d["ob"][:, qs, :], in0=O[:, 0:128], scalar1=rs[:, qs:qs + 1], scalar2=None, op0=ALU.mult),
                   reads=[bO, brs], writes=[d["bob"]], partial=(qs > 0))
        store_T(d, d["ob"], d["bob"], 1, OB_d, "OB_d", h, q0, tw, sps)

    attn_branch(MH, 1, 128, 192 ** -0.5,
                lambda h, comp: [(KN_d[h], "KN_d", 128), (KR_d, "KR_d", 64)],
                lambda h, comp: [(QN_d[h], "QN_d", 128), (QR_d[h], "QR_d", 64)],
                lambda h: (MV_d[:, h * 128:(h + 1) * 128], "MV_d"), mla_final, mla_extra)
    if upto <= 6:
        return

    NR = c.NG + c.NE; NE = c.NE; NG = c.NG; EPG = c.EPG; DE = c.DE; HC = DE // 128
    NT = NQ // 128; NBLK = 2 * NT + NE
    g_ffn = inp("g_ffn", [1, D]); w_r = inp("w_r", [D, NR]); b_r = inp("b_r", [1, NR]); g_final = inp("g_final", [1, D])
    MG_d = scratch("MG_d", [KD, 128, NQ], BF16); X1_d = scratch("X1_d", [NQ, D], F32); H2_d = scratch("H2_d", [NQ, D], BF16)
    Xs_d = scratch("Xs_d", [NBLK * 128, D], BF16); Ys_d = scratch("Ys_d", [NBLK * 128, D], F32)
    KA = c.DAW // 128; KB = c.MW // 128

    with ExitStack() as ph:
        was = Ring("wa", [128, KA, 512], BF16, 2, ph); wbs = Ring("wb", [128, KB, 512], BF16, 2, ph)
        oas = Ring("oa", [128, KA, 512], BF16, 2, ph); obs = Ring("ob", [128, KB, 512], BF16, 2, ph)
        gas = Ring("gat", [128, 512], BF16, 2, ph); gbs = Ring("gbt", [128, 512], BF16, 2, ph)
        t1s = Ring("mt1", [128, 512], F32, 2, ph); t2s = Ring("mt2", [128, 512], F32, 2, ph); mgs = Ring("mgo", [128, 512], BF16, 3, ph)
        pss = Ring("mps", [128, 512], F32, 6, ph, psum=True)
        for g0 in range(0, D, 512):
            gw = min(512, D - g0)
            wa, bwa = was.next(); wb, bwb = wbs.next()
            pool.dma(lambda e: e.dma_start(out=wa[:, :, 0:gw], in_=w_br_a[:, g0:g0 + gw].rearrange("(k p) n -> p k n", p=128)), k.db("w_br_a"), bwa, bwa, partial=False)
            pool.dma(lambda e: e.dma_start(out=wb[:, :, 0:gw], in_=w_br_b[:, g0:g0 + gw].rearrange("(k p) n -> p k n", p=128)), k.db("w_br_b"), bwb, bwb, partial=False)
            for t0 in range(0, NQ, 512):
                tw = min(512, NQ - t0)
                oa, boa = oas.next(); ob, bob = obs.next()
                sp.dma(lambda e: e.dma_start(out=oa[:, :, 0:tw], in_=OA_d[:, :, t0:t0 + tw].rearrange("k p n -> p k n")), k.db("OA_d"), boa, boa, partial=False)
                sp.dma(lambda e: e.dma_start(out=ob[:, :, 0:tw], in_=OB_d[:, :, t0:t0 + tw].rearrange("k p n -> p k n")), k.db("OB_d"), bob, bob, partial=False)
                for j in range(gw // 128):
                    dc = (g0 + j * 128) // 128
                    psa, bpa = pss.next(); psb, bpb = pss.next()
                    for kc in range(KA):
                        pe.op(lambda e: e.matmul(psa[:, 0:tw], lhsT=wa[:, kc, j * 128:(j + 1) * 128], rhs=oa[:, kc, 0:tw], start=(kc == 0), stop=(kc == KA - 1)),
                              reads=[bwa, boa], writes=[bpa], partial=(kc > 0), signal=(kc == KA - 1))
                    for kc in range(KB):
                        pe.op(lambda e: e.matmul(psb[:, 0:tw], lhsT=wb[:, kc, j * 128:(j + 1) * 128], rhs=ob[:, kc, 0:tw], start=(kc == 0), stop=(kc == KB - 1)),
                              reads=[bwb, bob], writes=[bpb], partial=(kc > 0), signal=(kc == KB - 1))
                    ga, bga = gas.next(); gb, bgb = gbs.next()
                    sp.dma(lambda e: e.dma_start(out=ga[:, 0:tw], in_=GA_d[dc, :, t0:t0 + tw]), k.db("GA_d"), bga, bga, partial=False)
                    sp.dma(lambda e: e.dma_start(out=gb[:, 0:tw], in_=GB_d[dc, :, t0:t0 + tw]), k.db("GB_d"), bgb, bgb, partial=False)
                    t1, bt1 = t1s.next(); t2, bt2 = t2s.next(); mg, bmg = mgs.next()
                    dve.op(lambda e: e.tensor_tensor(out=t1[:, 0:tw], in0=psa[:, 0:tw], in1=ga[:, 0:tw], op=ALU.mult), reads=[bpa, bga], writes=[bt1])
                    dve.op(lambda e: e.tensor_tensor(out=t2[:, 0:tw], in0=psb[:, 0:tw], in1=gb[:, 0:tw], op=ALU.mult), reads=[bpb, bgb], writes=[bt2])
                    pool.op(lambda e: e.tensor_tensor(out=mg[:, 0:tw], in0=t1[:, 0:tw], in1=t2[:, 0:tw], op=ALU.add), reads=[bt1, bt2], writes=[bmg])
                    act.dma(lambda e: e.dma_start(out=MG_d[dc, :, t0:t0 + tw], in_=mg[:, 0:tw]), bmg, k.db("MG_d"), bmg)
        sc.barrier()
    if upto <= 7:
        return

    with ExitStack() as ph:
        wos = Ring("wo", [128, KD, 512], BF16, 2, ph); mgt = Ring("mgt", [128, KD, 128], BF16, 3, ph)
        gts = Ring("gt1", [128, 512], F32, 2, ph); xs = Ring("xr", [128, 512], F32, 3, ph)
        t1s = Ring("ot1", [128, 512], F32, 2, ph); x1s = Ring("x1o", [128, 512], F32, 3, ph)
        pss = Ring("ops", [128, 512], F32, 4, ph, psum=True)
        for g0 in range(0, D, 512):
            gw = min(512, D - g0)
            wo, bwo = wos.next(); gt, bgt = gts.next()
            pool.dma(lambda e: e.dma_start(out=wo[:, :, 0:gw], in_=w_out[:, g0:g0 + gw].rearrange("(k p) n -> p k n", p=128)), k.db("w_out"), bwo, bwo, partial=False)
            sp.dma(lambda e: e.dma_start(out=gt[:, 0:gw], in_=mod_bc[:, g0:g0 + gw]), k.db("mod_bc"), bgt, bgt, partial=False)
            for tt in range(NT):
                mg, bmg = mgt.next(); xr, bxr = xs.next()
                sp.dma(lambda e: e.dma_start(out=mg[:], in_=MG_d[:, :, tt * 128:(tt + 1) * 128].rearrange("k p n -> p k n")), k.db("MG_d"), bmg, bmg, partial=False)
                sp.dma(lambda e: e.dma_start(out=xr[:, 0:gw], in_=xk[tt * 128:(tt + 1) * 128, g0:g0 + gw]), k.db("xk"), bxr, bxr, partial=False)
                ps, bps = pss.next()
                for kc in range(KD):
                    pe.op(lambda e: e.matmul(ps[:, 0:gw], lhsT=mg[:, kc, :], rhs=wo[:, kc, 0:gw], start=(kc == 0), stop=(kc == KD - 1)),
                          reads=[bmg, bwo], writes=[bps], partial=(kc > 0), signal=(kc == KD - 1))
                t1, bt1 = t1s.next(); x1, bx1 = x1s.next()
                dve.op(lambda e: e.tensor_tensor(out=t1[:, 0:gw], in0=ps[:, 0:gw], in1=gt[:, 0:gw], op=ALU.mult), reads=[bps, bgt], writes=[bt1])
                pool.op(lambda e: e.tensor_tensor(out=x1[:, 0:gw], in0=t1[:, 0:gw], in1=xr[:, 0:gw], op=ALU.add), reads=[bt1, bxr], writes=[bx1])
                act.dma(lambda e: e.dma_start(out=X1_d[tt * 128:(tt + 1) * 128, g0:g0 + gw], in_=x1[:, 0:gw]), bx1, k.db("X1_d"), bx1)
        sc.barrier()
    if upto <= 8:
        return

    moe = ExitStack(); top.enter_context(moe)
    Ind_all = S_("Ind_all", [128, NT, NE], BF16, moe); bInd = Buf("Ind_all")
    I1_all = S_("I1_all", [128, NT, NE], F32, moe); bI1 = Buf("I1_all")
    I2_all = S_("I2_all", [128, NT, NE], F32, moe); bI2 = Buf("I2_all")
    wsel = S_("wsel", [128, NT, 2], F32, moe); bwsel = Buf("wsel")
    idx_all = S_("idx_all", [128, NT, 2], I32, moe); bidx = Buf("idx_all")
    NSP_ = max(1, (KD * DE * 4 + 32767) // 32768)
    widx_i = S_("widx_i", [128, 4 * NSP_, NBLK], I32, moe); bwidx = Buf("widx_i")
    Uf = S_("Uf", [128, 128], F32, moe); bUf = Buf("Uf")
    Ub = S_("Ub", [128, 128], BF16, moe); bUb = Buf("Ub")
    jji = S_("jji", [128, 128], I32, moe); bjji = Buf("jji")
    pool.op(lambda e: e.iota(out=jji[:], pattern=[[1, 128]], base=0, channel_multiplier=0), writes=[bjji])
    ppi = S_("ppi", [128, 1], I32, moe); bppi = Buf("ppi")
    pool.op(lambda e: e.iota(out=ppi[:], pattern=[[0, 1]], base=0, channel_multiplier=1), writes=[bppi])
    ppf = S_("ppf", [128, 1], F32, moe); bppf = Buf("ppf")
    dve.op(lambda e: e.tensor_copy(out=ppf[:], in_=ppi[:]), reads=[bppi], writes=[bppf])
    dve.op(lambda e: e.tensor_copy(out=Uf[:], in_=jji[:]), reads=[bjji], writes=[bUf])
    dve.op(lambda e: e.tensor_scalar(out=Uf[:], in0=Uf[:], scalar1=ppf[:, 0:1], scalar2=None, op0=ALU.is_gt), reads=[bUf, bppf], writes=[bUf])
    dve.op(lambda e: e.tensor_copy(out=Ub[:], in_=Uf[:]), reads=[bUf], writes=[bUb])

    def bcast_row(row_ap, row_name, dst, bdst, st, pss, post=None):
        rw = S_("bc_row", [1, D], F32, st); brw = Buf("bc_row")
        sp.dma(lambda e: e.dma_start(out=rw[:], in_=row_ap), k.db(row_name), brw, brw)
        for g0 in range(0, D, 512):
            gw = min(512, D - g0)
            ps, bps = pss.next()
            pe.op(lambda e: e.matmul(ps[:, 0:gw], lhsT=ones_f[0:1, :], rhs=rw[0:1, g0:g0 + gw], start=True, stop=True), reads=[b_ones, brw], writes=[bps])
            if post is None:
                dve.op(lambda e: e.tensor_copy(out=dst[:, g0:g0 + gw], in_=ps[:, 0:gw]), reads=[bps], writes=[bdst], partial=True)
            else:
                post(ps, bps, g0, gw)

    with ExitStack() as ph:
        A2 = S_("A2", [128, D], F32, ph); bA2 = Buf("A2"); B2 = S_("B2", [128, D], F32, ph); bB2 = Buf("B2")
        pss = Ring("rps", [128, 512], F32, 4, ph, psum=True)
        sp.dma(lambda e: e.dma_start(out=A2[:], in_=mod_bc[:, 2 * D:3 * D]), k.db("mod_bc"), bA2, bA2)
        sp.dma(lambda e: e.dma_start(out=B2[:], in_=mod_bc[:, D:2 * D]), k.db("mod_bc"), bB2, bB2)
        bcast_row(g_ffn, "g_ffn", A2, bA2, ph, pss,
                  post=lambda ps, bps, g0, gw: dve.op(lambda e: e.scalar_tensor_tensor(out=A2[:, g0:g0 + gw], in0=A2[:, g0:g0 + gw], scalar=1.0, in1=ps[:, 0:gw],
                                                                                       op0=ALU.add, op1=ALU.mult), reads=[bps, bA2], writes=[bA2]))
        wr = S_("wr", [128, KD, NR], F32, ph); bwr = Buf("wr")
        sp.dma(lambda e: e.dma_start(out=wr[:], in_=w_r.rearrange("(k p) n -> p k n", p=128)), k.db("w_r"), bwr, bwr)
        brr = S_("brr", [1, NR], F32, ph); bbrr = Buf("brr")
        sp.dma(lambda e: e.dma_start(out=brr[:], in_=b_r), k.db("b_r"), bbrr, bbrr)
        x1s = Ring("cx1", [128, D], F32, 2, ph)
        h2 = S_("h2", [128, D], F32, ph); bh2 = Buf("h2")
        h2bs = Ring("h2b", [128, D], BF16, 2, ph)
        h2T = S_("h2T", [128, KD, 128], F32, ph); bh2T = Buf("h2T")
        sm = S_("rsm", [128, 16], F32, ph); bsm = Buf("rsm")
        lg = S_("lg", [128, NR], F32, ph); blg = Buf("lg")
        rt = S_("rt", [128, 8, NE], F32, ph); brt = Buf("rt")
        jk = S_("cjk", [128, D], BF16, ph); bjk = Buf("cjk")
        for tt in range(NT):
            x1, bx1 = x1s.next(); h2b, bh2b = h2bs.next()
            sp.dma(lambda e: e.dma_start(out=x1[:], in_=X1_d[tt * 128:(tt + 1) * 128, :]), k.db("X1_d"), bx1, bx1, partial=False)
            act.op(lambda e: e.activation(out=jk[:], in_=x1[:], func=AF.Square, accum_out=sm[:, 0:1]), reads=[bx1], writes=[bjk, bsm])
            dve.op(lambda e: e.tensor_scalar(out=sm[:, 1:2], in0=sm[:, 0:1], scalar1=1.0 / D, scalar2=1e-6, op0=ALU.mult, op1=ALU.add), reads=[bsm], writes=[bsm])
            act.op(lambda e: e.activation(out=sm[:, 1:2], in_=sm[:, 1:2], func=AF.Ln), reads=[bsm], writes=[bsm])
            act.op(lambda e: e.activation(out=sm[:, 1:2], in_=sm[:, 1:2], func=AF.Exp, scale=-0.5), reads=[bsm], writes=[bsm])
            dve.op(lambda e: e.scalar_tensor_tensor(out=h2[:], in0=x1[:], scalar=sm[:, 1:2], in1=A2[:], op0=ALU.mult, op1=ALU.mult),
                   reads=[bx1, bsm, bA2], writes=[bh2])
            pool.op(lambda e: e.tensor_tensor(out=h2[:], in0=h2[:], in1=B2[:], op=ALU.add), reads=[bh2, bB2], writes=[bh2])
            act.op(lambda e: e.copy(out=h2b[:], in_=h2[:]), reads=[bh2], writes=[bh2b])
            act.dma(lambda e: e.dma_start(out=H2_d[tt * 128:(tt + 1) * 128, :], in_=h2b[:]), bh2b, k.db("H2_d"), bh2b)
            for g0 in range(0, KD, 4):
                n4 = min(4, KD - g0)
                ps, bps = pss.next()
                for j in range(n4):
                    kc = g0 + j
                    pe.op(lambda e: e.transpose(out=ps[:, j * 128:(j + 1) * 128], in_=h2[:, kc * 128:(kc + 1) * 128], identity=identf[:]),
                          reads=[bh2, b_identf], writes=[bps], partial=(j > 0), signal=(j == n4 - 1))
                dve.op(lambda e: e.tensor_copy(out=h2T[:, g0:g0 + n4, :], in_=ps[:, 0:n4 * 128].rearrange("p (k n) -> p k n", n=128)),
                       reads=[bps], writes=[bh2T], partial=True)
            ps, bps = pss.next()
            for kc in range(KD):
                pe.op(lambda e: e.matmul(ps[:, 0:NR], lhsT=h2T[:, kc, :], rhs=wr[:, kc, :], start=(kc == 0), stop=False),
                      reads=[bh2T, bwr], writes=[bps], partial=(kc > 0), signal=False)
            pe.op(lambda e: e.matmul(ps[:, 0:NR], lhsT=ones_f[0:1, :], rhs=brr[0:1, :], start=False, stop=True), reads=[b_ones, bbrr], writes=[bps], partial=True)
            dve.op(lambda e: e.tensor_copy(out=lg[:], in_=ps[:, 0:NR]), reads=[bps], writes=[blg])
            gl = lg[:, 0:NG]; el = lg[:, NG:NR].rearrange("p (g e) -> p g e", e=EPG)
            ohg = rt[:, 0, 0:NG]; eg = rt[:, 6, 0:NG]
            R = lambda fn, **kw: dve.op(fn, reads=[blg, brt, bsm], writes=[brt, bsm], **kw)
            R(lambda e: e.reduce_max(out=sm[:, 2:3], in_=gl, axis=AX.X))
            R(lambda e: e.tensor_scalar(out=ohg, in0=gl, scalar1=sm[:, 2:3], scalar2=None, op0=ALU.is_ge))
            R(lambda e: e.tensor_scalar(out=sm[:, 3:4], in0=sm[:, 2:3], scalar1=-1.0, scalar2=None, op0=ALU.mult))
            act.op(lambda e: e.activation(out=eg, in_=gl, func=AF.Exp, bias=sm[:, 3:4], scale=1.0, accum_out=sm[:, 4:5]), reads=[blg, bsm], writes=[brt, bsm])
            R(lambda e: e.reciprocal(out=sm[:, 5:6], in_=sm[:, 4:5]))
            tmp = rt[:, 1, :].rearrange("p (g e) -> p g e", e=EPG)
            R(lambda e: e.tensor_tensor(out=tmp, in0=el, in1=ohg.unsqueeze(2).to_broadcast([128, NG, EPG]), op=ALU.mult))
            sel = rt[:, 2, 0:EPG]; mk1 = rt[:, 3, 0:EPG]; sel2 = rt[:, 4, 0:EPG]; mk2 = rt[:, 5, 0:EPG]
            R(lambda e: e.tensor_reduce(out=sel, in_=tmp.rearrange("p g e -> p e g"), axis=AX.X, op=ALU.add))
            R(lambda e: e.reduce_max(out=sm[:, 6:7], in_=sel, axis=AX.X))
            R(lambda e: e.tensor_scalar(out=mk1, in0=sel, scalar1=sm[:, 6:7], scalar2=None, op0=ALU.is_ge))
            R(lambda e: e.scalar_tensor_tensor(out=sel2, in0=mk1, scalar=-1e30, in1=sel, op0=ALU.mult, op1=ALU.add))
            R(lambda e: e.reduce_max(out=sm[:, 7:8], in_=sel2, axis=AX.X))
            R(lambda e: e.tensor_scalar(out=mk2, in0=sel2, scalar1=sm[:, 7:8], scalar2=None, op0=ALU.is_ge))
            R(lambda e: e.tensor_tensor(out=sm[:, 8:9], in0=sm[:, 7:8], in1=sm[:, 6:7], op=ALU.subtract))
            act.op(lambda e: e.activation(out=sm[:, 9:10], in_=sm[:, 8:9], func=AF.Exp), reads=[bsm], writes=[bsm])
            R(lambda e: e.tensor_scalar(out=sm[:, 10:11], in0=sm[:, 9:10], scalar1=1.0, scalar2=None, op0=ALU.add))
            R(lambda e: e.reciprocal(out=sm[:, 10:11], in_=sm[:, 10:11]))
            dve.op(lambda e: e.tensor_tensor(out=wsel[:, tt, 0:1], in0=sm[:, 5:6], in1=sm[:, 10:11], op=ALU.mult), reads=[bsm], writes=[bwsel], partial=True)
            dve.op(lambda e: e.tensor_tensor(out=wsel[:, tt, 1:2], in0=wsel[:, tt, 0:1], in1=sm[:, 9:10], op=ALU.mult), reads=[bsm, bwsel], writes=[bwsel], partial=True)
            bo = lambda a: a.unsqueeze(2).to_broadcast([128, NG, EPG])
            be_ = lambda a: a.unsqueeze(1).to_broadcast([128, NG, EPG])
            v3 = lambda a: a.rearrange("p (g e) -> p g e", e=EPG)
            dve.op(lambda e: e.tensor_tensor(out=v3(I1_all[:, tt, :]), in0=bo(ohg), in1=be_(mk1), op=ALU.mult), reads=[brt], writes=[bI1], partial=True)
            dve.op(lambda e: e.tensor_tensor(out=v3(I2_all[:, tt, :]), in0=bo(ohg), in1=be_(mk2), op=ALU.mult), reads=[brt], writes=[bI2], partial=True)
            dve.op(lambda e: e.tensor_tensor(out=Ind_all[:, tt, :], in0=I1_all[:, tt, :], in1=I2_all[:, tt, :], op=ALU.add), reads=[bI1, bI2], writes=[bInd], partial=True)
        cnt = S_("cnt", [128, 4, NE], F32, ph); bcnt = Buf("cnt")
        ps, bps = pss.next()
        for tt in range(NT):
            pe.op(lambda e: e.matmul(ps[:, 0:NE], lhsT=ones_b[:], rhs=Ind_all[:, tt, :], start=(tt == 0), stop=(tt == NT - 1)),
                  reads=[b_onesb, bInd], writes=[bps], partial=(tt > 0), signal=(tt == NT - 1))
        C = lambda fn, **kw: dve.op(fn, reads=[bcnt], writes=[bcnt], **kw)
        dve.op(lambda e: e.tensor_copy(out=cnt[:, 0, :], in_=ps[:, 0:NE]), reads=[bps], writes=[bcnt])
        thri = S_("thri", [128, NT], I32, ph); bthri = Buf("thri")
        pool.op(lambda e: e.iota(out=thri[:], pattern=[[128, NT]], base=0, channel_multiplier=0), writes=[bthri])
        thrf = S_("thrf", [128, NT], F32, ph); bthrf = Buf("thrf")
        dve.op(lambda e: e.tensor_copy(out=thrf[:], in_=thri[:]), reads=[bthri], writes=[bthrf])
        cmpc = S_("cmpc", [128, NE, NT], F32, ph); bcmpc = Buf("cmpc")
        dve.op(lambda e: e.tensor_tensor(out=cmpc[:], in0=cnt[:, 0, :].unsqueeze(2).to_broadcast([128, NE, NT]),
                                         in1=thrf[:].unsqueeze(1).to_broadcast([128, NE, NT]), op=ALU.is_gt), reads=[bcnt, bthrf], writes=[bcmpc])
        dve.op(lambda e: e.reduce_sum(out=cnt[:, 1, :], in_=cmpc[:], axis=AX.X), reads=[bcmpc], writes=[bcnt])
        C(lambda e: e.tensor_scalar(out=cnt[:, 1, :], in0=cnt[:, 1, :], scalar1=128.0, scalar2=None, op0=ALU.mult))
        ps, bps = pss.next()
        pe.op(lambda e: e.transpose(out=ps[0:NE, 0:128], in_=cnt[:, 1, :], identity=identf[:]), reads=[bcnt, b_identf], writes=[bps])
        pcT = S_("pcT", [128, 128], F32, ph); bpcT = Buf("pcT")
        dve.op(lambda e: e.tensor_copy(out=pcT[0:NE, :], in_=ps[0:NE, 0:128]), reads=[bps], writes=[bpcT])
        ps, bps = pss.next()
        pe.op(lambda e: e.matmul(ps[:, 0:NE], lhsT=pcT[0:NE, :], rhs=Uf[0:NE, 0:NE], start=True, stop=True), reads=[bpcT, bUf], writes=[bps])
        dve.op(lambda e: e.tensor_copy(out=cnt[:, 2, :], in_=ps[:, 0:NE]), reads=[bps], writes=[bcnt])
        C(lambda e: e.tensor_tensor(out=cnt[:, 3, :], in0=cnt[:, 2, :], in1=cnt[:, 1, :], op=ALU.add))
        posf = S_("posf", [128, NT, 2], F32, ph); bposf = Buf("posf")
        pz = S_("pz", [128, 2, NE], F32, ph); bpz = Buf("pz")
        for tt in range(NT):
            ps, bps = pss.next()
            for t2 in range(tt):
                pe.op(lambda e: e.matmul(ps[:, 0:NE], lhsT=ones_b[:], rhs=Ind_all[:, t2, :], start=(t2 == 0), stop=False),
                      reads=[b_onesb, bInd], writes=[bps], partial=(t2 > 0), signal=False)
            pe.op(lambda e: e.matmul(ps[:, 0:NE], lhsT=Ub[:], rhs=Ind_all[:, tt, :], start=(tt == 0), stop=True), reads=[bUb, bInd], writes=[bps], partial=(tt > 0))
            dve.op(lambda e: e.tensor_tensor(out=pz[:, 0, :], in0=ps[:, 0:NE], in1=cnt[:, 2, :], op=ALU.add), reads=[bps, bcnt], writes=[bpz])
            dve.op(lambda e: e.tensor_tensor(out=pz[:, 1, :], in0=pz[:, 0, :], in1=I1_all[:, tt, :], op=ALU.mult), reads=[bpz, bI1], writes=[bpz])
            dve.op(lambda e: e.reduce_sum(out=posf[:, tt, 0:1], in_=pz[:, 1, :], axis=AX.X), reads=[bpz], writes=[bposf], partial=True)
            dve.op(lambda e: e.tensor_tensor(out=pz[:, 1, :], in0=pz[:, 0, :], in1=I2_all[:, tt, :], op=ALU.mult), reads=[bpz, bI2], writes=[bpz])
            dve.op(lambda e: e.reduce_sum(out=posf[:, tt, 1:2], in_=pz[:, 1, :], axis=AX.X), reads=[bpz], writes=[bposf], partial=True)
        dve.op(lambda e: e.tensor_copy(out=idx_all[:], in_=posf[:]), reads=[bposf], writes=[bidx])
        bsi = S_("bsi", [128, NBLK], I32, ph); bbsi = Buf("bsi")
        pool.op(lambda e: e.iota(out=bsi[:], pattern=[[128, NBLK]], base=0, channel_multiplier=0), writes=[bbsi])
        bsf = S_("bsf", [128, NBLK], F32, ph); bbsf = Buf("bsf")
        dve.op(lambda e: e.tensor_copy(out=bsf[:], in_=bsi[:]), reads=[bbsi], writes=[bbsf])
        pidi = S_("pidi", [128, 1], I32, ph); bpidi = Buf("pidi")
        pool.op(lambda e: e.iota(out=pidi[:], pattern=[[0, 1]], base=0, channel_multiplier=1), writes=[bpidi])
        pidf = S_("pidf", [128, 1], F32, ph); bpidf = Buf("pidf")
        dve.op(lambda e: e.tensor_copy(out=pidf[:], in_=pidi[:]), reads=[bpidi], writes=[bpidf])
        cmp_ = S_("cmp", [128, NBLK, NE], F32, ph); bcmp = Buf("cmp")
        dve.op(lambda e: e.tensor_tensor(out=cmp_[:], in0=cnt[:, 3, :].unsqueeze(1).to_broadcast([128, NBLK, NE]),
                                         in1=bsf[:].unsqueeze(2).to_broadcast([128, NBLK, NE]), op=ALU.is_le), reads=[bcnt, bbsf], writes=[bcmp])
        bef = S_("bef", [128, NBLK], F32, ph); bbef = Buf("bef")
        dve.op(lambda e: e.reduce_sum(out=bef[:], in_=cmp_[:], axis=AX.X), reads=[bcmp], writes=[bbef])
        dve.op(lambda e: e.tensor_scalar(out=bef[:], in0=bef[:], scalar1=float(NE - 1), scalar2=None, op0=ALU.min), reads=[bbef], writes=[bbef])
        wq = S_("wq", [128, 3, NBLK], F32, ph); bwq = Buf("wq")
        nsame = S_("nsame", [128, NBLK], F32, ph); bns = Buf("nsame")
        dve.op(lambda e: e.memset(nsame[:], 1.0), writes=[bns])
        dve.op(lambda e: e.tensor_tensor(out=nsame[:, 1:NBLK], in0=bef[:, 1:NBLK], in1=bef[:, 0:NBLK - 1], op=ALU.not_equal), reads=[bbef], writes=[bns])
        for q in range(NP):
            dve.op(lambda e: e.tensor_scalar(out=wq[:, 0, :], in0=bef[:], scalar1=float(q * EPP), scalar2=None, op0=ALU.is_ge), reads=[bbef], writes=[bwq])
            dve.op(lambda e: e.tensor_scalar(out=wq[:, 1, :], in0=bef[:], scalar1=float((q + 1) * EPP), scalar2=None, op0=ALU.is_lt), reads=[bbef], writes=[bwq], partial=True)
            dve.op(lambda e: e.tensor_tensor(out=wq[:, 0, :], in0=wq[:, 0, :], in1=wq[:, 1, :], op=ALU.mult), reads=[bwq], writes=[bwq])
            dve.op(lambda e: e.tensor_tensor(out=wq[:, 0, :], in0=wq[:, 0, :], in1=nsame[:], op=ALU.mult), reads=[bwq, bns], writes=[bwq])
            dve.op(lambda e: e.tensor_scalar(out=wq[:, 1, :], in0=bef[:], scalar1=128.0, scalar2=pidf[:, 0:1], op0=ALU.mult, op1=ALU.add),
                   reads=[bbef, bpidf, bwq], writes=[bwq])
            dve.op(lambda e: e.tensor_scalar(out=wq[:, 1, :], in0=wq[:, 1, :], scalar1=-float(q * EPP * 128) - 1.0e6, scalar2=None, op0=ALU.add), reads=[bwq], writes=[bwq])
            dve.op(lambda e: e.tensor_tensor(out=wq[:, 1, :], in0=wq[:, 1, :], in1=wq[:, 0, :], op=ALU.mult), reads=[bwq], writes=[bwq])
            dve.op(lambda e: e.tensor_scalar(out=wq[:, 1, :], in0=wq[:, 1, :], scalar1=1.0e6, scalar2=None, op0=ALU.add), reads=[bwq], writes=[bwq])
            for si in range(NSP_):
                dve.op(lambda e: e.tensor_scalar(out=wq[:, 2, :], in0=wq[:, 1, :], scalar1=float(NSP_), scalar2=float(si), op0=ALU.mult, op1=ALU.add),
                       reads=[bwq], writes=[bwq])
                dve.op(lambda e: e.tensor_copy(out=widx_i[:, q * NSP_ + si, :], in_=wq[:, 2, :]), reads=[bwq], writes=[bwidx], partial=(q + si > 0))
        if "dbg_route" in debug_outs:
            for nm, t_, b_, shp, dt_ in (("dbg_idx", idx_all, bidx, [128, NT * 2], I32), ("dbg_widx", widx_i, bwidx, [128, 4 * NSP_ * NBLK], I32),
                                         ("dbg_wsel", wsel, bwsel, [128, NT * 2], F32)):
                o_ = nc.dram_tensor(nm, shp, dt_, kind="ExternalOutput").ap(); k.dbufs[nm] = Buf(nm)
                src_ = t_[:].rearrange("p a b -> p (a b)") if len(t_.shape) == 3 else t_[:]
                sp.dma(lambda e: e.dma_start(out=o_, in_=src_), b_, k.db(nm), b_)
        sc.barrier()
    if upto <= 9:
        return

    NSP = max(1, (KD * DE * 4 + 32767) // 32768)
    assert NSP == max(1, (HC * D * 4 + 32767) // 32768) and KD % NSP == 0 and HC % NSP == 0
    bnd_reg = nc.gpsimd.to_reg(EPP * 128 * NSP - 1)
    w1v = [w.rearrange("(r c) f -> r (c f)", c=KD // NSP) for w in w1p]; w3v = [w.rearrange("(r c) f -> r (c f)", c=KD // NSP) for w in w3p]
    w2v = [w.rearrange("(r c) f -> r (c f)", c=HC // NSP) for w in w2p]
    with ExitStack() as ph:
        zt = S_("zt", [128, D], BF16, ph); bzt = Buf("zt")
        dve.op(lambda e: e.memset(zt[:], 0.0), writes=[bzt])
        for b in range(NBLK):
            sp.dma(lambda e: e.dma_start(out=Xs_d[b * 128:(b + 1) * 128, :], in_=zt[:]), bzt, k.db("Xs_d"), bzt)
        hbs = Ring("sh2", [128, D], BF16, 2, ph)
        for tt in range(NT):
            hb, bhb = hbs.next()
            sp.dma(lambda e: e.dma_start(out=hb[:], in_=H2_d[tt * 128:(tt + 1) * 128, :]), k.db("H2_d"), bhb, bhb, partial=False)
            for kk in range(2):
                pool.wait(dict(bidx.w));
                pool.dma(lambda e: e.indirect_dma_start(out=Xs_d[:, :], out_offset=bass.IndirectOffsetOnAxis(ap=idx_all[:, tt, kk:kk + 1], axis=0),
                                                        in_=hb[:, :], in_offset=None), bhb, k.db("Xs_d"), bhb, partial=False)
        w1s = S_("w1s", [128, KD * DE], BF16, ph); bw1 = Buf("w1s")
        w3s = S_("w3s", [128, KD * DE], BF16, ph); bw3 = Buf("w3s")
        w2s = S_("w2s", [128, HC * D], BF16, ph); bw2 = Buf("w2s")
        xbs = Ring("xb", [128, D], BF16, 2, ph); xts = Ring("xbT", [128, KD, 128], BF16, 2, ph)
        hid = S_("hidT", [128, HC, 128], BF16, ph); bhid = Buf("hidT")
        sl = Ring("sil", [128, 128], F32, 2, ph)
        yb = S_("yb", [128, D], F32, ph); byb = Buf("yb")
        pst = Ring("bpt", [128, 1024], BF16, 2, ph, psum=True)
        pss = Ring("bps", [128, 512], F32, 4, ph, psum=True)
        for b in range(NBLK):
            pool.wait(dict(bwidx.w))
            for (ws, bw, wv, nm, rowlen) in ((w1s, bw1, w1v, "w1", KD * DE), (w3s, bw3, w3v, "w3", KD * DE), (w2s, bw2, w2v, "w2", HC * D)):
                cw = rowlen // NSP
                for q in range(NP):
                    for si in range(NSP):
                        pool.dma(lambda e: e.indirect_dma_start(out=ws[:, si * cw:(si + 1) * cw], out_offset=None, in_=wv[q][:, :],
                                                                in_offset=bass.IndirectOffsetOnAxis(ap=widx_i[:, q * NSP + si, b:b + 1], axis=0),
                                                                bounds_check=bnd_reg, oob_is_err=False),
                                 k.db("%s_%d" % (nm, q)), bw, bw, partial=(q + si > 0))
            xb, bxb = xbs.next(); xT, bxT = xts.next()
            sp.dma(lambda e: e.dma_start(out=xb[:], in_=Xs_d[b * 128:(b + 1) * 128, :]), k.db("Xs_d"), bxb, bxb, partial=False)
            for g0 in range(0, KD, 8):
                n8 = min(8, KD - g0)
                ps, bps = pst.next()
                for j in range(n8):
                    cc = g0 + j
                    pe.op(lambda e: e.transpose(out=ps[:, j * 128:(j + 1) * 128], in_=xb[:, cc:D:KD], identity=ident[:]),
                          reads=[bxb, b_ident], writes=[bps], partial=(j > 0), signal=(j == n8 - 1))
                dve.op(lambda e: e.tensor_copy(out=xT[:, g0:g0 + n8, :], in_=ps[:, 0:n8 * 128].rearrange("p (k n) -> p k n", n=128)),
                       reads=[bps], writes=[bxT], partial=True)
            for c2 in range(HC):
                p1, bp1 = pss.next(); p3, bp3 = pss.next()
                for (pp, bpp, ws, bw) in ((p1, bp1, w1s, bw1), (p3, bp3, w3s, bw3)):
                    for cc in range(KD):
                        pe.op(lambda e: e.matmul(pp[:, 0:128], lhsT=ws[:, cc * DE + c2:(cc + 1) * DE:HC], rhs=xT[:, cc, :], start=(cc == 0), stop=(cc == KD - 1)),
                              reads=[bw, bxT], writes=[bpp], partial=(cc > 0), signal=(cc == KD - 1))
                s_, bs_ = sl.next()
                act.op(lambda e: e.activation(out=s_[:], in_=p1[:, 0:128], func=AF.Silu), reads=[bp1], writes=[bs_])
                dve.op(lambda e: e.tensor_tensor(out=hid[:, c2, :], in0=p3[:, 0:128], in1=s_[:], op=ALU.mult), reads=[bp3, bs_], writes=[bhid], partial=(c2 > 0))
            for gi, g0 in enumerate(range(0, D, 512)):
                gw = min(512, D - g0)
                ps, bps = pss.next()
                for c2 in range(HC):
                    pe.op(lambda e: e.matmul(ps[:, 0:gw], lhsT=hid[:, c2, :], rhs=w2s[:, c2 * D + g0:c2 * D + g0 + gw], start=(c2 == 0), stop=(c2 == HC - 1)),
                          reads=[bhid, bw2], writes=[bps], partial=(c2 > 0), signal=(c2 == HC - 1))
                if gi % 2 == 0:
                    dve.op(lambda e: e.tensor_copy(out=yb[:, g0:g0 + gw], in_=ps[:, 0:gw]), reads=[bps], writes=[byb], partial=(gi > 0))
                else:
                    act.op(lambda e: e.copy(out=yb[:, g0:g0 + gw], in_=ps[:, 0:gw]), reads=[bps], writes=[byb], partial=True)
            act.dma(lambda e: e.dma_start(out=Ys_d[b * 128:(b + 1) * 128, :], in_=yb[:]), byb, k.db("Ys_d"), byb)
        sc.barrier()
    if upto <= 10:
        return

    with ExitStack() as ph:
        pss = Ring("fps", [128, 512], F32, 2, ph, psum=True)
        gt2 = S_("gt2", [128, D], F32, ph); bgt2 = Buf("gt2"); gfb = S_("gfb", [128, D], F32, ph); bgfb = Buf("gfb")
        sp.dma(lambda e: e.dma_start(out=gt2[:], in_=mod_bc[:, 3 * D:4 * D]), k.db("mod_bc"), bgt2, bgt2)
        bcast_row(g_final, "g_final", gfb, bgfb, ph, pss)
        y0 = S_("y0", [128, D], F32, ph); by0 = Buf("y0"); y1 = S_("y1", [128, D], F32, ph); by1 = Buf("y1")
        x1s = Ring("fx1", [128, D], F32, 2, ph)
        fo = S_("fo", [128, D], F32, ph); bfo = Buf("fo")
        fjk = S_("fjk", [128, D], BF16, ph); bfjk = Buf("fjk")
        fs = S_("fs", [128, 2], F32, ph); bfs = Buf("fs")
        for tt in range(NT):
            x1, bx1 = x1s.next()
            sp.dma(lambda e: e.dma_start(out=x1[:], in_=X1_d[tt * 128:(tt + 1) * 128, :]), k.db("X1_d"), bx1, bx1, partial=False)
            pool.wait(dict(bidx.w))
            pool.dma(lambda e: e.indirect_dma_start(out=y0[:, :], out_offset=None, in_=Ys_d[:, :],
                                                    in_offset=bass.IndirectOffsetOnAxis(ap=idx_all[:, tt, 0:1], axis=0)), k.db("Ys_d"), by0, by0, partial=False)
            pool.dma(lambda e: e.indirect_dma_start(out=y1[:, :], out_offset=None, in_=Ys_d[:, :],
                                                    in_offset=bass.IndirectOffsetOnAxis(ap=idx_all[:, tt, 1:2], axis=0)), k.db("Ys_d"), by1, by1, partial=False)
            dve.op(lambda e: e.tensor_scalar(out=y0[:], in0=y0[:], scalar1=wsel[:, tt, 0:1], scalar2=None, op0=ALU.mult), reads=[by0, bwsel], writes=[by0])
            dve.op(lambda e: e.scalar_tensor_tensor(out=y0[:], in0=y1[:], scalar=wsel[:, tt, 1:2], in1=y0[:], op0=ALU.mult, op1=ALU.add),
                   reads=[by1, by0, bwsel], writes=[by0])
            pool.op(lambda e: e.tensor_tensor(out=y0[:], in0=y0[:], in1=gt2[:], op=ALU.mult), reads=[by0, bgt2], writes=[by0])
            dve.op(lambda e: e.tensor_tensor(out=fo[:], in0=y0[:], in1=x1[:], op=ALU.add), reads=[by0, bx1], writes=[bfo])
            act.op(lambda e: e.activation(out=fjk[:], in_=fo[:], func=AF.Square, accum_out=fs[:, 0:1]), reads=[bfo], writes=[bfjk, bfs])
            dve.op(lambda e: e.tensor_scalar(out=fs[:, 1:2], in0=fs[:, 0:1], scalar1=1.0 / D, scalar2=1e-6, op0=ALU.mult, op1=ALU.add), reads=[bfs], writes=[bfs])
            act.op(lambda e: e.activation(out=fs[:, 1:2], in_=fs[:, 1:2], func=AF.Ln), reads=[bfs], writes=[bfs])
            act.op(lambda e: e.activation(out=fs[:, 1:2], in_=fs[:, 1:2], func=AF.Exp, scale=-0.5), reads=[bfs], writes=[bfs])
            dve.op(lambda e: e.scalar_tensor_tensor(out=fo[:], in0=fo[:], scalar=fs[:, 1:2], in1=gfb[:], op0=ALU.mult, op1=ALU.mult),
                   reads=[bfo, bfs, bgfb], writes=[bfo])
            sp.dma(lambda e: e.dma_start(out=out[tt * 128:(tt + 1) * 128, :], in_=fo[:]), bfo, k.db("out"), bfo)
        sc.barrier()


def rope_tables(cfg, half):
    c = cfg
    pos = np.concatenate([np.arange(c.NQ) + half * c.NQ, np.arange(c.NQ) + (1 - half) * c.NQ])
    row = (pos // c.GW).astype(np.float64); col = (pos % c.GW).astype(np.float64)
    def tab(rot):
        q = rot // 4
        inv = 10000.0 ** (-np.arange(q, dtype=np.float64) / q)
        ang = np.concatenate([row[:, None] * inv, col[:, None] * inv], axis=-1)
        hd = rot // 2
        cs = np.ones((rot, c.NK), np.float32); sn = np.zeros((rot, c.NK), np.float32)
        cs[:hd, :c.S] = np.cos(ang).T; cs[hd:, :c.S] = np.cos(ang).T
        sn[:hd, :c.S] = -np.sin(ang).T; sn[hd:, :c.S] = np.sin(ang).T
        return cs, sn
    cd, sd = tab(128); cm, sm = tab(64)
    return dict(cosd=cd, sind=sd, cosm=cm, sinm=sm)


def weight_pieces(c, inp):
    g = lambda n: np.asarray(inp[n][0]).reshape(-1, np.asarray(inp[n]).shape[-1])
    out = {}
    wm = g("w_mod")
    out["w_mod_0"] = wm[:, :3 * c.D]; out["w_mod_1"] = wm[:, 3 * c.D:]
    for n in ("w_in", "w_uq", "w_ukv", "w_br_a", "w_br_b", "w_out"):
        out[n] = g(n)
    epp = c.NE // 4
    for n, rpe in (("w1", c.D), ("w3", c.D), ("w2", c.DE)):
        w = g(n)
        for q in range(4):
            out["%s_%d" % (n, q)] = w[q * epp * rpe:(q + 1) * epp * rpe]
    return out


SHARD_POS = {0: 0, 4: 1, 1: 2, 5: 3, 2: 4, 6: 5, 3: 6, 7: 7}


GATHER_CHUNK_BYTES = 512 * 1024


def gather_unit(rows, cols):
    r8 = rows // 8
    u = max(1, min(r8, (GATHER_CHUNK_BYTES) // (cols * 4)))
    while r8 % u:
        u -= 1
    return u


def prep_core_inputs(cfg, inp, core, gather):
    c = cfg
    b, half = (core // 2, core % 2) if c.NCORES > 1 else (0, 0)
    f = lambda a: np.ascontiguousarray(np.asarray(a, dtype=np.float32))
    x = np.asarray(inp["x"][b]); ctx = np.asarray(inp["ctx"][b])
    own = x[half * c.NQ:(half + 1) * c.NQ]; oth = x[(1 - half) * c.NQ:(2 - half) * c.NQ]
    m = dict(xk=f(np.concatenate([own, oth, ctx], axis=0)), c_b=f(inp["c"][b:b + 1]), c_ctx=f(np.asarray(inp["c_ctx"])[None, :]))
    m.update(rope_tables(c, half))
    for n in ("b_mod", "g_attn", "g_q", "g_kv", "g_subln", "g_ffn", "lam_q1", "lam_k1", "lam_q2", "lam_k2"):
        m[n] = f(np.asarray(inp[n]).reshape(1, -1))
    m["g_final"] = f(np.asarray(inp["g_final"]).reshape(1, -1))
    m["w_r"] = f(np.concatenate([np.asarray(inp["w_grp"][0]), np.asarray(inp["w_exp"][0])], axis=1))
    m["b_r"] = f(np.concatenate([np.asarray(inp["b_grp"][0]), np.asarray(inp["b_exp"][0])])[None, :])
    for n, w2 in weight_pieces(c, inp).items():
        if gather:
            rows, cols = w2.shape
            u = gather_unit(rows, cols); nch = rows // 8 // u
            t_ = SHARD_POS[core]
            m[n + "_sh"] = f(w2.reshape(nch, 8, u, cols)[:, t_].reshape(nch * u, cols))
        else:
            m[n] = f(w2)
    return m


_NC_CACHE = {}


def kernel(**inputs):
    cfg = Cfg()
    n = cfg.NCORES
    if "nc" not in _NC_CACHE:
        _NC_CACHE["nc"] = build_program(cfg, gather=True)
    nc = _NC_CACHE["nc"]
    need = set()
    for a in nc.allocations:
        try:
            if a.kind == "ExternalInput":
                need.add(a.memorylocations[0].name)
        except Exception:
            pass
    in_maps = []
    for core in range(n):
        m = prep_core_inputs(cfg, inputs, core, gather=True)
        in_maps.append({k_: v for k_, v in m.items() if k_ in need})
    res = run_bass_kernel_spmd(nc, in_maps, core_ids=list(range(n)))
    out = np.empty((cfg.B, cfg.S, cfg.D), np.float32)
    for core in range(n):
        b, half = core // 2, core % 2
        out[b, half * cfg.NQ:(half + 1) * cfg.NQ] = res.results[core]["out"]
    return out
```

```python
import math
from contextlib import ExitStack
import numpy as np
import concourse.bass as bass
import concourse.mybir as mybir
from concourse.bass_utils import run_bass_kernel_spmd

F32 = mybir.dt.float32
BF16 = mybir.dt.bfloat16
I32 = mybir.dt.int32
AF = mybir.ActivationFunctionType
ALU = mybir.AluOpType
AX = mybir.AxisListType


class Cfg:
    def __init__(self, **kw):
        self.D = 4096; self.B = 4; self.S = 4096; self.GW = 64; self.C = 256
        self.DAH = 8; self.MH = 16; self.QL = 1024; self.KVL = 512
        self.NG = 8; self.EPG = 8; self.DE = 512; self.NCORES = 8
        self.lambda_init = 0.8 - 0.6 * math.exp(-0.3 * 0)
        for k, v in kw.items():
            setattr(self, k, v)
        self.KD = self.D // 128
        self.NQ = self.S // 2
        self.NK = self.S + self.C
        self.NE = self.NG * self.EPG
        self.DAW = self.DAH * 256
        self.MW = self.MH * 128
        self.INC = 3 * self.DAW + self.QL + self.KVL + 64 + 2 * self.D
        self.o_dq = 0; self.o_dk = self.DAW; self.o_dv = 2 * self.DAW
        self.o_cq = 3 * self.DAW; self.o_ckv = self.o_cq + self.QL
        self.o_ga = self.o_ckv + self.KVL + 64; self.o_gb = self.o_ga + self.D


class Buf:
    def __init__(self, name, sem=None):
        self.name = name
        self.w = {}
        self.r = {}
        self.sem = sem
        self.ndma = 0


def _merge(d, s):
    for k, v in s.items():
        if d.get(k, 0) < v:
            d[k] = v


class Eng:
    def __init__(self, sched, name, e, sem):
        self.s = sched; self.name = name; self.e = e; self.sem = sem
        self.cnt = 0
        self.seen = {}
        self.pend_r = []; self.pend_w = []

    def wait(self, deps):
        for k, v in deps.items():
            if self.seen.get(k, 0) < v:
                self.e.wait_ge(self.s.sems[k], v)
                self.seen[k] = v

    def _deps(self, reads, writes, partial):
        deps = {}
        for t in reads:
            _merge(deps, t.w)
        for t in writes:
            _merge(deps, t.r)
            if not partial:
                _merge(deps, t.w)
        return deps

    def op(self, fn, reads=(), writes=(), partial=False, signal=True):
        self.wait(self._deps(reads, writes, partial))
        ins = fn(self.e)
        self.pend_r += list(reads); self.pend_w += list(writes)
        if signal:
            self.cnt += 1
            ins.then_inc(self.sem_h, 1)
            ev = {self.sem: self.cnt}
            self.s.last[self.sem] = self.cnt
            for t in self.pend_r:
                _merge(t.r, ev)
            for t in self.pend_w:
                _merge(t.w, ev)
            self.pend_r = []; self.pend_w = []
        return ins

    def dma(self, fn, src, dst, via, partial=True, inc=16):
        self.wait(self._deps([src], [dst], partial))
        ins = fn(self.e)
        k = self.s.sem_of(via, self.name == 'pool')
        self.s.semcnt[k] = self.s.semcnt.get(k, 0) + inc
        ins.then_inc(self.s.sems[k], inc)
        ev = {k: self.s.semcnt[k]}
        self.s.last[k] = self.s.semcnt[k]
        _merge(src.r, ev)
        _merge(dst.w, ev)
        return ins


class Sched:
    def __init__(self, nc, stack):
        self.nc = nc
        self.sems = {}
        self.last = {}
        self.stack = stack
        self.n = 0
        self.dpool = []
        self.dpool_sw = []
        self.n_sw = 0
        self.semcnt = {}
        self.pe = self._eng("pe", nc.tensor)
        self.act = self._eng("act", nc.scalar)
        self.dve = self._eng("dve", nc.vector)
        self.pool = self._eng("pool", nc.gpsimd)
        self.sp = self._eng("sp", nc.sync)
        self.engs = [self.pe, self.act, self.dve, self.pool, self.sp]
        self.ccsem = self._newsem('ccsem'); self.ccn = 0

    def _newsem(self, name):
        h = self.stack.enter_context(self.nc.semaphore(name))
        k = name
        self.sems[k] = h
        return k

    def _eng(self, name, e):
        k = self._newsem("e_" + name)
        en = Eng(self, name, e, k)
        en.sem_h = self.sems[k]
        return en

    def sem_of(self, buf, sw=False):
        key = "sem_sw" if sw else "sem"
        if getattr(buf, key, None) is None:
            pl = self.dpool_sw if sw else self.dpool
            if len(pl) < 40:
                pl.append(self._newsem(("s%d" if sw else "d%d") % len(pl)))
            cnt = self.n_sw if sw else self.n
            setattr(buf, key, pl[cnt % 40])
            if sw:
                self.n_sw += 1
            else:
                self.n += 1
        return getattr(buf, key)

    def barrier(self, engs=None):
        d = dict(self.last)
        d.pop(self.ccsem, None)
        for en in (engs or self.engs):
            en.wait(d)


class K:
    def __init__(self, cfg, gather):
        self.cfg = cfg
        self.gather = gather
        self.nc = bass.Bass("TRN2", target_bir_lowering=False)
        self.ins = {}
        self.dbufs = {}

    def dram(self, name, shape, dt, kind="Internal"):
        t = self.nc.dram_tensor(name, list(shape), dt, kind=kind).ap()
        self.dbufs[name] = Buf(name)
        return t

    def db(self, name):
        return self.dbufs[name]


def build_program(cfg, gather=True, upto=99, debug_outs=()):
    k = K(cfg, gather)
    nc = k.nc
    c = cfg
    D, KD, NQ, NK, S, C = c.D, c.KD, c.NQ, c.NK, c.S, c.C
    with ExitStack() as top:
        sc = Sched(nc, top)
        pe, act, dve, pool, sp = sc.pe, sc.act, sc.dve, sc.pool, sc.sp
        _build(k, sc, top, upto, debug_outs)
    return nc


def _build(k, sc, top, upto, debug_outs):
    nc = k.nc; c = k.cfg
    D, KD, NQ, NK, S, C = c.D, c.KD, c.NQ, c.NK, c.S, c.C
    pe, act, dve, pool, sp = sc.pe, sc.act, sc.dve, sc.pool, sc.sp
    dbg = lambda n: ("ExternalOutput" if n in debug_outs else "Internal")

    def inp(name, shape, dt=F32):
        t = nc.dram_tensor(name, list(shape), dt, kind="ExternalInput").ap()
        k.dbufs[name] = Buf(name)
        return t

    def scratch(name, shape, dt):
        return k.dram(name, shape, dt, kind=dbg(name))

    pending_gathers = []

    def gather_group():
        grp = list(pending_gathers); del pending_gathers[:]
        for (name, shi, pair, full, u, nch) in grp:
            pool.wait(dict(k.db(name + "_shi").w))
        for (name, shi, pair, full, u, nch) in grp:
            for i in range(nch):
                ins = pool.e.collective_compute("AllGather", ALU.bypass, replica_groups=[[0, 4], [1, 5], [2, 6], [3, 7]],
                                                ins=[shi[i * u:(i + 1) * u, :]], outs=[pair[i * 2 * u:(i + 1) * 2 * u, :]])
                sc.ccn += 1
                ins.then_inc(sc.sems[sc.ccsem], 1)
        for (name, shi, pair, full, u, nch) in grp:
            for i in range(nch):
                ins = pool.e.collective_compute("AllGather", ALU.bypass, replica_groups=[[0, 1, 2, 3], [4, 5, 6, 7]],
                                                ins=[pair[i * 2 * u:(i + 1) * 2 * u, :]], outs=[full[i * 8 * u:(i + 1) * 8 * u, :]])
                sc.ccn += 1
                ins.then_inc(sc.sems[sc.ccsem], 1)
            sc.last[sc.ccsem] = sc.ccn
            _merge(k.db(name).w, {sc.ccsem: sc.ccn})

    def weight(name, rows, cols):
        if not k.gather:
            return inp(name, [rows, cols])
        r8 = rows // 8
        u = gather_unit(rows, cols); nch = r8 // u
        sh = inp(name + "_sh", [r8, cols])
        shi = k.dram(name + "_shi", [r8, cols], F32)
        pair = k.dram(name + "_pr", [2 * r8, cols], F32)
        full = k.dram(name, [rows, cols], F32)
        hc = Buf("cp_" + name)
        sp.dma(lambda e: e.dma_start(out=shi, in_=sh), k.db(name + "_sh"), k.db(name + "_shi"), hc)
        pending_gathers.append((name, shi, pair, full, u, nch))
        return full

    xk = inp("xk", [NK, D])
    c_b = inp("c_b", [1, D]); c_ctx = inp("c_ctx", [1, D])
    b_mod = inp("b_mod", [1, 6 * D]); g_attn = inp("g_attn", [1, D])
    out = nc.dram_tensor("out", [NQ, D], F32, kind="ExternalOutput").ap(); k.dbufs["out"] = Buf("out")
    w_mod_p = [weight("w_mod_%d" % i, D, 3 * D) for i in range(2)]
    if k.gather:
        gather_group()
    w_in = weight("w_in", D, c.INC)
    w_uq = weight("w_uq", c.QL, c.MH * 192); w_ukv = weight("w_ukv", c.KVL, c.MH * 256)
    w_br_a = weight("w_br_a", c.DAW, D); w_br_b = weight("w_br_b", c.MW, D); w_out = weight("w_out", D, D)
    NP = 4; EPP = c.NE // NP
    w1p = [weight("w1_%d" % q, EPP * D, c.DE) for q in range(NP)]; w3p = [weight("w3_%d" % q, EPP * D, c.DE) for q in range(NP)]
    w2p = [weight("w2_%d" % q, EPP * c.DE, D) for q in range(NP)]
    if k.gather:
        gather_group()

    uid = [0]
    def S_(name, shape, dt, st):
        uid[0] += 1
        return st.enter_context(nc.sbuf_tensor("%s_%d" % (name, uid[0]), list(shape), dt))
    def P_(name, shape, dt, st):
        uid[0] += 1
        return st.enter_context(nc.psum_tensor("%s_%d" % (name, uid[0]), list(shape), dt))

    mod_bc = scratch("mod_bc", [128, 4 * D], F32)
    A1T = S_("A1T", [128, KD, 2], F32, top); B1T = S_("B1T", [128, KD, 2], F32, top)
    bA1T = Buf("A1T"); bB1T = Buf("B1T")
    ones_f = S_("ones_f", [128, 128], F32, top); b_ones = Buf("ones_f")
    dve.op(lambda e: e.memset(ones_f[:], 1.0), writes=[b_ones])

    with ExitStack() as ph:
        GC = 256
        cT = S_("cT", [128, KD, 2], F32, ph); bcT = Buf("cT")
        sT = S_("sT", [128, KD, 2], F32, ph); bsT = Buf("sT")
        srep = S_("srep", [128, KD, 128], F32, ph); bsrep = Buf("srep")
        bmT = S_("bmT", [128, 2 * KD], F32, ph); bbmT = Buf("bmT")
        gaT = S_("gaT", [128, KD], F32, ph); bgaT = Buf("gaT")
        brow = S_("brow", [1, 4 * D], F32, ph); bbrow = Buf("brow")
        modT = S_("modT", [128, 2 * KD, 2], F32, ph); bmodT = Buf("modT")
        wts = [S_("wm%d" % i, [128, KD, GC], F32, ph) for i in range(2)]; bwts = [Buf("wm%d" % i) for i in range(2)]
        evs = [S_("mev%d" % i, [128, GC], F32, ph) for i in range(2)]; bevs = [Buf("mev%d" % i) for i in range(2)]
        pss = [P_("mps%d" % i, [128, 512], F32, ph) for i in range(2)]; bpss = [Buf("mps%d" % i) for i in range(2)]
        sp.dma(lambda e: e.dma_start(out=cT[:, :, 0], in_=c_b.rearrange("o (k p) -> p (o k)", p=128), allow_slow_non_contiguous=True),
               k.db("c_b"), bcT, bcT)
        sp.dma(lambda e: e.dma_start(out=cT[:, :, 1], in_=c_ctx.rearrange("o (k p) -> p (o k)", p=128), allow_slow_non_contiguous=True),
               k.db("c_ctx"), bcT, bcT)
        sp.dma(lambda e: e.dma_start(out=bmT[:], in_=b_mod[:, 0:2 * D].rearrange("o (k p) -> p (o k)", p=128), allow_slow_non_contiguous=True),
               k.db("b_mod"), bbmT, bbmT)
        sp.dma(lambda e: e.dma_start(out=gaT[:], in_=g_attn.rearrange("o (k p) -> p (o k)", p=128), allow_slow_non_contiguous=True),
               k.db("g_attn"), bgaT, bgaT)
        sp.dma(lambda e: e.dma_start(out=brow[:], in_=b_mod[:, 2 * D:6 * D]), k.db("b_mod"), bbrow, bbrow)
        act.op(lambda e: e.activation(out=sT[:], in_=cT[:], func=AF.Silu), reads=[bcT], writes=[bsT])
        for kc in range(KD):
            dve.op(lambda e: e.tensor_copy(out=srep[:, kc, :], in_=sT[:, kc, 0:1].to_broadcast([128, 128])),
                   reads=[bsT], writes=[bsrep], partial=True)
        ng = 6 * D // GC
        for g in range(ng):
            wt = wts[g % 2]; bwt = bwts[g % 2]; ps = pss[g % 2]; bps = bpss[g % 2]
            pi_ = (g * GC) // (3 * D); pc0 = g * GC - pi_ * 3 * D
            sp.dma(lambda e: e.dma_start(out=wt[:], in_=w_mod_p[pi_][:, pc0:pc0 + GC].rearrange("(k p) n -> p k n", p=128)),
                   k.db("w_mod_%d" % pi_), bwt, bwt, partial=False)
            if g * GC < 2 * D:
                for j in range(GC // 128):
                    for kc in range(KD):
                        pe.op(lambda e: e.matmul(ps[:, j * 2:j * 2 + 2], lhsT=wt[:, kc, j * 128:(j + 1) * 128], rhs=sT[:, kc, :],
                                                 start=(kc == 0), stop=(kc == KD - 1)),
                              reads=[bwt, bsT], writes=[bps], partial=(kc > 0 or j > 0), signal=(kc == KD - 1 and j == GC // 128 - 1))
                ch0 = g * GC // 128
                dve.op(lambda e: e.tensor_tensor(out=modT[:, ch0:ch0 + GC // 128, :],
                                                 in0=ps[:, 0:2 * (GC // 128)].rearrange("p (j t) -> p j t", t=2),
                                                 in1=bmT[:, ch0:ch0 + GC // 128].unsqueeze(2).to_broadcast([128, GC // 128, 2]), op=ALU.add),
                       reads=[bps, bbmT], writes=[bmodT], partial=True)
            else:
                ev = evs[g % 2]; bev = bevs[g % 2]
                c0 = g * GC - 2 * D
                for kc in range(KD):
                    pe.op(lambda e: e.matmul(ps[:, 0:GC], lhsT=srep[:, kc, :], rhs=wt[:, kc, :], start=(kc == 0), stop=False),
                          reads=[bwt, bsrep], writes=[bps], partial=(kc > 0), signal=False)
                pe.op(lambda e: e.matmul(ps[:, 0:GC], lhsT=ones_f[0:1, :], rhs=brow[0:1, c0:c0 + GC], start=False, stop=True),
                      reads=[b_ones, bbrow], writes=[bps], partial=True)
                dve.op(lambda e: e.tensor_copy(out=ev[:], in_=ps[:, 0:GC]), reads=[bps], writes=[bev])
                sp.dma(lambda e: e.dma_start(out=mod_bc[:, c0:c0 + GC], in_=ev[:]), bev, k.db("mod_bc"), bev)
        dve.op(lambda e: e.tensor_scalar(out=A1T[:], in0=modT[:, KD:2 * KD, :], scalar1=1.0, scalar2=None, op0=ALU.add),
               reads=[bmodT], writes=[bA1T])
        dve.op(lambda e: e.tensor_tensor(out=A1T[:], in0=A1T[:], in1=gaT[:].unsqueeze(2).to_broadcast([128, KD, 2]), op=ALU.mult),
               reads=[bA1T, bgaT], writes=[bA1T])
        dve.op(lambda e: e.tensor_copy(out=B1T[:], in_=modT[:, 0:KD, :]), reads=[bmodT], writes=[bB1T])
        if "A1T" in debug_outs:
            o1 = nc.dram_tensor("dbg_A1T", [128, KD * 2], F32, kind="ExternalOutput").ap(); k.dbufs["dbg_A1T"] = Buf("dbg_A1T")
            o2 = nc.dram_tensor("dbg_B1T", [128, KD * 2], F32, kind="ExternalOutput").ap(); k.dbufs["dbg_B1T"] = Buf("dbg_B1T")
            sp.dma(lambda e: e.dma_start(out=o1, in_=A1T[:].rearrange("p k t -> p (k t)")), bA1T, k.db("dbg_A1T"), bA1T)
            sp.dma(lambda e: e.dma_start(out=o2, in_=B1T[:].rearrange("p k t -> p (k t)")), bB1T, k.db("dbg_B1T"), bB1T)
        sc.barrier()
    if upto <= 1:
        return

    class Ring:
        def __init__(self, name, shape, dt, n, st, psum=False):
            mk = P_ if psum else S_
            self.t = [mk("%s%d" % (name, i), shape, dt, st) for i in range(n)]
            self.b = [Buf("%s%d" % (name, i)) for i in range(n)]
            self.i = -1
        def next(self):
            self.i = (self.i + 1) % len(self.t)
            return self.t[self.i], self.b[self.i]

    ident = S_("ident", [128, 128], BF16, top); b_ident = Buf("ident")
    identf = S_("identf", [128, 128], F32, top); b_identf = Buf("identf")
    pool.op(lambda e: e.memset(identf[:], 0.0), writes=[b_identf])
    pool.op(lambda e: e.affine_select(out=identf[:], in_=ones_f[:], pattern=[[-1, 128]], compare_op=ALU.is_equal, fill=0.0,
                                      base=0, channel_multiplier=1), reads=[b_ones], writes=[b_identf])
    dve.op(lambda e: e.tensor_copy(out=ident[:], in_=identf[:]), reads=[b_identf], writes=[b_ident])
    ones_b = S_("ones_b", [128, 128], BF16, top); b_onesb = Buf("ones_b")
    dve.op(lambda e: e.memset(ones_b[:], 1.0), writes=[b_onesb])

    hT_d = scratch("hT_d", [KD, 128, NK], BF16)
    with ExitStack() as ph:
        xts = Ring("xt", [128, D], F32, 2, ph)
        xns = Ring("xn", [128, D], BF16, 2, ph)
        junk = S_("junk", [128, D], BF16, ph); bjunk = Buf("junk")
        sss = Ring("ss", [128, 2], F32, 2, ph)
        hts = Ring("hts", [128, KD, 128], BF16, 2, ph)
        pst = Ring("pst", [128, 1024], BF16, 2, ph, psum=True)
        import os as _os
        for t in range(int(_os.environ.get('P2T', NK // 128))):
            col = 0 if t * 128 < S else 1
            xt, bxt = xts.next(); xn, bxn = xns.next(); ss, bss = sss.next(); ht, bht = hts.next()
            sp.dma(lambda e: e.dma_start(out=xt[:], in_=xk[t * 128:(t + 1) * 128, :]), k.db("xk"), bxt, bxt, partial=False)
            act.op(lambda e: e.activation(out=junk[:], in_=xt[:], func=AF.Square, accum_out=ss[:, 0:1]), reads=[bxt], writes=[bjunk, bss])
            dve.op(lambda e: e.tensor_scalar(out=ss[:, 1:2], in0=ss[:, 0:1], scalar1=1.0 / D, scalar2=1e-6, op0=ALU.mult, op1=ALU.add),
                   reads=[bss], writes=[bss])
            act.op(lambda e: e.activation(out=ss[:, 1:2], in_=ss[:, 1:2], func=AF.Ln), reads=[bss], writes=[bss]); act.op(lambda e: e.activation(out=ss[:, 1:2], in_=ss[:, 1:2], func=AF.Exp, scale=-0.5), reads=[bss], writes=[bss])
            dve.op(lambda e: e.tensor_scalar(out=xn[:], in0=xt[:], scalar1=ss[:, 1:2], scalar2=None, op0=ALU.mult), reads=[bxt, bss], writes=[bxn])
            _stg = int(_os.environ.get('P2STAGE', 5))
            for g0 in range(0, KD if _stg >= 3 else 0, 8):
                ps, bps = pst.next()
                ng_ = min(8, KD - g0)
                for j in range(ng_):
                    kc = g0 + j
                    pe.op(lambda e: e.transpose(out=ps[:, j * 128:(j + 1) * 128], in_=xn[:, kc * 128:(kc + 1) * 128], identity=ident[:]),
                          reads=[bxn, b_ident], writes=[bps], partial=(j > 0), signal=(j == ng_ - 1))
                for j in range(ng_ if _stg >= 4 else 0):
                    kc = g0 + j
                    if True:
                        dve.op(lambda e: e.tensor_scalar(out=ht[:, kc, :], in0=ps[:, j * 128:(j + 1) * 128], scalar1=A1T[:, kc, col:col + 1],
                                                         scalar2=B1T[:, kc, col:col + 1], op0=ALU.mult, op1=ALU.add),
                               reads=[bps, bA1T, bB1T], writes=[bht], partial=True)
                    else:
                        act.op(lambda e: e.activation(out=ht[:, kc, :], in_=ps[:, j * 128:(j + 1) * 128], func=AF.Identity,
                                                      scale=A1T[:, kc, col:col + 1], bias=B1T[:, kc, col:col + 1]),
                               reads=[bps, bA1T, bB1T], writes=[bht], partial=True)
            if _stg >= 5:
                sp.dma(lambda e: e.dma_start(out=hT_d[:, :, t * 128:(t + 1) * 128].rearrange("k p n -> p k n"), in_=ht[:]),
                       bht, k.db("hT_d"), bht)
        sc.barrier()
    if upto <= 2:
        return

    cosd = inp("cosd", [128, NK]); sind = inp("sind", [128, NK]); cosm = inp("cosm", [64, NK]); sinm = inp("sinm", [64, NK])
    DAH, MH, QL, KVL = c.DAH, c.MH, c.QL, c.KVL
    QT_d = scratch("QT_d", [2 * DAH, 128, NQ], BF16); KT_d = scratch("KT_d", [2 * DAH, 128, NK], BF16)
    V_d = scratch("V_d", [NK, DAH * 256], BF16)
    CQ_d = scratch("CQ_d", [QL // 128, 128, NQ], F32); CKV_d = scratch("CKV_d", [KVL // 128, 128, NK], F32)
    KR_d = scratch("KR_d", [64, NK], BF16)
    GA_d = scratch("GA_d", [KD, 128, NQ], BF16); GB_d = scratch("GB_d", [KD, 128, NQ], BF16)

    def rope_evac(st, ps, bps, M, t0, tw, cos_d, sin_d, cname, sname, dst_ap, dst_name, rings):
        ct, bct = rings["cos"].next(); sn, bsn = rings["sin"].next()
        t1, bt1 = rings["t1"].next(); t2, bt2 = rings["t2"].next(); ob, bob = rings["ob"].next()
        h = M // 2
        sp.dma(lambda e: e.dma_start(out=ct[0:M, 0:tw], in_=cos_d[0:M, t0:t0 + tw]), k.db(cname), bct, bct, partial=False)
        sp.dma(lambda e: e.dma_start(out=sn[0:M, 0:tw], in_=sin_d[0:M, t0:t0 + tw]), k.db(sname), bsn, bsn, partial=False)
        dve.op(lambda e: e.tensor_tensor(out=t1[0:M, 0:tw], in0=ps[0:M, 0:tw], in1=ct[0:M, 0:tw], op=ALU.mult), reads=[bps, bct], writes=[bt1])
        dve.op(lambda e: e.tensor_tensor(out=t2[0:h, 0:tw], in0=ps[h:M, 0:tw], in1=sn[0:h, 0:tw], op=ALU.mult), reads=[bps, bsn], writes=[bt2])
        dve.op(lambda e: e.tensor_tensor(out=t2[h:M, 0:tw], in0=ps[0:h, 0:tw], in1=sn[h:M, 0:tw], op=ALU.mult), reads=[bps, bsn], writes=[bt2], partial=True)
        pool.op(lambda e: e.tensor_tensor(out=ob[0:M, 0:tw], in0=t1[0:M, 0:tw], in1=t2[0:M, 0:tw], op=ALU.add), reads=[bt1, bt2], writes=[bob])
        act.dma(lambda e: e.dma_start(out=dst_ap, in_=ob[0:M, 0:tw]), bob, k.db(dst_name), bob)

    def mk_rope_rings(st):
        return dict(cos=Ring("rcos", [128, 512], F32, 2, st), sin=Ring("rsin", [128, 512], F32, 2, st),
                    t1=Ring("rt1", [128, 512], F32, 2, st), t2=Ring("rt2", [128, 512], F32, 2, st),
                    ob=Ring("rob", [128, 512], BF16, 3, st))

    with ExitStack() as ph:
        wgs = Ring("wg", [128, KD, 512], BF16, 2, ph)
        hxs = Ring("hx", [128, KD, 512], BF16, 2, ph)
        pss = Ring("pj", [128, 512], F32, 6, ph, psum=True)
        rr = mk_rope_rings(ph)
        evf = Ring("evf", [128, 512], F32, 3, ph)
        evb = Ring("evb", [128, 512], BF16, 3, ph)
        segs = [(c.o_dq, c.DAW, NQ, "dq"), (c.o_dk, c.DAW, NK, "dk"), (c.o_dv, c.DAW, NK, "dv"),
                (c.o_cq, QL, NQ, "cq"), (c.o_ckv, KVL + 64, NK, "ckv"), (c.o_ga, D, NQ, "ga"), (c.o_gb, D, NQ, "gb")]
        groups = []
        for (c0, ncol, ntok, kind) in segs:
            for g0 in range(0, ncol, 512):
                groups.append((c0, g0, min(512, ncol - g0), ntok, kind))
        wl = {}
        def loadW(gi):
            if gi < len(groups) and gi not in wl:
                c0, g0, gw, ntok, kind = groups[gi]
                wg, bwg = wgs.next()
                pool.dma(lambda e: e.dma_start(out=wg[:, :, 0:gw], in_=w_in[:, c0 + g0:c0 + g0 + gw].rearrange("(k p) n -> p k n", p=128)),
                         k.db("w_in"), bwg, bwg, partial=False)
                wl[gi] = (wg, bwg)
        items = [(gi, t0) for gi, g in enumerate(groups) for t0 in range(0, g[3], 512)]
        hl = {}
        def loadH(ii):
            if ii < len(items) and ii not in hl:
                gi, t0 = items[ii]
                tw = min(512, groups[gi][3] - t0)
                hx, bhx = hxs.next()
                sp.dma(lambda e: e.dma_start(out=hx[:, :, 0:tw], in_=hT_d[:, :, t0:t0 + tw].rearrange("k p n -> p k n")),
                       k.db("hT_d"), bhx, bhx, partial=False)
                hl[ii] = (hx, bhx)
        loadW(0); loadH(0)
        for ii, (gi, t0) in enumerate(items):
            c0, g0, gw, ntok, kind = groups[gi]
            tw = min(512, ntok - t0)
            if t0 == 0:
                loadW(gi + 1)
            loadH(ii + 1)
            wg, bwg = wl[gi]; hx, bhx = hl[ii]
            if kind == "dv":
                for tt in range(tw // 128):
                    ps, bps = pss.next()
                    for kc in range(KD):
                        pe.op(lambda e: e.matmul(ps[:, 0:gw], lhsT=hx[:, kc, tt * 128:(tt + 1) * 128], rhs=wg[:, kc, 0:gw],
                                                 start=(kc == 0), stop=(kc == KD - 1)),
                              reads=[bhx, bwg], writes=[bps], partial=(kc > 0), signal=(kc == KD - 1))
                    ob, bob = evb.next()
                    act.op(lambda e: e.copy(out=ob[:, 0:gw], in_=ps[:, 0:gw]), reads=[bps], writes=[bob])
                    r0 = t0 + tt * 128
                    act.dma(lambda e: e.dma_start(out=V_d[r0:r0 + 128, g0:g0 + gw], in_=ob[:, 0:gw]), bob, k.db("V_d"), bob)
                continue
            nch = (gw + 127) // 128
            for j in range(nch):
                M = min(128, gw - j * 128)
                ch = (g0 + j * 128) // 128
                ps, bps = pss.next()
                for kc in range(KD):
                    pe.op(lambda e: e.matmul(ps[0:M, 0:tw], lhsT=wg[:, kc, j * 128:j * 128 + M], rhs=hx[:, kc, 0:tw],
                                             start=(kc == 0), stop=(kc == KD - 1)),
                          reads=[bhx, bwg], writes=[bps], partial=(kc > 0), signal=(kc == KD - 1))
                if kind == "dq":
                    rope_evac(ph, ps, bps, 128, t0, tw, cosd, sind, "cosd", "sind", QT_d[ch, :, t0:t0 + tw], "QT_d", rr)
                elif kind == "dk":
                    rope_evac(ph, ps, bps, 128, t0, tw, cosd, sind, "cosd", "sind", KT_d[ch, :, t0:t0 + tw], "KT_d", rr)
                elif kind == "ckv" and M == 64:
                    rope_evac(ph, ps, bps, 64, t0, tw, cosm, sinm, "cosm", "sinm", KR_d[:, t0:t0 + tw], "KR_d", rr)
                elif kind in ("cq", "ckv"):
                    ob, bob = evf.next()
                    dve.op(lambda e: e.tensor_copy(out=ob[:, 0:tw], in_=ps[:, 0:tw]), reads=[bps], writes=[bob])
                    dst, dn = (CQ_d, "CQ_d") if kind == "cq" else (CKV_d, "CKV_d")
                    act.dma(lambda e: e.dma_start(out=dst[ch, :, t0:t0 + tw], in_=ob[:, 0:tw]), bob, k.db(dn), bob)
                else:
                    ob, bob = evb.next()
                    act.op(lambda e: e.activation(out=ob[:, 0:tw], in_=ps[:, 0:tw], func=AF.Sigmoid), reads=[bps], writes=[bob])
                    dst, dn = (GA_d, "GA_d") if kind == "ga" else (GB_d, "GB_d")
                    act.dma(lambda e: e.dma_start(out=dst[ch, :, t0:t0 + tw], in_=ob[:, 0:tw]), bob, k.db(dn), bob)
        sc.barrier()
    if upto <= 3:
        return

    g_q = inp("g_q", [1, QL]); g_kv = inp("g_kv", [1, KVL])
    QN_d = scratch("QN_d", [MH, 128, NQ], BF16); QR_d = scratch("QR_d", [MH, 64, NQ], BF16)
    KN_d = scratch("KN_d", [MH, 128, NK], BF16); MV_d = scratch("MV_d", [NK, MH * 128], BF16)

    def latent_up(lat_d, lat_name, nch, ntok, g_ap, g_name, W, Wname, wcols, emit):
        with ExitStack() as ph:
            wsb = S_("lw", [128, nch, wcols], BF16, ph); bw = Buf("lw")
            pool.dma(lambda e: e.dma_start(out=wsb[:], in_=W.rearrange("(k p) n -> p k n", p=128)), k.db(Wname), bw, bw, partial=False)
            gT = S_("lg", [128, nch], F32, ph); bg = Buf("lg")
            sp.dma(lambda e: e.dma_start(out=gT[:], in_=g_ap.rearrange("o (k p) -> p (o k)", p=128), allow_slow_non_contiguous=True),
                   k.db(g_name), bg, bg)
            Ls = Ring("lL", [128, nch, 512], F32, 2, ph)
            sq = S_("lsq", [128, nch, 512], F32, ph); bsq = Buf("lsq")
            Lns = Ring("lLn", [128, nch, 512], BF16, 2, ph)
            rs = Ring("lrs", [128, 512], F32, 2, ph)
            pss = Ring("lps", [128, 512], F32, 5, ph, psum=True)
            rr = mk_rope_rings(ph)
            evb = Ring("levb", [128, 512], BF16, 3, ph)
            for t0 in range(0, ntok, 512):
                tw = min(512, ntok - t0)
                L, bL = Ls.next(); Ln, bLn = Lns.next(); r_, br = rs.next()
                sp.dma(lambda e: e.dma_start(out=L[:, :, 0:tw], in_=lat_d[:, :, t0:t0 + tw].rearrange("k p n -> p k n")),
                       k.db(lat_name), bL, bL, partial=False)
                act.op(lambda e: e.activation(out=sq[:, :, 0:tw], in_=L[:, :, 0:tw], func=AF.Square), reads=[bL], writes=[bsq])
                ps, bps = pss.next()
                for kc in range(nch):
                    pe.op(lambda e: e.matmul(ps[:, 0:tw], lhsT=ones_f[:], rhs=sq[:, kc, 0:tw], start=(kc == 0), stop=(kc == nch - 1)),
                          reads=[b_ones, bsq], writes=[bps], partial=(kc > 0), signal=(kc == nch - 1))
                dve.op(lambda e: e.tensor_scalar(out=r_[:, 0:tw], in0=ps[:, 0:tw], scalar1=1.0 / (nch * 128), scalar2=1e-6, op0=ALU.mult, op1=ALU.add),
                       reads=[bps], writes=[br])
                act.op(lambda e: e.activation(out=r_[:, 0:tw], in_=r_[:, 0:tw], func=AF.Ln), reads=[br], writes=[br]); act.op(lambda e: e.activation(out=r_[:, 0:tw], in_=r_[:, 0:tw], func=AF.Exp, scale=-0.5), reads=[br], writes=[br])
                for kc in range(nch):
                    dve.op(lambda e: e.scalar_tensor_tensor(out=Ln[:, kc, 0:tw], in0=L[:, kc, 0:tw], scalar=gT[:, kc:kc + 1], in1=r_[:, 0:tw],
                                                            op0=ALU.mult, op1=ALU.mult),
                           reads=[bL, bg, br], writes=[bLn], partial=(kc > 0))
                emit(Ln, bLn, wsb, bw, t0, tw, nch, pss, rr, evb)
            sc.barrier()

    def fm_head(Ln, bLn, wsb, bw, nch, col0, M, tw, pss):
        ps, bps = pss.next()
        for kc in range(nch):
            pe.op(lambda e: e.matmul(ps[0:M, 0:tw], lhsT=wsb[:, kc, col0:col0 + M], rhs=Ln[:, kc, 0:tw], start=(kc == 0), stop=(kc == nch - 1)),
                  reads=[bw, bLn], writes=[bps], partial=(kc > 0), signal=(kc == nch - 1))
        return ps, bps

    def emit_q(Ln, bLn, wsb, bw, t0, tw, nch, pss, rr, evb):
        for h in range(MH):
            ps, bps = fm_head(Ln, bLn, wsb, bw, nch, h * 192, 128, tw, pss)
            ob, bob = evb.next()
            act.op(lambda e: e.copy(out=ob[:, 0:tw], in_=ps[:, 0:tw]), reads=[bps], writes=[bob])
            act.dma(lambda e: e.dma_start(out=QN_d[h, :, t0:t0 + tw], in_=ob[:, 0:tw]), bob, k.db("QN_d"), bob)
            ps, bps = fm_head(Ln, bLn, wsb, bw, nch, h * 192 + 128, 64, tw, pss)
            rope_evac(None, ps, bps, 64, t0, tw, cosm, sinm, "cosm", "sinm", QR_d[h, :, t0:t0 + tw], "QR_d", rr)

    def emit_kv(Ln, bLn, wsb, bw, t0, tw, nch, pss, rr, evb):
        for h in range(MH):
            ps, bps = fm_head(Ln, bLn, wsb, bw, nch, h * 256, 128, tw, pss)
            ob, bob = evb.next()
            act.op(lambda e: e.copy(out=ob[:, 0:tw], in_=ps[:, 0:tw]), reads=[bps], writes=[bob])
            act.dma(lambda e: e.dma_start(out=KN_d[h, :, t0:t0 + tw], in_=ob[:, 0:tw]), bob, k.db("KN_d"), bob)
        for tt in range(tw // 128):
            for h0 in range(0, MH, 4):
                nh = min(4, MH - h0)
                ps, bps = pss.next()
                for kc in range(nch):
                    pe.op(lambda e: e.matmul(ps[:, 0:nh * 128].rearrange("p (h e) -> p h e", e=128), lhsT=Ln[:, kc, tt * 128:(tt + 1) * 128],
                                             rhs=wsb[:, kc, :].rearrange("p (h e) -> p h e", e=256)[:, h0:h0 + nh, 128:256],
                                             start=(kc == 0), stop=(kc == nch - 1)),
                          reads=[bw, bLn], writes=[bps], partial=(kc > 0), signal=(kc == nch - 1))
                ob, bob = evb.next()
                dve.op(lambda e: e.tensor_copy(out=ob[:, 0:nh * 128], in_=ps[:, 0:nh * 128]), reads=[bps], writes=[bob])
                r0 = t0 + tt * 128
                act.dma(lambda e: e.dma_start(out=MV_d[r0:r0 + 128, h0 * 128:(h0 + nh) * 128], in_=ob[:, 0:nh * 128]), bob, k.db("MV_d"), bob)

    latent_up(CQ_d, "CQ_d", QL // 128, NQ, g_q, "g_q", w_uq, "w_uq", MH * 192, emit_q)
    latent_up(CKV_d, "CKV_d", KVL // 128, NK, g_kv, "g_kv", w_ukv, "w_ukv", MH * 256, emit_kv)
    if upto <= 4:
        return

    OA_d = scratch("OA_d", [c.DAW // 128, 128, NQ], BF16); OB_d = scratch("OB_d", [c.MW // 128, 128, NQ], BF16)
    g_subln = inp("g_subln", [1, 256])
    lam_in = [inp(n, [1, 128]) for n in ("lam_q1", "lam_k1", "lam_q2", "lam_k2")]
    NKC = NK // 128

    def attn_branch(H, ncomp, E, scale, k_parts, q_parts, v_src, finalize, extra=None):
        with ExitStack() as ph:
            np_ = len(k_parts(0, 0))
            kts = Ring("akt", [128, ncomp * np_, NK], BF16, 2, ph)
            vts = Ring("avt", [128, NKC, E + 1], BF16, 2, ph)
            for i in range(2):
                pool.op(lambda e: e.memset(vts.t[i][:, :, E:E + 1], 1.0), writes=[vts.b[i]])
            qts = Ring("aqt", [128, ncomp * np_, 512], BF16, 2, ph)
            pts = Ring("apt", [128, 1024], BF16, 3, ph)
            ops_ = Ring("aop", [128, 512], F32, 4, ph, psum=True)
            sps = Ring("asp", [128, 1024], F32, 2, ph, psum=True)
            ctx_ = extra(ph, sps) if extra else None
            for h in range(H):
                kt, bkt = kts.next(); vt, bvt = vts.next()
                for comp in range(ncomp):
                    for pi, (ap_, nm, Kr) in enumerate(k_parts(h, comp)):
                        sp.dma(lambda e: e.dma_start(out=kt[0:Kr, comp * np_ + pi, :], in_=ap_), k.db(nm), bkt, bkt, partial=(comp + pi > 0))
                vap, vnm = v_src(h)
                sp.dma(lambda e: e.dma_start(out=vt[:, :, 0:E], in_=vap.rearrange("(k p) e -> p k e", p=128)), k.db(vnm), bvt, bvt)
                for q0 in range(0, NQ, 512):
                    tw = min(512, NQ - q0); nqs = tw // 128
                    qt, bqt = qts.next()
                    for comp in range(ncomp):
                        for pi, (ap_, nm, Kr) in enumerate(q_parts(h, comp)):
                            sp.dma(lambda e: e.dma_start(out=qt[0:Kr, comp * np_ + pi, 0:tw], in_=ap_[:, q0:q0 + tw]), k.db(nm), bqt, bqt,
                                   partial=(comp + pi > 0))
                    for comp in range(ncomp):
                        Os = [ops_.next() for _ in range(nqs)]
                        parts = k_parts(h, comp)
                        for kc0 in range(0, NKC, 2):
                            n2 = min(2, NKC - kc0)
                            sp_, bsp = sps.next()
                            for j in range(n2):
                                for pi, (_, _, Kr) in enumerate(parts):
                                    pe.op(lambda e: e.matmul(sp_[:, j * 512:j * 512 + tw], lhsT=kt[0:Kr, comp * np_ + pi, (kc0 + j) * 128:(kc0 + j + 1) * 128],
                                                             rhs=qt[0:Kr, comp * np_ + pi, 0:tw], start=(pi == 0), stop=(pi == np_ - 1)),
                                          reads=[bkt, bqt], writes=[bsp], partial=(j + pi > 0), signal=(j == n2 - 1 and pi == np_ - 1))
                            pt, bpt = pts.next()
                            if tw == 512:
                                act.op(lambda e: e.activation(out=pt[:, 0:n2 * 512], in_=sp_[:, 0:n2 * 512], func=AF.Exp, scale=scale), reads=[bsp], writes=[bpt])
                            else:
                                for j in range(n2):
                                    act.op(lambda e: e.activation(out=pt[:, j * 512:j * 512 + tw], in_=sp_[:, j * 512:j * 512 + tw], func=AF.Exp, scale=scale),
                                           reads=[bsp], writes=[bpt], partial=(j > 0))
                            for j in range(n2):
                                kc = kc0 + j
                                for qs in range(nqs):
                                    O, bO = Os[qs]
                                    pe.op(lambda e: e.matmul(O[:, 0:E + 1], lhsT=pt[:, j * 512 + qs * 128:j * 512 + (qs + 1) * 128], rhs=vt[:, kc, 0:E + 1],
                                                             start=(kc == 0), stop=(kc == NKC - 1)),
                                          reads=[bpt, bvt], writes=[bO], partial=(kc > 0), signal=(kc == NKC - 1 or (j == n2 - 1 and qs == nqs - 1)))
                        finalize(ctx_, h, comp, q0, tw, Os, sps)
            sc.barrier()

    def store_T(ctx_, src_bf, bsrc, nE, dst_d, dst_name, ch0, q0, tw, sps):
        nqs = tw // 128
        for ec in range(nE):
            sp_, bsp = sps.next()
            pb = sp_[:].bitcast(BF16)
            for qs in range(nqs):
                pe.op(lambda e: e.transpose(out=pb[:, qs * 128:(qs + 1) * 128], in_=src_bf[:, qs, ec * 128:(ec + 1) * 128], identity=ident[:]),
                      reads=[bsrc, b_ident], writes=[bsp], partial=(qs > 0), signal=(qs == nqs - 1))
            ob, bob = ctx_["oT"].next()
            dve.op(lambda e: e.tensor_copy(out=ob[:, 0:tw], in_=pb[:, 0:tw]), reads=[bsp], writes=[bob])
            act.dma(lambda e: e.dma_start(out=dst_d[ch0 + ec, :, q0:q0 + tw], in_=ob[:, 0:tw]), bob, k.db(dst_name), bob)

    def da_extra(ph, sps):
        d = {}
        d["o0"] = S_("da_o0", [128, 4, 256], F32, ph); d["bo0"] = Buf("da_o0")
        d["o1"] = S_("da_o1", [128, 4, 256], F32, ph); d["bo1"] = Buf("da_o1")
        d["ob"] = S_("da_ob", [128, 4, 256], BF16, ph); d["bob"] = Buf("da_ob")
        d["jk"] = S_("da_jk", [128, 256], F32, ph); d["bjk"] = Buf("da_jk")
        d["rs"] = S_("da_rs", [128, 8], F32, ph); d["brs"] = Buf("da_rs")
        d["oT"] = Ring("da_oT", [128, 512], BF16, 3, ph)
        lv = S_("da_lv", [128, 4], F32, ph); blv = Buf("da_lv")
        for i in range(4):
            sp.dma(lambda e: e.dma_start(out=lv[:, i:i + 1], in_=lam_in[i].rearrange("o p -> p o"), allow_slow_non_contiguous=True),
                   k.db(("lam_q1", "lam_k1", "lam_q2", "lam_k2")[i]), blv, blv)
        lp = S_("da_lp", [128, 2], F32, ph); blp = Buf("da_lp")
        dve.op(lambda e: e.tensor_tensor(out=lp[:, 0:1], in0=lv[:, 0:1], in1=lv[:, 1:2], op=ALU.mult), reads=[blv], writes=[blp])
        dve.op(lambda e: e.tensor_tensor(out=lp[:, 1:2], in0=lv[:, 2:3], in1=lv[:, 3:4], op=ALU.mult), reads=[blv], writes=[blp], partial=True)
        with ExitStack() as tmp:
            ps, bps = sps.next()
            pe.op(lambda e: e.matmul(ps[:, 0:2], lhsT=ones_f[:], rhs=lp[:], start=True, stop=True), reads=[b_ones, blp], writes=[bps])
            le = S_("da_le", [128, 2], F32, ph); ble = Buf("da_le")
            act.op(lambda e: e.activation(out=le[:], in_=ps[:, 0:2], func=AF.Exp), reads=[bps], writes=[ble])
            nl = S_("da_nl", [128, 1], F32, ph); bnl = Buf("da_nl")
            dve.op(lambda e: e.tensor_tensor(out=nl[:], in0=le[:, 1:2], in1=le[:, 0:1], op=ALU.subtract), reads=[ble], writes=[bnl])
            dve.op(lambda e: e.tensor_scalar(out=nl[:], in0=nl[:], scalar1=-c.lambda_init, scalar2=None, op0=ALU.add), reads=[bnl], writes=[bnl])
            d["nl"] = nl; d["bnl"] = bnl
            gr = S_("da_gr", [1, 256], F32, ph); bgr = Buf("da_gr")
            sp.dma(lambda e: e.dma_start(out=gr[:], in_=g_subln), k.db("g_subln"), bgr, bgr)
            pe.op(lambda e: e.matmul(ps[:, 256:512], lhsT=ones_f[0:1, :], rhs=gr[0:1, :], start=True, stop=True), reads=[b_ones, bgr], writes=[bps])
            gb_ = S_("da_gb", [128, 256], F32, ph); bgb = Buf("da_gb")
            dve.op(lambda e: e.tensor_scalar(out=gb_[:], in0=ps[:, 256:512], scalar1=1.0 - c.lambda_init, scalar2=None, op0=ALU.mult),
                   reads=[bps], writes=[bgb])
            d["gb"] = gb_; d["bgb"] = bgb
            sc.barrier([pe, dve, act])
        return d

    def da_final(d, h, comp, q0, tw, Os, sps):
        nqs = tw // 128
        rs, brs = d["rs"], d["brs"]
        dst, bdst = (d["o0"], d["bo0"]) if comp == 0 else (d["o1"], d["bo1"])
        for qs in range(nqs):
            O, bO = Os[qs]
            dve.op(lambda e: e.reciprocal(out=rs[:, qs:qs + 1], in_=O[:, 256:257]), reads=[bO], writes=[brs], partial=(qs > 0))
            dve.op(lambda e: e.tensor_scalar(out=dst[:, qs, :], in0=O[:, 0:256], scalar1=rs[:, qs:qs + 1], scalar2=None, op0=ALU.mult),
                   reads=[bO, brs], writes=[bdst], partial=(qs > 0))
        if comp == 0:
            return
        o0, bo0, o1, bo1 = d["o0"], d["bo0"], d["o1"], d["bo1"]
        for qs in range(nqs):
            dve.op(lambda e: e.scalar_tensor_tensor(out=o0[:, qs, :], in0=o1[:, qs, :], scalar=d["nl"][:, 0:1], in1=o0[:, qs, :], op0=ALU.mult, op1=ALU.add),
                   reads=[bo1, bo0, d["bnl"]], writes=[bo0])
            act.op(lambda e: e.activation(out=d["jk"][:], in_=o0[:, qs, :], func=AF.Square, accum_out=rs[:, 4 + qs:5 + qs]),
                   reads=[bo0], writes=[d["bjk"], brs])
            dve.op(lambda e: e.tensor_scalar(out=rs[:, 4 + qs:5 + qs], in0=rs[:, 4 + qs:5 + qs], scalar1=1.0 / 256, scalar2=1e-6, op0=ALU.mult, op1=ALU.add),
                   reads=[brs], writes=[brs])
            act.op(lambda e: e.activation(out=rs[:, 4 + qs:5 + qs], in_=rs[:, 4 + qs:5 + qs], func=AF.Ln), reads=[brs], writes=[brs]); act.op(lambda e: e.activation(out=rs[:, 4 + qs:5 + qs], in_=rs[:, 4 + qs:5 + qs], func=AF.Exp, scale=-0.5), reads=[brs], writes=[brs])
            dve.op(lambda e: e.scalar_tensor_tensor(out=d["ob"][:, qs, :], in0=o0[:, qs, :], scalar=rs[:, 4 + qs:5 + qs], in1=d["gb"][:], op0=ALU.mult, op1=ALU.mult),
                   reads=[bo0, brs, d["bgb"]], writes=[d["bob"]], partial=(qs > 0))
        store_T(d, d["ob"], d["bob"], 2, OA_d, "OA_d", h * 2, q0, tw, sps)

    attn_branch(DAH, 2, 256, 128 ** -0.5,
                lambda h, comp: [(KT_d[comp * DAH + h], "KT_d", 128)],
                lambda h, comp: [(QT_d[comp * DAH + h], "QT_d", 128)],
                lambda h: (V_d[:, h * 256:(h + 1) * 256], "V_d"), da_final, da_extra)
    if upto <= 5:
        return

    def mla_extra(ph, sps):
        d = {}
        d["ob"] = S_("ml_ob", [128, 4, 128], BF16, ph); d["bob"] = Buf("ml_ob")
        d["rs"] = S_("ml_rs", [128, 4], F32, ph); d["brs"] = Buf("ml_rs")
        d["oT"] = Ring("ml_oT", [128, 512], BF16, 3, ph)
        return d

    def mla_final(d, h, comp, q0, tw, Os, sps):
        nqs = tw // 128
        rs, brs = d["rs"], d["brs"]
        for qs in range(nqs):
            O, bO = Os[qs]
            dve.op(lambda e: e.reciprocal(out=rs[:, qs:qs + 1], in_=O[:, 128:129]), reads=[bO], writes=[brs], partial=(qs > 0))
            dve.op(lambda e: e.tensor_scalar(out=d["ob"][:, qs, :], in0=O[:, 0:128], scalar1=rs[:, qs:qs + 1], scalar2=None, op0=ALU.mult),
                   reads=[bO, brs], writes=[d["bob"]], partial=(qs > 0))
        store_T(d, d["ob"], d["bob"], 1, OB_d, "OB_d", h, q0, tw, sps)

    attn_branch(MH, 1, 128, 192 ** -0.5,
                lambda h, comp: [(KN_d[h], "KN_d", 128), (KR_d, "KR_d", 64)],
                lambda h, comp: [(QN_d[h], "QN_d", 128), (QR_d[h], "QR_d", 64)],
                lambda h: (MV_d[:, h * 128:(h + 1) * 128], "MV_d"), mla_final, mla_extra)
    if upto <= 6:
        return

    NR = c.NG + c.NE; NE = c.NE; NG = c.NG; EPG = c.EPG; DE = c.DE; HC = DE // 128
    NT = NQ // 128; NBLK = 2 * NT + NE
    g_ffn = inp("g_ffn", [1, D]); w_r = inp("w_r", [D, NR]); b_r = inp("b_r", [1, NR]); g_final = inp("g_final", [1, D])
    MG_d = scratch("MG_d", [KD, 128, NQ], BF16); X1_d = scratch("X1_d", [NQ, D], F32); H2_d = scratch("H2_d", [NQ, D], BF16)
    Xs_d = scratch("Xs_d", [NBLK * 128, D], BF16); Ys_d = scratch("Ys_d", [NBLK * 128, D], F32)
    KA = c.DAW // 128; KB = c.MW // 128

    with ExitStack() as ph:
        was = Ring("wa", [128, KA, 512], BF16, 2, ph); wbs = Ring("wb", [128, KB, 512], BF16, 2, ph)
        oas = Ring("oa", [128, KA, 512], BF16, 2, ph); obs = Ring("ob", [128, KB, 512], BF16, 2, ph)
        gas = Ring("gat", [128, 512], BF16, 2, ph); gbs = Ring("gbt", [128, 512], BF16, 2, ph)
        t1s = Ring("mt1", [128, 512], F32, 2, ph); t2s = Ring("mt2", [128, 512], F32, 2, ph); mgs = Ring("mgo", [128, 512], BF16, 3, ph)
        pss = Ring("mps", [128, 512], F32, 6, ph, psum=True)
        for g0 in range(0, D, 512):
            gw = min(512, D - g0)
            wa, bwa = was.next(); wb, bwb = wbs.next()
            pool.dma(lambda e: e.dma_start(out=wa[:, :, 0:gw], in_=w_br_a[:, g0:g0 + gw].rearrange("(k p) n -> p k n", p=128)), k.db("w_br_a"), bwa, bwa, partial=False)
            pool.dma(lambda e: e.dma_start(out=wb[:, :, 0:gw], in_=w_br_b[:, g0:g0 + gw].rearrange("(k p) n -> p k n", p=128)), k.db("w_br_b"), bwb, bwb, partial=False)
            for t0 in range(0, NQ, 512):
                tw = min(512, NQ - t0)
                oa, boa = oas.next(); ob, bob = obs.next()
                sp.dma(lambda e: e.dma_start(out=oa[:, :, 0:tw], in_=OA_d[:, :, t0:t0 + tw].rearrange("k p n -> p k n")), k.db("OA_d"), boa, boa, partial=False)
                sp.dma(lambda e: e.dma_start(out=ob[:, :, 0:tw], in_=OB_d[:, :, t0:t0 + tw].rearrange("k p n -> p k n")), k.db("OB_d"), bob, bob, partial=False)
                for j in range(gw // 128):
                    dc = (g0 + j * 128) // 128
                    psa, bpa = pss.next(); psb, bpb = pss.next()
                    for kc in range(KA):
                        pe.op(lambda e: e.matmul(psa[:, 0:tw], lhsT=wa[:, kc, j * 128:(j + 1) * 128], rhs=oa[:, kc, 0:tw], start=(kc == 0), stop=(kc == KA - 1)),
                              reads=[bwa, boa], writes=[bpa], partial=(kc > 0), signal=(kc == KA - 1))
                    for kc in range(KB):
                        pe.op(lambda e: e.matmul(psb[:, 0:tw], lhsT=wb[:, kc, j * 128:(j + 1) * 128], rhs=ob[:, kc, 0:tw], start=(kc == 0), stop=(kc == KB - 1)),
                              reads=[bwb, bob], writes=[bpb], partial=(kc > 0), signal=(kc == KB - 1))
                    ga, bga = gas.next(); gb, bgb = gbs.next()
                    sp.dma(lambda e: e.dma_start(out=ga[:, 0:tw], in_=GA_d[dc, :, t0:t0 + tw]), k.db("GA_d"), bga, bga, partial=False)
                    sp.dma(lambda e: e.dma_start(out=gb[:, 0:tw], in_=GB_d[dc, :, t0:t0 + tw]), k.db("GB_d"), bgb, bgb, partial=False)
                    t1, bt1 = t1s.next(); t2, bt2 = t2s.next(); mg, bmg = mgs.next()
                    dve.op(lambda e: e.tensor_tensor(out=t1[:, 0:tw], in0=psa[:, 0:tw], in1=ga[:, 0:tw], op=ALU.mult), reads=[bpa, bga], writes=[bt1])
                    dve.op(lambda e: e.tensor_tensor(out=t2[:, 0:tw], in0=psb[:, 0:tw], in1=gb[:, 0:tw], op=ALU.mult), reads=[bpb, bgb], writes=[bt2])
                    pool.op(lambda e: e.tensor_tensor(out=mg[:, 0:tw], in0=t1[:, 0:tw], in1=t2[:, 0:tw], op=ALU.add), reads=[bt1, bt2], writes=[bmg])
                    act.dma(lambda e: e.dma_start(out=MG_d[dc, :, t0:t0 + tw], in_=mg[:, 0:tw]), bmg, k.db("MG_d"), bmg)
        sc.barrier()
    if upto <= 7:
        return

    with ExitStack() as ph:
        wos = Ring("wo", [128, KD, 512], BF16, 2, ph); mgt = Ring("mgt", [128, KD, 128], BF16, 3, ph)
        gts = Ring("gt1", [128, 512], F32, 2, ph); xs = Ring("xr", [128, 512], F32, 3, ph)
        t1s = Ring("ot1", [128, 512], F32, 2, ph); x1s = Ring("x1o", [128, 512], F32, 3, ph)
        pss = Ring("ops", [128, 512], F32, 4, ph, psum=True)
        for g0 in range(0, D, 512):
            gw = min(512, D - g0)
            wo, bwo = wos.next(); gt, bgt = gts.next()
            pool.dma(lambda e: e.dma_start(out=wo[:, :, 0:gw], in_=w_out[:, g0:g0 + gw].rearrange("(k p) n -> p k n", p=128)), k.db("w_out"), bwo, bwo, partial=False)
            sp.dma(lambda e: e.dma_start(out=gt[:, 0:gw], in_=mod_bc[:, g0:g0 + gw]), k.db("mod_bc"), bgt, bgt, partial=False)
            for tt in range(NT):
                mg, bmg = mgt.next(); xr, bxr = xs.next()
                sp.dma(lambda e: e.dma_start(out=mg[:], in_=MG_d[:, :, tt * 128:(tt + 1) * 128].rearrange("k p n -> p k n")), k.db("MG_d"), bmg, bmg, partial=False)
                sp.dma(lambda e: e.dma_start(out=xr[:, 0:gw], in_=xk[tt * 128:(tt + 1) * 128, g0:g0 + gw]), k.db("xk"), bxr, bxr, partial=False)
                ps, bps = pss.next()
                for kc in range(KD):
                    pe.op(lambda e: e.matmul(ps[:, 0:gw], lhsT=mg[:, kc, :], rhs=wo[:, kc, 0:gw], start=(kc == 0), stop=(kc == KD - 1)),
                          reads=[bmg, bwo], writes=[bps], partial=(kc > 0), signal=(kc == KD - 1))
                t1, bt1 = t1s.next(); x1, bx1 = x1s.next()
                dve.op(lambda e: e.tensor_tensor(out=t1[:, 0:gw], in0=ps[:, 0:gw], in1=gt[:, 0:gw], op=ALU.mult), reads=[bps, bgt], writes=[bt1])
                pool.op(lambda e: e.tensor_tensor(out=x1[:, 0:gw], in0=t1[:, 0:gw], in1=xr[:, 0:gw], op=ALU.add), reads=[bt1, bxr], writes=[bx1])
                act.dma(lambda e: e.dma_start(out=X1_d[tt * 128:(tt + 1) * 128, g0:g0 + gw], in_=x1[:, 0:gw]), bx1, k.db("X1_d"), bx1)
        sc.barrier()
    if upto <= 8:
        return

    moe = ExitStack(); top.enter_context(moe)
    Ind_all = S_("Ind_all", [128, NT, NE], BF16, moe); bInd = Buf("Ind_all")
    I1_all = S_("I1_all", [128, NT, NE], F32, moe); bI1 = Buf("I1_all")
    I2_all = S_("I2_all", [128, NT, NE], F32, moe); bI2 = Buf("I2_all")
    wsel = S_("wsel", [128, NT, 2], F32, moe); bwsel = Buf("wsel")
    idx_all = S_("idx_all", [128, NT, 2], I32, moe); bidx = Buf("idx_all")
    NSP_ = max(1, (KD * DE * 4 + 32767) // 32768)
    widx_i = S_("widx_i", [128, 4 * NSP_, NBLK], I32, moe); bwidx = Buf("widx_i")
    Uf = S_("Uf", [128, 128], F32, moe); bUf = Buf("Uf")
    Ub = S_("Ub", [128, 128], BF16, moe); bUb = Buf("Ub")
    jji = S_("jji", [128, 128], I32, moe); bjji = Buf("jji")
    pool.op(lambda e: e.iota(out=jji[:], pattern=[[1, 128]], base=0, channel_multiplier=0), writes=[bjji])
    ppi = S_("ppi", [128, 1], I32, moe); bppi = Buf("ppi")
    pool.op(lambda e: e.iota(out=ppi[:], pattern=[[0, 1]], base=0, channel_multiplier=1), writes=[bppi])
    ppf = S_("ppf", [128, 1], F32, moe); bppf = Buf("ppf")
    dve.op(lambda e: e.tensor_copy(out=ppf[:], in_=ppi[:]), reads=[bppi], writes=[bppf])
    dve.op(lambda e: e.tensor_copy(out=Uf[:], in_=jji[:]), reads=[bjji], writes=[bUf])
    dve.op(lambda e: e.tensor_scalar(out=Uf[:], in0=Uf[:], scalar1=ppf[:, 0:1], scalar2=None, op0=ALU.is_gt), reads=[bUf, bppf], writes=[bUf])
    dve.op(lambda e: e.tensor_copy(out=Ub[:], in_=Uf[:]), reads=[bUf], writes=[bUb])

    def bcast_row(row_ap, row_name, dst, bdst, st, pss, post=None):
        rw = S_("bc_row", [1, D], F32, st); brw = Buf("bc_row")
        sp.dma(lambda e: e.dma_start(out=rw[:], in_=row_ap), k.db(row_name), brw, brw)
        for g0 in range(0, D, 512):
            gw = min(512, D - g0)
            ps, bps = pss.next()
            pe.op(lambda e: e.matmul(ps[:, 0:gw], lhsT=ones_f[0:1, :], rhs=rw[0:1, g0:g0 + gw], start=True, stop=True), reads=[b_ones, brw], writes=[bps])
            if post is None:
                dve.op(lambda e: e.tensor_copy(out=dst[:, g0:g0 + gw], in_=ps[:, 0:gw]), reads=[bps], writes=[bdst], partial=True)
            else:
                post(ps, bps, g0, gw)

    with ExitStack() as ph:
        A2 = S_("A2", [128, D], F32, ph); bA2 = Buf("A2"); B2 = S_("B2", [128, D], F32, ph); bB2 = Buf("B2")
        pss = Ring("rps", [128, 512], F32, 4, ph, psum=True)
        sp.dma(lambda e: e.dma_start(out=A2[:], in_=mod_bc[:, 2 * D:3 * D]), k.db("mod_bc"), bA2, bA2)
        sp.dma(lambda e: e.dma_start(out=B2[:], in_=mod_bc[:, D:2 * D]), k.db("mod_bc"), bB2, bB2)
        bcast_row(g_ffn, "g_ffn", A2, bA2, ph, pss,
                  post=lambda ps, bps, g0, gw: dve.op(lambda e: e.scalar_tensor_tensor(out=A2[:, g0:g0 + gw], in0=A2[:, g0:g0 + gw], scalar=1.0, in1=ps[:, 0:gw],
                                                                                       op0=ALU.add, op1=ALU.mult), reads=[bps, bA2], writes=[bA2]))
        wr = S_("wr", [128, KD, NR], F32, ph); bwr = Buf("wr")
        sp.dma(lambda e: e.dma_start(out=wr[:], in_=w_r.rearrange("(k p) n -> p k n", p=128)), k.db("w_r"), bwr, bwr)
        brr = S_("brr", [1, NR], F32, ph); bbrr = Buf("brr")
        sp.dma(lambda e: e.dma_start(out=brr[:], in_=b_r), k.db("b_r"), bbrr, bbrr)
        x1s = Ring("cx1", [128, D], F32, 2, ph)
        h2 = S_("h2", [128, D], F32, ph); bh2 = Buf("h2")
        h2bs = Ring("h2b", [128, D], BF16, 2, ph)
        h2T = S_("h2T", [128, KD, 128], F32, ph); bh2T = Buf("h2T")
        sm = S_("rsm", [128, 16], F32, ph); bsm = Buf("rsm")
        lg = S_("lg", [128, NR], F32, ph); blg = Buf("lg")
        rt = S_("rt", [128, 8, NE], F32, ph); brt = Buf("rt")
        jk = S_("cjk", [128, D], BF16, ph); bjk = Buf("cjk")
        for tt in range(NT):
            x1, bx1 = x1s.next(); h2b, bh2b = h2bs.next()
            sp.dma(lambda e: e.dma_start(out=x1[:], in_=X1_d[tt * 128:(tt + 1) * 128, :]), k.db("X1_d"), bx1, bx1, partial=False)
            act.op(lambda e: e.activation(out=jk[:], in_=x1[:], func=AF.Square, accum_out=sm[:, 0:1]), reads=[bx1], writes=[bjk, bsm])
            dve.op(lambda e: e.tensor_scalar(out=sm[:, 1:2], in0=sm[:, 0:1], scalar1=1.0 / D, scalar2=1e-6, op0=ALU.mult, op1=ALU.add), reads=[bsm], writes=[bsm])
            act.op(lambda e: e.activation(out=sm[:, 1:2], in_=sm[:, 1:2], func=AF.Ln), reads=[bsm], writes=[bsm])
            act.op(lambda e: e.activation(out=sm[:, 1:2], in_=sm[:, 1:2], func=AF.Exp, scale=-0.5), reads=[bsm], writes=[bsm])
            dve.op(lambda e: e.scalar_tensor_tensor(out=h2[:], in0=x1[:], scalar=sm[:, 1:2], in1=A2[:], op0=ALU.mult, op1=ALU.mult),
                   reads=[bx1, bsm, bA2], writes=[bh2])
            pool.op(lambda e: e.tensor_tensor(out=h2[:], in0=h2[:], in1=B2[:], op=ALU.add), reads=[bh2, bB2], writes=[bh2])
            act.op(lambda e: e.copy(out=h2b[:], in_=h2[:]), reads=[bh2], writes=[bh2b])
            act.dma(lambda e: e.dma_start(out=H2_d[tt * 128:(tt + 1) * 128, :], in_=h2b[:]), bh2b, k.db("H2_d"), bh2b)
            for g0 in range(0, KD, 4):
                n4 = min(4, KD - g0)
                ps, bps = pss.next()
                for j in range(n4):
                    kc = g0 + j
                    pe.op(lambda e: e.transpose(out=ps[:, j * 128:(j + 1) * 128], in_=h2[:, kc * 128:(kc + 1) * 128], identity=identf[:]),
                          reads=[bh2, b_identf], writes=[bps], partial=(j > 0), signal=(j == n4 - 1))
                dve.op(lambda e: e.tensor_copy(out=h2T[:, g0:g0 + n4, :], in_=ps[:, 0:n4 * 128].rearrange("p (k n) -> p k n", n=128)),
                       reads=[bps], writes=[bh2T], partial=True)
            ps, bps = pss.next()
            for kc in range(KD):
                pe.op(lambda e: e.matmul(ps[:, 0:NR], lhsT=h2T[:, kc, :], rhs=wr[:, kc, :], start=(kc == 0), stop=False),
                      reads=[bh2T, bwr], writes=[bps], partial=(kc > 0), signal=False)
            pe.op(lambda e: e.matmul(ps[:, 0:NR], lhsT=ones_f[0:1, :], rhs=brr[0:1, :], start=False, stop=True), reads=[b_ones, bbrr], writes=[bps], partial=True)
            dve.op(lambda e: e.tensor_copy(out=lg[:], in_=ps[:, 0:NR]), reads=[bps], writes=[blg])
            gl = lg[:, 0:NG]; el = lg[:, NG:NR].rearrange("p (g e) -> p g e", e=EPG)
            ohg = rt[:, 0, 0:NG]; eg = rt[:, 6, 0:NG]
            R = lambda fn, **kw: dve.op(fn, reads=[blg, brt, bsm], writes=[brt, bsm], **kw)
            R(lambda e: e.reduce_max(out=sm[:, 2:3], in_=gl, axis=AX.X))
            R(lambda e: e.tensor_scalar(out=ohg, in0=gl, scalar1=sm[:, 2:3], scalar2=None, op0=ALU.is_ge))
            R(lambda e: e.tensor_scalar(out=sm[:, 3:4], in0=sm[:, 2:3], scalar1=-1.0, scalar2=None, op0=ALU.mult))
            act.op(lambda e: e.activation(out=eg, in_=gl, func=AF.Exp, bias=sm[:, 3:4], scale=1.0, accum_out=sm[:, 4:5]), reads=[blg, bsm], writes=[brt, bsm])
            R(lambda e: e.reciprocal(out=sm[:, 5:6], in_=sm[:, 4:5]))
            tmp = rt[:, 1, :].rearrange("p (g e) -> p g e", e=EPG)
            R(lambda e: e.tensor_tensor(out=tmp, in0=el, in1=ohg.unsqueeze(2).to_broadcast([128, NG, EPG]), op=ALU.mult))
            sel = rt[:, 2, 0:EPG]; mk1 = rt[:, 3, 0:EPG]; sel2 = rt[:, 4, 0:EPG]; mk2 = rt[:, 5, 0:EPG]
            R(lambda e: e.tensor_reduce(out=sel, in_=tmp.rearrange("p g e -> p e g"), axis=AX.X, op=ALU.add))
            R(lambda e: e.reduce_max(out=sm[:, 6:7], in_=sel, axis=AX.X))
            R(lambda e: e.tensor_scalar(out=mk1, in0=sel, scalar1=sm[:, 6:7], scalar2=None, op0=ALU.is_ge))
            R(lambda e: e.scalar_tensor_tensor(out=sel2, in0=mk1, scalar=-1e30, in1=sel, op0=ALU.mult, op1=ALU.add))
            R(lambda e: e.reduce_max(out=sm[:, 7:8], in_=sel2, axis=AX.X))
            R(lambda e: e.tensor_scalar(out=mk2, in0=sel2, scalar1=sm[:, 7:8], scalar2=None, op0=ALU.is_ge))
            R(lambda e: e.tensor_tensor(out=sm[:, 8:9], in0=sm[:, 7:8], in1=sm[:, 6:7], op=ALU.subtract))
            act.op(lambda e: e.activation(out=sm[:, 9:10], in_=sm[:, 8:9], func=AF.Exp), reads=[bsm], writes=[bsm])
            R(lambda e: e.tensor_scalar(out=sm[:, 10:11], in0=sm[:, 9:10], scalar1=1.0, scalar2=None, op0=ALU.add))
            R(lambda e: e.reciprocal(out=sm[:, 10:11], in_=sm[:, 10:11]))
            dve.op(lambda e: e.tensor_tensor(out=wsel[:, tt, 0:1], in0=sm[:, 5:6], in1=sm[:, 10:11], op=ALU.mult), reads=[bsm], writes=[bwsel], partial=True)
            dve.op(lambda e: e.tensor_tensor(out=wsel[:, tt, 1:2], in0=wsel[:, tt, 0:1], in1=sm[:, 9:10], op=ALU.mult), reads=[bsm, bwsel], writes=[bwsel], partial=True)
            bo = lambda a: a.unsqueeze(2).to_broadcast([128, NG, EPG])
            be_ = lambda a: a.unsqueeze(1).to_broadcast([128, NG, EPG])
            v3 = lambda a: a.rearrange("p (g e) -> p g e", e=EPG)
            dve.op(lambda e: e.tensor_tensor(out=v3(I1_all[:, tt, :]), in0=bo(ohg), in1=be_(mk1), op=ALU.mult), reads=[brt], writes=[bI1], partial=True)
            dve.op(lambda e: e.tensor_tensor(out=v3(I2_all[:, tt, :]), in0=bo(ohg), in1=be_(mk2), op=ALU.mult), reads=[brt], writes=[bI2], partial=True)
            dve.op(lambda e: e.tensor_tensor(out=Ind_all[:, tt, :], in0=I1_all[:, tt, :], in1=I2_all[:, tt, :], op=ALU.add), reads=[bI1, bI2], writes=[bInd], partial=True)
        cnt = S_("cnt", [128, 4, NE], F32, ph); bcnt = Buf("cnt")
        ps, bps = pss.next()
        for tt in range(NT):
            pe.op(lambda e: e.matmul(ps[:, 0:NE], lhsT=ones_b[:], rhs=Ind_all[:, tt, :], start=(tt == 0), stop=(tt == NT - 1)),
                  reads=[b_onesb, bInd], writes=[bps], partial=(tt > 0), signal=(tt == NT - 1))
        C = lambda fn, **kw: dve.op(fn, reads=[bcnt], writes=[bcnt], **kw)
        dve.op(lambda e: e.tensor_copy(out=cnt[:, 0, :], in_=ps[:, 0:NE]), reads=[bps], writes=[bcnt])
        thri = S_("thri", [128, NT], I32, ph); bthri = Buf("thri")
        pool.op(lambda e: e.iota(out=thri[:], pattern=[[128, NT]], base=0, channel_multiplier=0), writes=[bthri])
        thrf = S_("thrf", [128, NT], F32, ph); bthrf = Buf("thrf")
        dve.op(lambda e: e.tensor_copy(out=thrf[:], in_=thri[:]), reads=[bthri], writes=[bthrf])
        cmpc = S_("cmpc", [128, NE, NT], F32, ph); bcmpc = Buf("cmpc")
        dve.op(lambda e: e.tensor_tensor(out=cmpc[:], in0=cnt[:, 0, :].unsqueeze(2).to_broadcast([128, NE, NT]),
                                         in1=thrf[:].unsqueeze(1).to_broadcast([128, NE, NT]), op=ALU.is_gt), reads=[bcnt, bthrf], writes=[bcmpc])
        dve.op(lambda e: e.reduce_sum(out=cnt[:, 1, :], in_=cmpc[:], axis=AX.X), reads=[bcmpc], writes=[bcnt])
        C(lambda e: e.tensor_scalar(out=cnt[:, 1, :], in0=cnt[:, 1, :], scalar1=128.0, scalar2=None, op0=ALU.mult))
        ps, bps = pss.next()
        pe.op(lambda e: e.transpose(out=ps[0:NE, 0:128], in_=cnt[:, 1, :], identity=identf[:]), reads=[bcnt, b_identf], writes=[bps])
        pcT = S_("pcT", [128, 128], F32, ph); bpcT = Buf("pcT")
        dve.op(lambda e: e.tensor_copy(out=pcT[0:NE, :], in_=ps[0:NE, 0:128]), reads=[bps], writes=[bpcT])
        ps, bps = pss.next()
        pe.op(lambda e: e.matmul(ps[:, 0:NE], lhsT=pcT[0:NE, :], rhs=Uf[0:NE, 0:NE], start=True, stop=True), reads=[bpcT, bUf], writes=[bps])
        dve.op(lambda e: e.tensor_copy(out=cnt[:, 2, :], in_=ps[:, 0:NE]), reads=[bps], writes=[bcnt])
        C(lambda e: e.tensor_tensor(out=cnt[:, 3, :], in0=cnt[:, 2, :], in1=cnt[:, 1, :], op=ALU.add))
        posf = S_("posf", [128, NT, 2], F32, ph); bposf = Buf("posf")
        pz = S_("pz", [128, 2, NE], F32, ph); bpz = Buf("pz")
        for tt in range(NT):
            ps, bps = pss.next()
            for t2 in range(tt):
                pe.op(lambda e: e.matmul(ps[:, 0:NE], lhsT=ones_b[:], rhs=Ind_all[:, t2, :], start=(t2 == 0), stop=False),
                      reads=[b_onesb, bInd], writes=[bps], partial=(t2 > 0), signal=False)
            pe.op(lambda e: e.matmul(ps[:, 0:NE], lhsT=Ub[:], rhs=Ind_all[:, tt, :], start=(tt == 0), stop=True), reads=[bUb, bInd], writes=[bps], partial=(tt > 0))
            dve.op(lambda e: e.tensor_tensor(out=pz[:, 0, :], in0=ps[:, 0:NE], in1=cnt[:, 2, :], op=ALU.add), reads=[bps, bcnt], writes=[bpz])
            dve.op(lambda e: e.tensor_tensor(out=pz[:, 1, :], in0=pz[:, 0, :], in1=I1_all[:, tt, :], op=ALU.mult), reads=[bpz, bI1], writes=[bpz])
            dve.op(lambda e: e.reduce_sum(out=posf[:, tt, 0:1], in_=pz[:, 1, :], axis=AX.X), reads=[bpz], writes=[bposf], partial=True)
            dve.op(lambda e: e.tensor_tensor(out=pz[:, 1, :], in0=pz[:, 0, :], in1=I2_all[:, tt, :], op=ALU.mult), reads=[bpz, bI2], writes=[bpz])
            dve.op(lambda e: e.reduce_sum(out=posf[:, tt, 1:2], in_=pz[:, 1, :], axis=AX.X), reads=[bpz], writes=[bposf], partial=True)
        dve.op(lambda e: e.tensor_copy(out=idx_all[:], in_=posf[:]), reads=[bposf], writes=[bidx])
        bsi = S_("bsi", [128, NBLK], I32, ph); bbsi = Buf("bsi")
        pool.op(lambda e: e.iota(out=bsi[:], pattern=[[128, NBLK]], base=0, channel_multiplier=0), writes=[bbsi])
        bsf = S_("bsf", [128, NBLK], F32, ph); bbsf = Buf("bsf")
        dve.op(lambda e: e.tensor_copy(out=bsf[:], in_=bsi[:]), reads=[bbsi], writes=[bbsf])
        pidi = S_("pidi", [128, 1], I32, ph); bpidi = Buf("pidi")
        pool.op(lambda e: e.iota(out=pidi[:], pattern=[[0, 1]], base=0, channel_multiplier=1), writes=[bpidi])
        pidf = S_("pidf", [128, 1], F32, ph); bpidf = Buf("pidf")
        dve.op(lambda e: e.tensor_copy(out=pidf[:], in_=pidi[:]), reads=[bpidi], writes=[bpidf])
        cmp_ = S_("cmp", [128, NBLK, NE], F32, ph); bcmp = Buf("cmp")
        dve.op(lambda e: e.tensor_tensor(out=cmp_[:], in0=cnt[:, 3, :].unsqueeze(1).to_broadcast([128, NBLK, NE]),
                                         in1=bsf[:].unsqueeze(2).to_broadcast([128, NBLK, NE]), op=ALU.is_le), reads=[bcnt, bbsf], writes=[bcmp])
        bef = S_("bef", [128, NBLK], F32, ph); bbef = Buf("bef")
        dve.op(lambda e: e.reduce_sum(out=bef[:], in_=cmp_[:], axis=AX.X), reads=[bcmp], writes=[bbef])
        dve.op(lambda e: e.tensor_scalar(out=bef[:], in0=bef[:], scalar1=float(NE - 1), scalar2=None, op0=ALU.min), reads=[bbef], writes=[bbef])
        wq = S_("wq", [128, 3, NBLK], F32, ph); bwq = Buf("wq")
        nsame = S_("nsame", [128, NBLK], F32, ph); bns = Buf("nsame")
        dve.op(lambda e: e.memset(nsame[:], 1.0), writes=[bns])
        dve.op(lambda e: e.tensor_tensor(out=nsame[:, 1:NBLK], in0=bef[:, 1:NBLK], in1=bef[:, 0:NBLK - 1], op=ALU.not_equal), reads=[bbef], writes=[bns])
        for q in range(NP):
            dve.op(lambda e: e.tensor_scalar(out=wq[:, 0, :], in0=bef[:], scalar1=float(q * EPP), scalar2=None, op0=ALU.is_ge), reads=[bbef], writes=[bwq])
            dve.op(lambda e: e.tensor_scalar(out=wq[:, 1, :], in0=bef[:], scalar1=float((q + 1) * EPP), scalar2=None, op0=ALU.is_lt), reads=[bbef], writes=[bwq], partial=True)
            dve.op(lambda e: e.tensor_tensor(out=wq[:, 0, :], in0=wq[:, 0, :], in1=wq[:, 1, :], op=ALU.mult), reads=[bwq], writes=[bwq])
            dve.op(lambda e: e.tensor_tensor(out=wq[:, 0, :], in0=wq[:, 0, :], in1=nsame[:], op=ALU.mult), reads=[bwq, bns], writes=[bwq])
            dve.op(lambda e: e.tensor_scalar(out=wq[:, 1, :], in0=bef[:], scalar1=128.0, scalar2=pidf[:, 0:1], op0=ALU.mult, op1=ALU.add),
                   reads=[bbef, bpidf, bwq], writes=[bwq])
            dve.op(lambda e: e.tensor_scalar(out=wq[:, 1, :], in0=wq[:, 1, :], scalar1=-float(q * EPP * 128) - 1.0e6, scalar2=None, op0=ALU.add), reads=[bwq], writes=[bwq])
            dve.op(lambda e: e.tensor_tensor(out=wq[:, 1, :], in0=wq[:, 1, :], in1=wq[:, 0, :], op=ALU.mult), reads=[bwq], writes=[bwq])
            dve.op(lambda e: e.tensor_scalar(out=wq[:, 1, :], in0=wq[:, 1, :], scalar1=1.0e6, scalar2=None, op0=ALU.add), reads=[bwq], writes=[bwq])
            for si in range(NSP_):
                dve.op(lambda e: e.tensor_scalar(out=wq[:, 2, :], in0=wq[:, 1, :], scalar1=float(NSP_), scalar2=float(si), op0=ALU.mult, op1=ALU.add),
                       reads=[bwq], writes=[bwq])
                dve.op(lambda e: e.tensor_copy(out=widx_i[:, q * NSP_ + si, :], in_=wq[:, 2, :]), reads=[bwq], writes=[bwidx], partial=(q + si > 0))
        if "dbg_route" in debug_outs:
            for nm, t_, b_, shp, dt_ in (("dbg_idx", idx_all, bidx, [128, NT * 2], I32), ("dbg_widx", widx_i, bwidx, [128, 4 * NSP_ * NBLK], I32),
                                         ("dbg_wsel", wsel, bwsel, [128, NT * 2], F32)):
                o_ = nc.dram_tensor(nm, shp, dt_, kind="ExternalOutput").ap(); k.dbufs[nm] = Buf(nm)
                src_ = t_[:].rearrange("p a b -> p (a b)") if len(t_.shape) == 3 else t_[:]
                sp.dma(lambda e: e.dma_start(out=o_, in_=src_), b_, k.db(nm), b_)
        sc.barrier()
    if upto <= 9:
        return

    NSP = max(1, (KD * DE * 4 + 32767) // 32768)
    assert NSP == max(1, (HC * D * 4 + 32767) // 32768) and KD % NSP == 0 and HC % NSP == 0
    bnd_reg = nc.gpsimd.to_reg(EPP * 128 * NSP - 1)
    w1v = [w.rearrange("(r c) f -> r (c f)", c=KD // NSP) for w in w1p]; w3v = [w.rearrange("(r c) f -> r (c f)", c=KD // NSP) for w in w3p]
    w2v = [w.rearrange("(r c) f -> r (c f)", c=HC // NSP) for w in w2p]
    with ExitStack() as ph:
        zt = S_("zt", [128, D], BF16, ph); bzt = Buf("zt")
        dve.op(lambda e: e.memset(zt[:], 0.0), writes=[bzt])
        for b in range(NBLK):
            sp.dma(lambda e: e.dma_start(out=Xs_d[b * 128:(b + 1) * 128, :], in_=zt[:]), bzt, k.db("Xs_d"), bzt)
        hbs = Ring("sh2", [128, D], BF16, 2, ph)
        for tt in range(NT):
            hb, bhb = hbs.next()
            sp.dma(lambda e: e.dma_start(out=hb[:], in_=H2_d[tt * 128:(tt + 1) * 128, :]), k.db("H2_d"), bhb, bhb, partial=False)
            for kk in range(2):
                pool.wait(dict(bidx.w));
                pool.dma(lambda e: e.indirect_dma_start(out=Xs_d[:, :], out_offset=bass.IndirectOffsetOnAxis(ap=idx_all[:, tt, kk:kk + 1], axis=0),
                                                        in_=hb[:, :], in_offset=None), bhb, k.db("Xs_d"), bhb, partial=False)
        w1s = S_("w1s", [128, KD * DE], BF16, ph); bw1 = Buf("w1s")
        w3s = S_("w3s", [128, KD * DE], BF16, ph); bw3 = Buf("w3s")
        w2s = S_("w2s", [128, HC * D], BF16, ph); bw2 = Buf("w2s")
        xbs = Ring("xb", [128, D], BF16, 2, ph); xts = Ring("xbT", [128, KD, 128], BF16, 2, ph)
        hid = S_("hidT", [128, HC, 128], BF16, ph); bhid = Buf("hidT")
        sl = Ring("sil", [128, 128], F32, 2, ph)
        yb = S_("yb", [128, D], F32, ph); byb = Buf("yb")
        pst = Ring("bpt", [128, 1024], BF16, 2, ph, psum=True)
        pss = Ring("bps", [128, 512], F32, 4, ph, psum=True)
        for b in range(NBLK):
            pool.wait(dict(bwidx.w))
            for (ws, bw, wv, nm, rowlen) in ((w1s, bw1, w1v, "w1", KD * DE), (w3s, bw3, w3v, "w3", KD * DE), (w2s, bw2, w2v, "w2", HC * D)):
                cw = rowlen // NSP
                for q in range(NP):
                    for si in range(NSP):
                        pool.dma(lambda e: e.indirect_dma_start(out=ws[:, si * cw:(si + 1) * cw], out_offset=None, in_=wv[q][:, :],
                                                                in_offset=bass.IndirectOffsetOnAxis(ap=widx_i[:, q * NSP + si, b:b + 1], axis=0),
                                                                bounds_check=bnd_reg, oob_is_err=False),
                                 k.db("%s_%d" % (nm, q)), bw, bw, partial=(q + si > 0))
            xb, bxb = xbs.next(); xT, bxT = xts.next()
            sp.dma(lambda e: e.dma_start(out=xb[:], in_=Xs_d[b * 128:(b + 1) * 128, :]), k.db("Xs_d"), bxb, bxb, partial=False)
            for g0 in range(0, KD, 8):
                n8 = min(8, KD - g0)
                ps, bps = pst.next()
                for j in range(n8):
                    cc = g0 + j
                    pe.op(lambda e: e.transpose(out=ps[:, j * 128:(j + 1) * 128], in_=xb[:, cc:D:KD], identity=ident[:]),
                          reads=[bxb, b_ident], writes=[bps], partial=(j > 0), signal=(j == n8 - 1))
                dve.op(lambda e: e.tensor_copy(out=xT[:, g0:g0 + n8, :], in_=ps[:, 0:n8 * 128].rearrange("p (k n) -> p k n", n=128)),
                       reads=[bps], writes=[bxT], partial=True)
            for c2 in range(HC):
                p1, bp1 = pss.next(); p3, bp3 = pss.next()
                for (pp, bpp, ws, bw) in ((p1, bp1, w1s, bw1), (p3, bp3, w3s, bw3)):
                    for cc in range(KD):
                        pe.op(lambda e: e.matmul(pp[:, 0:128], lhsT=ws[:, cc * DE + c2:(cc + 1) * DE:HC], rhs=xT[:, cc, :], start=(cc == 0), stop=(cc == KD - 1)),
                              reads=[bw, bxT], writes=[bpp], partial=(cc > 0), signal=(cc == KD - 1))
                s_, bs_ = sl.next()
                act.op(lambda e: e.activation(out=s_[:], in_=p1[:, 0:128], func=AF.Silu), reads=[bp1], writes=[bs_])
                dve.op(lambda e: e.tensor_tensor(out=hid[:, c2, :], in0=p3[:, 0:128], in1=s_[:], op=ALU.mult), reads=[bp3, bs_], writes=[bhid], partial=(c2 > 0))
            for gi, g0 in enumerate(range(0, D, 512)):
                gw = min(512, D - g0)
                ps, bps = pss.next()
                for c2 in range(HC):
                    pe.op(lambda e: e.matmul(ps[:, 0:gw], lhsT=hid[:, c2, :], rhs=w2s[:, c2 * D + g0:c2 * D + g0 + gw], start=(c2 == 0), stop=(c2 == HC - 1)),
                          reads=[bhid, bw2], writes=[bps], partial=(c2 > 0), signal=(c2 == HC - 1))
                if gi % 2 == 0:
                    dve.op(lambda e: e.tensor_copy(out=yb[:, g0:g0 + gw], in_=ps[:, 0:gw]), reads=[bps], writes=[byb], partial=(gi > 0))
                else:
                    act.op(lambda e: e.copy(out=yb[:, g0:g0 + gw], in_=ps[:, 0:gw]), reads=[bps], writes=[byb], partial=True)
            act.dma(lambda e: e.dma_start(out=Ys_d[b * 128:(b + 1) * 128, :], in_=yb[:]), byb, k.db("Ys_d"), byb)
        sc.barrier()
    if upto <= 10:
        return

    with ExitStack() as ph:
        pss = Ring("fps", [128, 512], F32, 2, ph, psum=True)
        gt2 = S_("gt2", [128, D], F32, ph); bgt2 = Buf("gt2"); gfb = S_("gfb", [128, D], F32, ph); bgfb = Buf("gfb")
        sp.dma(lambda e: e.dma_start(out=gt2[:], in_=mod_bc[:, 3 * D:4 * D]), k.db("mod_bc"), bgt2, bgt2)
        bcast_row(g_final, "g_final", gfb, bgfb, ph, pss)
        y0 = S_("y0", [128, D], F32, ph); by0 = Buf("y0"); y1 = S_("y1", [128, D], F32, ph); by1 = Buf("y1")
        x1s = Ring("fx1", [128, D], F32, 2, ph)
        fo = S_("fo", [128, D], F32, ph); bfo = Buf("fo")
        fjk = S_("fjk", [128, D], BF16, ph); bfjk = Buf("fjk")
        fs = S_("fs", [128, 2], F32, ph); bfs = Buf("fs")
        for tt in range(NT):
            x1, bx1 = x1s.next()
            sp.dma(lambda e: e.dma_start(out=x1[:], in_=X1_d[tt * 128:(tt + 1) * 128, :]), k.db("X1_d"), bx1, bx1, partial=False)
            pool.wait(dict(bidx.w))
            pool.dma(lambda e: e.indirect_dma_start(out=y0[:, :], out_offset=None, in_=Ys_d[:, :],
                                                    in_offset=bass.IndirectOffsetOnAxis(ap=idx_all[:, tt, 0:1], axis=0)), k.db("Ys_d"), by0, by0, partial=False)
            pool.dma(lambda e: e.indirect_dma_start(out=y1[:, :], out_offset=None, in_=Ys_d[:, :],
                                                    in_offset=bass.IndirectOffsetOnAxis(ap=idx_all[:, tt, 1:2], axis=0)), k.db("Ys_d"), by1, by1, partial=False)
            dve.op(lambda e: e.tensor_scalar(out=y0[:], in0=y0[:], scalar1=wsel[:, tt, 0:1], scalar2=None, op0=ALU.mult), reads=[by0, bwsel], writes=[by0])
            dve.op(lambda e: e.scalar_tensor_tensor(out=y0[:], in0=y1[:], scalar=wsel[:, tt, 1:2], in1=y0[:], op0=ALU.mult, op1=ALU.add),
                   reads=[by1, by0, bwsel], writes=[by0])
            pool.op(lambda e: e.tensor_tensor(out=y0[:], in0=y0[:], in1=gt2[:], op=ALU.mult), reads=[by0, bgt2], writes=[by0])
            dve.op(lambda e: e.tensor_tensor(out=fo[:], in0=y0[:], in1=x1[:], op=ALU.add), reads=[by0, bx1], writes=[bfo])
            act.op(lambda e: e.activation(out=fjk[:], in_=fo[:], func=AF.Square, accum_out=fs[:, 0:1]), reads=[bfo], writes=[bfjk, bfs])
            dve.op(lambda e: e.tensor_scalar(out=fs[:, 1:2], in0=fs[:, 0:1], scalar1=1.0 / D, scalar2=1e-6, op0=ALU.mult, op1=ALU.add), reads=[bfs], writes=[bfs])
            act.op(lambda e: e.activation(out=fs[:, 1:2], in_=fs[:, 1:2], func=AF.Ln), reads=[bfs], writes=[bfs])
            act.op(lambda e: e.activation(out=fs[:, 1:2], in_=fs[:, 1:2], func=AF.Exp, scale=-0.5), reads=[bfs], writes=[bfs])
            dve.op(lambda e: e.scalar_tensor_tensor(out=fo[:], in0=fo[:], scalar=fs[:, 1:2], in1=gfb[:], op0=ALU.mult, op1=ALU.mult),
                   reads=[bfo, bfs, bgfb], writes=[bfo])
            sp.dma(lambda e: e.dma_start(out=out[tt * 128:(tt + 1) * 128, :], in_=fo[:]), bfo, k.db("out"), bfo)
        sc.barrier()


def rope_tables(cfg, half):
    c = cfg
    pos = np.concatenate([np.arange(c.NQ) + half * c.NQ, np.arange(c.NQ) + (1 - half) * c.NQ])
    row = (pos // c.GW).astype(np.float64); col = (pos % c.GW).astype(np.float64)
    def tab(rot):
        q = rot // 4
        inv = 10000.0 ** (-np.arange(q, dtype=np.float64) / q)
        ang = np.concatenate([row[:, None] * inv, col[:, None] * inv], axis=-1)
        hd = rot // 2
        cs = np.ones((rot, c.NK), np.float32); sn = np.zeros((rot, c.NK), np.float32)
        cs[:hd, :c.S] = np.cos(ang).T; cs[hd:, :c.S] = np.cos(ang).T
        sn[:hd, :c.S] = -np.sin(ang).T; sn[hd:, :c.S] = np.sin(ang).T
        return cs, sn
    cd, sd = tab(128); cm, sm = tab(64)
    return dict(cosd=cd, sind=sd, cosm=cm, sinm=sm)


def weight_pieces(c, inp):
    g = lambda n: np.asarray(inp[n][0]).reshape(-1, np.asarray(inp[n]).shape[-1])
    out = {}
    wm = g("w_mod")
    out["w_mod_0"] = wm[:, :3 * c.D]; out["w_mod_1"] = wm[:, 3 * c.D:]
    for n in ("w_in", "w_uq", "w_ukv", "w_br_a", "w_br_b", "w_out"):
        out[n] = g(n)
    epp = c.NE // 4
    for n, rpe in (("w1", c.D), ("w3", c.D), ("w2", c.DE)):
        w = g(n)
        for q in range(4):
            out["%s_%d" % (n, q)] = w[q * epp * rpe:(q + 1) * epp * rpe]
    return out


SHARD_POS = {0: 0, 4: 1, 1: 2, 5: 3, 2: 4, 6: 5, 3: 6, 7: 7}


GATHER_CHUNK_BYTES = 512 * 1024


def gather_unit(rows, cols):
    r8 = rows // 8
    u = max(1, min(r8, (GATHER_CHUNK_BYTES) // (cols * 4)))
    while r8 % u:
        u -= 1
    return u


def prep_core_inputs(cfg, inp, core, gather):
    c = cfg
    b, half = (core // 2, core % 2) if c.NCORES > 1 else (0, 0)
    f = lambda a: np.ascontiguousarray(np.asarray(a, dtype=np.float32))
    x = np.asarray(inp["x"][b]); ctx = np.asarray(inp["ctx"][b])
    own = x[half * c.NQ:(half + 1) * c.NQ]; oth = x[(1 - half) * c.NQ:(2 - half) * c.NQ]
    m = dict(xk=f(np.concatenate([own, oth, ctx], axis=0)), c_b=f(inp["c"][b:b + 1]), c_ctx=f(np.asarray(inp["c_ctx"])[None, :]))
    m.update(rope_tables(c, half))
    for n in ("b_mod", "g_attn", "g_q", "g_kv", "g_subln", "g_ffn", "lam_q1", "lam_k1", "lam_q2", "lam_k2"):
        m[n] = f(np.asarray(inp[n]).reshape(1, -1))
    m["g_final"] = f(np.asarray(inp["g_final"]).reshape(1, -1))
    m["w_r"] = f(np.concatenate([np.asarray(inp["w_grp"][0]), np.asarray(inp["w_exp"][0])], axis=1))
    m["b_r"] = f(np.concatenate([np.asarray(inp["b_grp"][0]), np.asarray(inp["b_exp"][0])])[None, :])
    for n, w2 in weight_pieces(c, inp).items():
        if gather:
            rows, cols = w2.shape
            u = gather_unit(rows, cols); nch = rows // 8 // u
            t_ = SHARD_POS[core]
            m[n + "_sh"] = f(w2.reshape(nch, 8, u, cols)[:, t_].reshape(nch * u, cols))
        else:
            m[n] = f(w2)
    return m


_NC_CACHE = {}


def kernel(**inputs):
    cfg = Cfg()
    n = cfg.NCORES
    if "nc" not in _NC_CACHE:
        _NC_CACHE["nc"] = build_program(cfg, gather=True)
    nc = _NC_CACHE["nc"]
    need = set()
    for a in nc.allocations:
        try:
            if a.kind == "ExternalInput":
                need.add(a.memorylocations[0].name)
        except Exception:
            pass
    in_maps = []
    for core in range(n):
        m = prep_core_inputs(cfg, inputs, core, gather=True)
        in_maps.append({k_: v for k_, v in m.items() if k_ in need})
    res = run_bass_kernel_spmd(nc, in_maps, core_ids=list(range(n)))
    out = np.empty((cfg.B, cfg.S, cfg.D), np.float32)
    for core in range(n):
        b, half = core // 2, core % 2
        out[b, half * cfg.NQ:(half + 1) * cfg.NQ] = res.results[core]["out"]
    return out
```
